# Optimizing a Trainium2 kernel written in Bass

```python
import math
import jax, jax.numpy as jnp
from jax import lax
import numpy as np

D_MODEL = 1024
BATCH = 1
SEQ = 16384
DEPTH = 1

MIX_WIDTH = D_MODEL
HG_HEADS = 4
HG_VAL_DIM = (MIX_WIDTH // 2) // HG_HEADS
HG_KEY_DIM = HG_VAL_DIM
HG_CHUNK = 64
DA_HEADS = 4
DA_V_DIM = (MIX_WIDTH // 2) // DA_HEADS
DA_QK_DIM = DA_V_DIM // 2
Q_BLOCK = 128
ROPE_THETA = 500000.0
ROT_DIM = DA_QK_DIM // 4
N_GROUPS = 4
EXPERTS_PER_GROUP = 8
N_EXPERTS = N_GROUPS * EXPERTS_PER_GROUP
TOP_K_IN_GROUP = 2
D_EXPERT = D_MODEL // 2
NORM_EPS = 1e-6
SUBLN_EPS = 1e-5

IN_SPLITS = (HG_HEADS * HG_KEY_DIM, HG_HEADS * HG_KEY_DIM, HG_HEADS * HG_KEY_DIM,
             HG_HEADS * HG_VAL_DIM, HG_HEADS * HG_VAL_DIM,
             DA_HEADS * 2 * DA_QK_DIM, DA_HEADS * 2 * DA_QK_DIM, DA_HEADS * DA_V_DIM)
IN_WIDTH = sum(IN_SPLITS)

kernel_name = 'hybrid_hgrn2_diffattn_hmoe_encoder'


def rms_norm(x, gain, eps=NORM_EPS):
    xf = x.astype(jnp.float32)
    y = xf * lax.rsqrt(jnp.mean(xf * xf, axis=-1, keepdims=True) + eps)
    return (y * gain.astype(jnp.float32)).astype(x.dtype)


def partial_rotary(t, positions):
    half = ROT_DIM // 2
    inv_freq = jnp.float32(ROPE_THETA) ** (-jnp.arange(0, ROT_DIM, 2, dtype=jnp.float32) / ROT_DIM)
    ang = positions.astype(jnp.float32)[:, :, None, None] * inv_freq
    cos = jnp.cos(ang).astype(t.dtype)
    sin = jnp.sin(ang).astype(t.dtype)
    t1 = t[..., :half]
    t2 = t[..., half:ROT_DIM]
    return jnp.concatenate([t1 * cos - t2 * sin, t2 * cos + t1 * sin, t[..., ROT_DIM:]], axis=-1)


def hgrn2_chunkwise(q, k, v, log_f):
    G, S, K = q.shape
    V = v.shape[-1]
    n_chunks = S // HG_CHUNK

    def split(a):
        return jnp.moveaxis(a.reshape(G, n_chunks, HG_CHUNK, a.shape[-1]), 1, 0)

    tri = jnp.tril(jnp.ones((HG_CHUNK, HG_CHUNK), dtype=bool))[None, :, :, None]

    def step(state, blk):
        qc, kc, vc, lfc = blk
        b = jnp.cumsum(lfc, axis=1)
        o_inter = jnp.einsum('gck,gkv->gcv', qc * jnp.exp(b), state)
        rel = jnp.where(tri, b[:, :, None, :] - b[:, None, :, :], -jnp.inf)
        scores = jnp.einsum('gtk,gsk,gtsk->gts', qc, kc, jnp.exp(rel))
        o_intra = jnp.einsum('gts,gsv->gtv', scores, vc)
        b_last = b[:, -1]
        new_state = jnp.exp(b_last)[:, :, None] * state + jnp.einsum(
            'gsk,gsv->gkv', kc * jnp.exp(b_last[:, None, :] - b), vc)
        return new_state, o_inter + o_intra

    init = jnp.zeros((G, K, V), jnp.float32)
    _, o = lax.scan(step, init, (split(q), split(k), split(v), split(log_f)))
    return jnp.moveaxis(o, 0, 1).reshape(G, S, V)


def hgrn2_bidirectional(q, i, f_fwd, f_bwd):
    B, S, H, _ = q.shape

    def flip(a):
        return a[:, ::-1]

    def to_groups(a):
        return jnp.transpose(a, (0, 1, 3, 2, 4)).reshape(2 * B * H, S, a.shape[-1])

    f2 = jnp.stack([f_fwd, flip(f_bwd)]).astype(jnp.float32)
    q2 = to_groups(jnp.stack([q, flip(q)]).astype(jnp.float32))
    v2 = to_groups(jnp.stack([i, flip(i)]).astype(jnp.float32))
    o = hgrn2_chunkwise(q2, to_groups(1.0 - f2), v2, to_groups(jnp.log(f2)))
    o = jnp.transpose(o.reshape(2, B, H, S, -1), (0, 1, 3, 2, 4))
    return o[0] + flip(o[1])


def diff_attention(q, k, v, lam):
    B, S, H, _, d = q.shape
    n_blocks = S // Q_BLOCK
    qb = jnp.moveaxis(q.reshape(B, n_blocks, Q_BLOCK, H, 2, d), 1, 0)
    kf = k.astype(jnp.float32)
    vf = v.astype(jnp.float32)
    scale = d ** -0.5

    def block(q_blk):
        s = jnp.einsum('bqhcd,bkhcd->bhcqk', q_blk.astype(jnp.float32), kf) * scale
        p = jax.nn.softmax(s, axis=-1)
        a = p[:, :, 0] - lam * p[:, :, 1]
        return jnp.einsum('bhqk,bkhv->bqhv', a, vf)

    o = lax.map(block, qb)
    return jnp.moveaxis(o, 0, 1).reshape(B, S, H, -1)


def hierarchical_moe(h, rg_w, rg_b, re_w, re_b, w_gate, w_up, w_down):
    B, S, D = h.shape
    hf = h.reshape(B * S, D)
    group_prob = jax.nn.softmax((hf @ rg_w + rg_b).astype(jnp.float32), axis=-1)
    g_idx = jnp.argmax(group_prob, axis=-1)
    g_p = jnp.take_along_axis(group_prob, g_idx[:, None], axis=-1)
    exp_logits = (hf @ re_w + re_b).astype(jnp.float32).reshape(-1, N_GROUPS, EXPERTS_PER_GROUP)
    in_group = jnp.take_along_axis(exp_logits, g_idx[:, None, None], axis=1)[:, 0]
    top_p, top_e = lax.top_k(jax.nn.softmax(in_group, axis=-1), TOP_K_IN_GROUP)
    weights = g_p * top_p / jnp.sum(top_p, axis=-1, keepdims=True)
    expert_id = g_idx[:, None] * EXPERTS_PER_GROUP + top_e
    gates = jnp.sum(jax.nn.one_hot(expert_id, N_EXPERTS, dtype=jnp.float32) * weights[..., None], axis=1)
    y = jnp.zeros(hf.shape, jnp.float32)
    for e in range(N_EXPERTS):
        a = jax.nn.silu(hf @ w_gate[e]) * (hf @ w_up[e])
        y = y + gates[:, e:e + 1] * (a @ w_down[e]).astype(jnp.float32)
    return y.reshape(B, S, D).astype(h.dtype)


def setup_inputs(seed: int = 0) -> dict:
    key = jax.random.key(seed)
    ks = jax.random.split(key, 20)

    def nrm(k, shape, scale):
        return jax.random.normal(k, shape, jnp.float32) * scale

    return {
        'x': nrm(ks[0], (BATCH, SEQ, D_MODEL), 1.0),
        'positions': jnp.broadcast_to(jnp.arange(SEQ, dtype=jnp.int32), (BATCH, SEQ)),
        'norm1_gain': 1.0 + nrm(ks[1], (DEPTH, D_MODEL), 0.02),
        'w_in': nrm(ks[2], (DEPTH, D_MODEL, IN_WIDTH), D_MODEL ** -0.5),
        'hg_lower_bounds': 1.0 + nrm(ks[3], (2, DEPTH + 1, HG_HEADS * HG_KEY_DIM), 0.1),
        'hg_norm_gain': 1.0 + nrm(ks[4], (DEPTH, HG_VAL_DIM), 0.02),
        'diff_lambda': nrm(ks[5], (DEPTH, 4, DA_QK_DIM), 0.1),
        'diff_subln_gain': 1.0 + nrm(ks[6], (DEPTH, DA_V_DIM), 0.02),
        'w_out': nrm(ks[7], (DEPTH, MIX_WIDTH, D_MODEL), MIX_WIDTH ** -0.5),
        'norm2_gain': 1.0 + nrm(ks[8], (DEPTH, D_MODEL), 0.02),
        'router_group_w': nrm(ks[9], (DEPTH, D_MODEL, N_GROUPS), D_MODEL ** -0.5),
        'router_group_b': nrm(ks[10], (DEPTH, N_GROUPS), 0.01),
        'router_expert_w': nrm(ks[11], (DEPTH, D_MODEL, N_EXPERTS), D_MODEL ** -0.5),
        'router_expert_b': nrm(ks[12], (DEPTH, N_EXPERTS), 0.01),
        'moe_w_gate': nrm(ks[13], (DEPTH, N_EXPERTS, D_MODEL, D_EXPERT), D_MODEL ** -0.5),
        'moe_w_up': nrm(ks[14], (DEPTH, N_EXPERTS, D_MODEL, D_EXPERT), D_MODEL ** -0.5),
        'moe_w_down': nrm(ks[15], (DEPTH, N_EXPERTS, D_EXPERT, D_MODEL), D_EXPERT ** -0.5),
        'final_norm_gain': 1.0 + nrm(ks[16], (D_MODEL,), 0.02),
    }


def reference(x, positions, norm1_gain, w_in, hg_lower_bounds, hg_norm_gain, diff_lambda,
              diff_subln_gain, w_out, norm2_gain, router_group_w, router_group_b,
              router_expert_w, router_expert_b, moe_w_gate, moe_w_up, moe_w_down,
              final_norm_gain):
    B, S, _ = x.shape
    split_at = np.cumsum(IN_SPLITS)[:-1].tolist()
    lb_cum = jnp.cumsum(jax.nn.softmax(hg_lower_bounds.astype(jnp.float32), axis=1), axis=1)

    def heads(a, n):
        return a.reshape(B, S, n, -1)

    for layer in range(DEPTH):
        h = rms_norm(x, norm1_gain[layer])
        proj = h @ w_in[layer]
        hq, hff, hfb, hi, hg, dq, dk, dv = jnp.split(proj, split_at, axis=-1)

        lb = lb_cum[:, layer + 1] - lb_cum[:, 0]
        f_fwd = lb[0] + (1.0 - lb[0]) * jax.nn.sigmoid(hff.astype(jnp.float32))
        f_bwd = lb[1] + (1.0 - lb[1]) * jax.nn.sigmoid(hfb.astype(jnp.float32))
        o_hg = hgrn2_bidirectional(heads(hq, HG_HEADS), heads(hi, HG_HEADS),
                                   heads(f_fwd, HG_HEADS), heads(f_bwd, HG_HEADS))
        o_hg = (rms_norm(o_hg, hg_norm_gain[layer])
                * jax.nn.silu(heads(hg, HG_HEADS).astype(jnp.float32))).reshape(B, S, -1)

        qd = partial_rotary(heads(dq, 2 * DA_HEADS), positions).reshape(B, S, DA_HEADS, 2, DA_QK_DIM)
        kd = partial_rotary(heads(dk, 2 * DA_HEADS), positions).reshape(B, S, DA_HEADS, 2, DA_QK_DIM)
        lam_p = diff_lambda[layer].astype(jnp.float32)
        lambda_init = 0.8 - 0.6 * math.exp(-0.3 * layer)
        lam = (jnp.exp(jnp.sum(lam_p[0] * lam_p[1])) - jnp.exp(jnp.sum(lam_p[2] * lam_p[3]))
               + lambda_init)
        o_da = diff_attention(qd, kd, heads(dv, DA_HEADS), lam)
        o_da = (rms_norm(o_da, diff_subln_gain[layer], SUBLN_EPS) * (1.0 - lambda_init)).reshape(B, S, -1)

        mixed = jnp.concatenate([o_hg, o_da], axis=-1).astype(x.dtype)
        x = x + mixed @ w_out[layer]

        h = rms_norm(x, norm2_gain[layer])
        x = x + hierarchical_moe(h, router_group_w[layer], router_group_b[layer],
                                 router_expert_w[layer], router_expert_b[layer],
                                 moe_w_gate[layer], moe_w_up[layer], moe_w_down[layer])

    return rms_norm(x, final_norm_gain)
```

```python
import math
from contextlib import ExitStack

import numpy as np
import concourse.bass as bass
import concourse.mybir as mybir
from concourse.bass_utils import run_bass_kernel_spmd

F32 = mybir.dt.float32
BF16 = mybir.dt.bfloat16
I32 = mybir.dt.int32
ALU = mybir.AluOpType
AF = mybir.ActivationFunctionType
AX = mybir.AxisListType

D = 1024
NCORES = 8
ROPE_THETA = 500000.0
N_EXPERTS = 32
D_EXPERT = 512
LAMBDA_INIT = 0.8 - 0.6 * math.exp(0.0)
TWO_PI = float(2.0 * np.pi)
PI = float(np.pi)
SEM_ROT = 20000


class Buf:
    __slots__ = ("name", "w", "rs", "excl")

    def __init__(self, name="", excl=False):
        self.name = name
        self.w = None
        self.rs = []
        self.excl = excl


class Prog:
    ENGS = ["tensor", "vector", "scalar", "gpsimd", "sync"]

    def __init__(self, nc, stack):
        self.nc = nc
        self.stack = stack
        self.ops = {e: [] for e in self.ENGS}
        self.cur = {}
        self.allsems = []
        self.nsem = 0
        for e in self.ENGS:
            self._new_sem(e)
        self.waited = {e: {} for e in self.ENGS}
        self.n_ops = 0

    def _new_sem(self, e):
        s = self.stack.enter_context(self.nc.semaphore(f"tl_{e}_{self.nsem}"))
        self.nsem += 1
        self.cur[e] = [s, 0]
        self.allsems.append((e, self.cur[e]))

    def _collect(self, eng, reads, writes, extra):
        deps = []
        for b in reads:
            if b.w is not None:
                deps.append(b.w)
            if b.excl:
                deps.extend(r for r in b.rs if r[0] != eng)
        for b in writes:
            if b.w is not None:
                deps.append(b.w)
            deps.extend(b.rs)
        deps.extend([d for d in extra if d is not None])
        waits = []
        wd = self.waited[eng]
        for (feng, sem, n) in deps:
            if feng == eng and eng == "tensor":
                continue
            key = id(sem)
            if wd.get(key, 0) >= n:
                continue
            wd[key] = n
            waits.append((sem, n))
        return waits

    @staticmethod
    def _update(tok, reads, writes):
        for b in reads:
            b.rs.append(tok)
        for b in writes:
            b.w = tok
            b.rs = []

    def op(self, eng, fn, reads=(), writes=(), extra=()):
        waits = self._collect(eng, reads, writes, extra)
        c = self.cur[eng]
        if c[1] >= SEM_ROT:
            self._new_sem(eng)
            c = self.cur[eng]
        c[1] += 1
        sem = c[0]
        tok = (eng, sem, c[1])
        self._update(tok, reads, writes)

        def emit(e, waits=waits, fn=fn, sem=sem):
            for s, v in waits:
                e.wait_ge(s, v)
            fn(e).then_inc(sem, 1)
        self.ops[eng].append(emit)
        self.n_ops += 1
        return tok

    def group(self, eng, fns, reads=(), writes=(), extra=()):
        n = len(fns)
        if n == 1:
            return self.op(eng, fns[0], reads, writes, extra)
        waits = self._collect(eng, reads, writes, extra)
        first = fns[0]

        def emit0(e, waits=waits, fn=first):
            for s, v in waits:
                e.wait_ge(s, v)
            fn(e)
        self.ops[eng].append(emit0)
        for fn in fns[1:-1]:
            self.ops[eng].append(lambda e, fn=fn: fn(e))
        self.n_ops += n - 1
        return self.op(eng, fns[-1], reads, writes, ())

    def new_dma_sem(self, name):
        s = self.stack.enter_context(self.nc.semaphore(name))
        return [s, 0]

    def dma(self, eng, ds, out, in_, reads=(), writes=(), extra=()):
        waits = self._collect(eng, reads, writes, extra)
        ds[1] += 16
        tok = ("dma", ds[0], ds[1])
        self._update(tok, reads, writes)

        def emit(e, waits=waits, out=out, in_=in_, s=ds[0]):
            for sm, v in waits:
                e.wait_ge(sm, v)
            o_ap = out(e) if callable(out) else out
            i_ap = in_(e) if callable(in_) else in_
            try:
                e.dma_start(out=o_ap, in_=i_ap).then_inc(s, 16)
            except Exception:
                print("DMA FAIL", eng, str(o_ap)[:300], "<<<<", str(i_ap)[:300])
                raise
        self.ops[eng].append(emit)
        self.n_ops += 1
        return tok

    def custom(self, eng, fn, ds, inc, reads=(), writes=(), extra=()):
        waits = self._collect(eng, reads, writes, extra)
        ds[1] += (1 if inc is None else inc)
        tok = ("cc", ds[0], ds[1])
        self._update(tok, reads, writes)

        def emit(e, waits=waits, fn=fn, s=ds[0], inc=inc):
            for sm, v in waits:
                e.wait_ge(sm, v)
            ins = fn(e)
            if inc is None:
                ins.then_inc(s)
            else:
                ins.then_inc(s, inc)
        self.ops[eng].append(emit)
        return tok

    def barrier(self, dma_sems):
        marks = [(f, c[0], c[1]) for (f, c) in self.allsems if c[1] > 0]
        marks += [("dma", d[0], d[1]) for d in dma_sems if d[1] > 0]
        for eng in self.ENGS:
            wd = self.waited[eng]
            waits = []
            for (f, sem, n) in marks:
                if f == eng:
                    continue
                if wd.get(id(sem), 0) >= n:
                    continue
                wd[id(sem)] = n
                waits.append((sem, n))

            def emit(e, waits=waits):
                for s, v in waits:
                    e.wait_ge(s, v)
            self.ops[eng].append(emit)

    def wait_all(self, eng, toks):
        waits = [(t[1], t[2]) for t in toks if t is not None]

        def emit(e, waits=waits):
            for s, v in waits:
                e.wait_ge(s, v)
        self.ops[eng].append(emit)

    def emit_all(self, block):
        for name in self.ENGS:
            lst = self.ops[name]

            def body(e, lst=lst):
                for f in lst:
                    f(e)
            getattr(block, name)(body)


ARENA_BYTES = 204 * 1024
P_RES = 8 * 1024


def _dsize(dt):
    return 4 if dt in (F32, I32) else 2


def build(S, debug=()):
    NB = S // 512
    NT = S // 128
    SH = S // 2
    NQB = SH // 512
    SC = S // NCORES
    NTC = SC // 128
    NBC = SC // 512
    VW = 130
    nc = bass.Bass("TRN2", target_bir_lowering=False)

    def din(name, shape, dt=F32):
        return nc.dram_tensor(name, shape, dt, kind="ExternalInput").ap()

    def dout(name, shape, dt=F32):
        return nc.dram_tensor(name, shape, dt, kind="ExternalOutput").ap()

    xs = din("xs", [S, D])
    posd = din("pos", [NT, 128], I32)
    wa = din("wa", [D, 1024])
    g1d = din("g1", [128, 8])
    lbpd = din("lbp", [128, 2])
    identd = din("ident", [128, 128])
    blk1d = din("blk1", [128, 128])
    onesd = din("ones", [128, 128])
    trimd = din("trim", [128, 512])
    scmd = din("scm", [128, 512])
    dld = din("dl", [1, 256])
    sgaind = din("sgain", [1, 128])
    xod = din("xo", [SC, D])
    wgd = din("wg", [D, 512])
    wod = din("wo", [D, D])
    hgnd = din("hgn", [128, 1])
    g2d = din("g2", [1, D])
    gfd = din("gf", [1, D])
    wrd = din("wr", [D, 36])
    brd = din("br", [1, 36])
    wgated = din("wgate", [N_EXPERTS, D, D_EXPERT])
    wupd = din("wup", [N_EXPERTS, D, D_EXPERT])
    wdownd = din("wdown", [N_EXPERTS, D_EXPERT, D])
    outd = dout("out", [SC, D])

    hg_part = nc.dram_tensor("hg_part", [128, S], BF16)
    da_part = nc.dram_tensor("da_part", [128, SH], BF16)
    hg_all = nc.dram_tensor("hg_all", [NCORES * 128, S], BF16)
    da_all = nc.dram_tensor("da_all", [NCORES * 128, SH], BF16)

    final_toks = []
    with ExitStack() as st:
        P = Prog(nc, st)
        arena = st.enter_context(nc.sbuf_tensor("arena", [128, ARENA_BYTES // 2], BF16))

        class Region:
            def __init__(self, lo, hi, down):
                self.lo, self.hi, self.down = lo, hi, down
                self.pos = hi if down else lo

        def carve(reg, shape, dt):
            npart = shape[0]
            nel = 1
            for d_ in shape[1:]:
                nel *= d_
            nbytes = (nel * _dsize(dt) + 63) // 64 * 64
            if reg.down:
                reg.pos -= nbytes
                start = reg.pos
                assert start >= reg.lo, "arena overflow (down)"
            else:
                start = reg.pos
                reg.pos += nbytes
                assert reg.pos <= reg.hi, "arena overflow (up)"
            ap = arena[0:npart, start // 2:(start + nbytes) // 2]
            if dt != BF16:
                ap = ap.bitcast(dt)
            ap = ap[:, 0:nel]
            if len(shape) == 3:
                ap = ap.rearrange("p (a b) -> p a b", a=shape[1])
            return ap

        regP = Region(0, P_RES, False)

        pair_t = [st.enter_context(nc.psum_tensor(f"bkp{i}", [128, 2, 512], F32)) for i in range(4)]
        bkB = [Buf(f"bk{i}", excl=True) for i in range(8)]

        def bk32(i):
            return pair_t[i // 2][:, i % 2, :]

        def bk16(i):
            return pair_t[i // 2][:, i % 2, :].bitcast(BF16)

        sync_sems = {}

        def dsem(name):
            if name not in sync_sems:
                sync_sems[name] = P.new_dma_sem("d_" + name)
            return sync_sems[name]

        def stat_rstd(stt, Bst, n, dim, eps):
            P.op("vector", lambda e: e.tensor_scalar(out=stt[:, n:2 * n], in0=stt[:, 0:n], scalar1=1.0 / dim, scalar2=eps, op0=ALU.mult, op1=ALU.add),
                 reads=[Bst], writes=[Bst])
            P.op("scalar", lambda e: e.activation(out=stt[:, n:2 * n], in_=stt[:, n:2 * n], func=AF.Ln), reads=[Bst], writes=[Bst])
            P.op("scalar", lambda e: e.activation(out=stt[:, n:2 * n], in_=stt[:, n:2 * n], func=AF.Exp, scale=-0.5), reads=[Bst], writes=[Bst])

        ident_f = carve(regP, [128, 128], F32)
        ident_b = carve(regP, [128, 128], BF16)
        blk1_b = carve(regP, [128, 128], BF16)
        ones_b = carve(regP, [128, 128], BF16)
        g1 = carve(regP, [128, 8], F32)
        B_ident_f, B_ident_b, B_blk1, B_onesb, B_g1 = [Buf() for _ in range(5)]
        P.dma("sync", dsem("c0"), ident_f, identd, writes=[B_ident_f])
        P.dma("gpsimd", dsem("c1"), ident_b, identd, writes=[B_ident_b])
        P.dma("gpsimd", dsem("c3"), blk1_b, blk1d, writes=[B_blk1])
        P.dma("gpsimd", dsem("c2"), ones_b, onesd, writes=[B_onesb])
        P.dma("sync", dsem("c6"), g1, g1d, writes=[B_g1])

        regA = Region(P_RES, ARENA_BYTES, True)
        regA1 = Region(P_RES, ARENA_BYTES, False)
        K_sb = carve(regA, [128, S], BF16)
        V_sb = carve(regA, [128, NT, VW], BF16)
        Q_sb = carve(regA, [128, SH], BF16)
        cos8 = carve(regA, [128, NT * 8], BF16)
        sin8 = carve(regA, [128, NT * 8], BF16)
        nsin8 = carve(regA, [128, NT * 8], BF16)
        mx = carve(regA, [128, 4], F32)
        lbp = carve(regA, [128, 2], F32)
        lbt = carve(regA, [128, 4], F32)
        Sseq = [carve(regA, [128, 9, 128], F32) for _ in range(2)]
        B_K = [Buf() for _ in range(NB)]
        B_V = [Buf() for _ in range(NB)]
        B_Q = [Buf() for _ in range(NQB)]
        B_Vones, B_lbp, B_lbt, B_mx, B_cos, B_sin = [Buf() for _ in range(6)]
        B_Sseq = [Buf(), Buf()]

        regA0 = Region(regA.hi - S * 2, regA.hi, False)
        posi = carve(regA0, [NT, 128], I32)
        posf = carve(regA0, [NT, 128], F32)
        posT = carve(regA0, [128, NT], F32)
        ang = carve(regA0, [128, NT * 8], F32)
        rr = carve(regA0, [128, NT * 8], F32)
        ki = carve(regA0, [128, NT * 8], I32)
        B_pos, B_posT, B_ang, B_rr, B_ki = [Buf() for _ in range(5)]

        P.dma("sync", dsem("c7"), lbp, lbpd, writes=[B_lbp])
        P.op("vector", lambda e: e.tensor_sub(out=lbt[:, 0:1], in0=lbp[:, 1:2], in1=lbp[:, 0:1]), reads=[B_lbp], writes=[B_lbt])
        P.op("scalar", lambda e: e.activation(out=lbt[:, 1:2], in_=lbt[:, 0:1], func=AF.Exp), reads=[B_lbt], writes=[B_lbt])
        P.op("vector", lambda e: e.tensor_scalar_add(out=lbt[:, 1:2], in0=lbt[:, 1:2], scalar1=1.0), reads=[B_lbt], writes=[B_lbt])
        P.op("vector", lambda e: e.reciprocal(out=lbt[:, 2:3], in_=lbt[:, 1:2]), reads=[B_lbt], writes=[B_lbt])
        P.op("vector", lambda e: e.tensor_scalar_mul(out=lbt[:, 3:4], in0=lbt[:, 2:3], scalar1=-1.0), reads=[B_lbt], writes=[B_lbt])
        oml = lbt[:, 2:3]
        noml = lbt[:, 3:4]
        P.op("gpsimd", lambda e: e.memset(V_sb[:, :, 128:130], 1.0), writes=[B_Vones])
        P.op("gpsimd", lambda e: e.memset(mx, 0.0), writes=[B_mx])
        P.op("gpsimd", lambda e: e.memset(Sseq[1][:, 8, :], 0.0), writes=[B_Sseq[1]])

        wa_b = carve(regA1, [128, 8, 1024], BF16)
        xbuf = [carve(regA1, [128, 2, 1024], F32) for _ in range(2)]
        xn = carve(regA1, [128, 2, 1024], BF16)
        xnT = carve(regA1, [128, 8, 512], BF16)
        junk = carve(regA1, [128, 1024], BF16)
        trim = carve(regA1, [128, 512], F32)
        scm = carve(regA1, [128, 512], F32)
        tabC = [carve(regA1, [128, 4, 128], BF16) for _ in range(2)]
        tabS = [carve(regA1, [128, 4, 128], BF16) for _ in range(2)]
        CS = [carve(regA1, [128, 2, 512], BF16) for _ in range(2)]
        stat = [carve(regA1, [128, 8], F32) for _ in range(2)]
        qT_f = carve(regA1, [128, 512], F32)
        e1 = carve(regA1, [128, 512], F32)
        sgn = carve(regA1, [128, 512], F32)
        lf = carve(regA1, [128, 512], F32)
        bcs = carve(regA1, [128, 512], F32)
        eb = carve(regA1, [128, 512], F32)
        enb = carve(regA1, [128, 512], F32)
        qt_b = carve(regA1, [128, 512], BF16)
        kt_b = carve(regA1, [128, 512], BF16)
        kt_tok = carve(regA1, [128, 4, 128], BF16)
        v_hg = carve(regA1, [128, 4, 128], BF16)
        S_bf = carve(regA1, [128, 8, 128], BF16)
        Ttmp = carve(regA1, [128, 128], F32)
        PT_b = carve(regA1, [128, 512], BF16)
        o_bf = carve(regA1, [128, 4, 128], BF16)
        oT_sb = [carve(regA1, [128, 4, 128], BF16) for _ in range(2)]
        assert regA1.pos <= regA.pos, f"phase A1 does not fit: {regA1.pos} > {regA.pos}"
        t1, t2, sqk = e1, lf, PT_b
        (B_wa, B_xn, B_xnT, B_junk, B_trim, B_scm, B_qT, B_e1, B_sgn, B_lf, B_bcs, B_eb, B_enb, B_qt, B_kt, B_kttok, B_vhg,
         B_Sbf, B_T, B_PT, B_obf) = [Buf() for _ in range(21)]
        B_t1, B_t2, B_sqk = B_e1, B_lf, B_PT
        B_xbuf, B_tab, B_CS, B_stat, B_oT = [[Buf(), Buf()] for _ in range(5)]
        P.dma("sync", dsem("c4"), trim, trimd, writes=[B_trim])
        P.dma("sync", dsem("c5"), scm, scmd, writes=[B_scm])

        wav = wa.rearrange("(k p) c -> p k c", p=128)
        for r4 in range(4):
            sl = r4 % 2
            P.dma("sync", dsem(f"x{sl}"), xbuf[sl], wav[:, 2 * r4:2 * r4 + 2, :], writes=[B_xbuf[sl]])
            for kk in range(2):
                k = 2 * r4 + kk
                P.op("vector" if kk == 0 else "gpsimd",
                     lambda e, sl=sl, kk=kk, k=k: e.tensor_scalar(out=wa_b[:, k, :], in0=xbuf[sl][:, kk, :], scalar1=g1[:, k:k + 1], scalar2=None, op0=ALU.mult),
                     reads=[B_xbuf[sl], B_g1], writes=[B_wa])

        P.dma("sync", dsem("c8"), posi, posd, writes=[B_pos])
        P.op("vector", lambda e: e.tensor_copy(out=posf, in_=posi), reads=[B_pos], writes=[B_pos])
        P.op("tensor", lambda e: e.transpose(out=bk32(0)[:, 0:NT], in_=posf, identity=ident_f[0:NT, 0:NT]),
             reads=[B_pos, B_ident_f], writes=[bkB[0]])
        P.op("vector", lambda e: e.tensor_copy(out=posT, in_=bk32(0)[:, 0:NT]), reads=[bkB[0]], writes=[B_posT])
        ang3 = ang.rearrange("p (t i) -> p t i", i=8)
        for i in range(8):
            invf = float(np.float32(ROPE_THETA) ** np.float32(-(2.0 * i) / 16.0))
            P.op("vector", lambda e, i=i, invf=invf: e.tensor_scalar(out=ang3[:, :, i], in0=posT, scalar1=invf, scalar2=None, op0=ALU.mult),
                 reads=[B_posT], writes=[B_ang])

        def sin_of(dst, B_dst, shift):
            P.op("vector", lambda e: e.tensor_scalar(out=rr, in0=ang, scalar1=shift, scalar2=1.0 / TWO_PI, op0=ALU.add, op1=ALU.mult),
                 reads=[B_ang], writes=[B_rr])
            P.op("vector", lambda e: e.tensor_copy(out=ki, in_=rr), reads=[B_rr], writes=[B_ki])
            P.op("vector", lambda e: e.tensor_copy(out=rr, in_=ki), reads=[B_ki], writes=[B_rr])
            P.op("vector", lambda e: e.tensor_scalar(out=rr, in0=rr, scalar1=-TWO_PI, scalar2=shift, op0=ALU.mult, op1=ALU.add),
                 reads=[B_rr], writes=[B_rr])
            P.op("vector", lambda e: e.tensor_add(out=rr, in0=rr, in1=ang), reads=[B_rr, B_ang], writes=[B_rr])
            P.op("vector", lambda e: e.tensor_scalar(out=rr, in0=rr, scalar1=-PI, scalar2=PI, op0=ALU.max, op1=ALU.min),
                 reads=[B_rr], writes=[B_rr])
            P.op("scalar", lambda e: e.activation(out=dst, in_=rr, func=AF.Sin), reads=[B_rr], writes=[B_dst])

        sin_of(cos8, B_cos, PI / 2.0)
        sin_of(sin8, B_sin, 0.0)
        P.op("vector", lambda e: e.tensor_scalar_mul(out=nsin8, in0=sin8, scalar1=-1.0), reads=[B_sin], writes=[B_sin])
        cos3 = cos8.rearrange("p (t i) -> p t i", i=8)
        sin3 = sin8.rearrange("p (t i) -> p t i", i=8)
        nsin3 = nsin8.rearrange("p (t i) -> p t i", i=8)
        for i in range(2):
            P.op("gpsimd", lambda e, i=i: e.memset(tabC[i], 1.0), writes=[B_tab[i]])
            P.op("gpsimd", lambda e, i=i: e.memset(tabS[i], 0.0), writes=[B_tab[i]])
        P.barrier(sync_sems.values())

        xsv = xs.rearrange("(g t p) d -> g p t d", p=128, t=2)

        def load_x(g):
            P.dma("sync", dsem(f"x{g % 2}"), xbuf[g % 2], xsv[g], writes=[B_xbuf[g % 2]])

        def a1_block(b):
            stt = stat[b % 2]
            Bst = B_stat[b % 2]
            for hb in range(2):
                g = 2 * b + hb
                if g + 1 < 2 * NB:
                    load_x(g + 1)
                xb = xbuf[g % 2]
                Bx = B_xbuf[g % 2]
                for tt in range(2):
                    t = 2 * hb + tt
                    P.op("scalar", lambda e, tt=tt, t=t, xb=xb: e.activation(out=junk, in_=xb[:, tt, :], func=AF.Square, accum_out=stt[:, t:t + 1]),
                         reads=[Bx], writes=[B_junk, Bst])
                P.op("vector", lambda e, hb=hb: e.tensor_scalar(out=stt[:, 4 + 2 * hb:6 + 2 * hb], in0=stt[:, 2 * hb:2 * hb + 2], scalar1=1.0 / D, scalar2=1e-6, op0=ALU.mult, op1=ALU.add),
                     reads=[Bst], writes=[Bst])
                P.op("scalar", lambda e, hb=hb: e.activation(out=stt[:, 4 + 2 * hb:6 + 2 * hb], in_=stt[:, 4 + 2 * hb:6 + 2 * hb], func=AF.Ln), reads=[Bst], writes=[Bst])
                P.op("scalar", lambda e, hb=hb: e.activation(out=stt[:, 4 + 2 * hb:6 + 2 * hb], in_=stt[:, 4 + 2 * hb:6 + 2 * hb], func=AF.Exp, scale=-0.5), reads=[Bst], writes=[Bst])
                for tt in range(2):
                    t = 2 * hb + tt
                    P.op("gpsimd" if tt == 0 else "vector",
                         lambda e, tt=tt, t=t, xb=xb: e.tensor_scalar(out=xn[:, tt, :], in0=xb[:, tt, :], scalar1=stt[:, 4 + t:5 + t], scalar2=None, op0=ALU.mult),
                         reads=[Bx, Bst], writes=[B_xn])
                for kk in range(4):
                    bi = kk % 2
                    pv = bk16(bi).rearrange("p (a c) -> p a c", a=2)
                    fns = []
                    for a in range(2):
                        k = 2 * kk + a
                        for tt in range(2):
                            fns.append(lambda e, a=a, k=k, tt=tt, pv=pv: e.transpose(out=pv[:, a, tt * 128:(tt + 1) * 128], in_=xn[:, tt, k * 128:(k + 1) * 128], identity=ident_b))
                    P.group("tensor", fns, reads=[B_xn, B_ident_b], writes=[bkB[bi]])
                    if kk % 2 == 0:
                        P.op("scalar", lambda e, kk=kk, pv=pv, hb=hb: e.copy(out=xnT[:, 2 * kk:2 * kk + 2, hb * 256:(hb + 1) * 256], in_=pv[:, :, 0:256]), reads=[bkB[bi]], writes=[B_xnT])
                    else:
                        P.op("vector", lambda e, kk=kk, pv=pv, hb=hb: e.tensor_copy(out=xnT[:, 2 * kk:2 * kk + 2, hb * 256:(hb + 1) * 256], in_=pv[:, :, 0:256]), reads=[bkB[bi]], writes=[B_xnT])
            xT = xnT
            BxT = B_xnT
            tb = b % 2
            for (src, c0) in ((cos3, 0), (cos3, 8), (cos3, 64), (cos3, 72)):
                P.op("gpsimd", lambda e, src=src, c0=c0: e.tensor_copy(out=tabC[tb][:, :, c0:c0 + 8], in_=src[:, 4 * b:4 * b + 4, :]),
                     reads=[B_cos], writes=[B_tab[tb]])
            for (src, c0) in ((nsin3, 0), (sin3, 8), (nsin3, 64), (sin3, 72)):
                P.op("gpsimd", lambda e, src=src, c0=c0: e.tensor_copy(out=tabS[tb][:, :, c0:c0 + 8], in_=src[:, 4 * b:4 * b + 4, :]),
                     reads=[B_sin], writes=[B_tab[tb]])
            pv = bk16(2).rearrange("p (a c) -> p a c", a=2)
            fns = []
            for a, tab in ((0, tabC[tb]), (1, tabS[tb])):
                for t in range(4):
                    fns.append(lambda e, a=a, tab=tab, t=t, pv=pv: e.transpose(out=pv[:, a, t * 128:(t + 1) * 128], in_=tab[:, t, :], identity=ident_b))
            P.group("tensor", fns, reads=[B_tab[tb], B_ident_b], writes=[bkB[2]])
            P.op("vector", lambda e, pv=pv: e.tensor_copy(out=CS[tb], in_=pv), reads=[bkB[2]], writes=[B_CS[tb]])

            def fm_group(g, bank):
                fns = []
                for k in range(8):
                    fns.append(lambda e, k=k: e.matmul(out=bk32(bank), lhsT=wa_b[:, k, g * 128:(g + 1) * 128], rhs=xT[:, k, :], start=(k == 0), stop=(k == 7)))
                P.group("tensor", fns, reads=[B_wa, BxT], writes=[bkB[bank]])

            fm_group(0, 2)
            P.op("scalar", lambda e: e.copy(out=qT_f, in_=bk32(2)), reads=[bkB[2]], writes=[B_qT])
            fm_group(1, 3)
            P.op("scalar", lambda e: e.activation(out=e1, in_=bk32(3), func=AF.Exp), reads=[bkB[3]], writes=[B_e1])
            P.op("scalar", lambda e: e.activation(out=sgn, in_=e1, func=AF.Ln, bias=1.0), reads=[B_e1], writes=[B_sgn])
            P.op("scalar", lambda e: e.activation(out=sgn, in_=sgn, func=AF.Exp, scale=-1.0), reads=[B_sgn], writes=[B_sgn])
            P.op("scalar", lambda e: e.activation(out=lf, in_=sgn, func=AF.Ln, scale=noml, bias=1.0), reads=[B_sgn, B_lbt], writes=[B_lf])
            P.op("vector", lambda e: e.tensor_tensor_scan(out=bcs, data0=scm, data1=lf, initial=0.0, op0=ALU.mult, op1=ALU.add),
                 reads=[B_scm, B_lf], writes=[B_bcs])
            P.op("scalar", lambda e: e.activation(out=eb, in_=bcs, func=AF.Exp), reads=[B_bcs], writes=[B_eb])
            P.op("scalar", lambda e: e.activation(out=enb, in_=bcs, func=AF.Exp, scale=-1.0), reads=[B_bcs], writes=[B_enb])
            P.op("vector", lambda e: e.tensor_tensor(out=qt_b, in0=qT_f, in1=eb, op=ALU.mult), reads=[B_qT, B_eb], writes=[B_qt])
            P.op("vector", lambda e: e.scalar_tensor_tensor(out=kt_b, in0=sgn, scalar=oml, in1=enb, op0=ALU.mult, op1=ALU.mult),
                 reads=[B_sgn, B_enb, B_lbt], writes=[B_kt])

            def rot_group(g, dst_ap, B_dst, is_k):
                fm_group(g, 2)
                P.op("vector", lambda e: e.tensor_tensor(out=t1, in0=bk32(2), in1=CS[tb][:, 0, :], op=ALU.mult), reads=[bkB[2], B_CS[tb]], writes=[B_t1])
                fm_group(g + 1, 3)
                P.op("vector", lambda e: e.tensor_tensor(out=t2, in0=bk32(3), in1=CS[tb][:, 1, :], op=ALU.mult), reads=[bkB[3], B_CS[tb]], writes=[B_t2])
                P.op("gpsimd", lambda e: e.tensor_tensor(out=dst_ap, in0=t1, in1=t2, op=ALU.add), reads=[B_t1, B_t2], writes=[B_dst])
                P.op("gpsimd", lambda e: e.tensor_tensor(out=sqk, in0=dst_ap, in1=dst_ap, op=ALU.mult), reads=[B_dst], writes=[B_sqk])
                P.op("tensor", lambda e: e.matmul(out=bk32(2), lhsT=blk1_b, rhs=sqk, start=True, stop=True), reads=[B_blk1, B_sqk], writes=[bkB[2]])
                col = 0 if is_k else 1
                P.op("vector", lambda e: e.reduce_max(out=mx[:, 2:3], in_=bk32(2), axis=AX.X), reads=[bkB[2]], writes=[B_mx])
                P.op("vector", lambda e: e.tensor_max(out=mx[:, col:col + 1], in0=mx[:, col:col + 1], in1=mx[:, 2:3]), reads=[B_mx], writes=[B_mx])

            rot_group(2, K_sb[:, b * 512:(b + 1) * 512], B_K[b], True)
            if b < NQB:
                rot_group(4, Q_sb[:, b * 512:(b + 1) * 512], B_Q[b], False)

            for rnd in range(2):
                pvv = bk32(4).rearrange("p (a c) -> p a c", a=2)
                fns = []
                for a in range(2):
                    t = 2 * rnd + a
                    for k in range(8):
                        fns.append(lambda e, a=a, t=t, k=k, pvv=pvv: e.matmul(out=pvv[:, a, :], lhsT=xT[:, k, t * 128:(t + 1) * 128], rhs=wa_b[:, k, 768:1024], start=(k == 0), stop=(k == 7)))
                P.group("tensor", fns, reads=[B_wa, BxT], writes=[bkB[4]])
                P.op("scalar", lambda e, rnd=rnd, pvv=pvv: e.copy(out=v_hg[:, 2 * rnd:2 * rnd + 2, :], in_=pvv[:, :, 0:128]), reads=[bkB[4]], writes=[B_vhg])
                for a in range(2):
                    P.op("scalar", lambda e, rnd=rnd, pvv=pvv, a=a: e.copy(out=V_sb[:, 4 * b + 2 * rnd + a, 0:128], in_=pvv[:, a, 128:256]),
                         reads=[bkB[4]], writes=[B_V[b]])

            pk = bk16(4).rearrange("p (a c) -> p a c", a=8)
            fns = [lambda e, t=t, pk=pk: e.transpose(out=pk[:, t, :], in_=kt_b[:, t * 128:(t + 1) * 128], identity=ident_b) for t in range(4)]
            P.group("tensor", fns, reads=[B_kt, B_ident_b], writes=[bkB[4]])
            P.op("scalar", lambda e, pk=pk: e.copy(out=kt_tok, in_=pk[:, 0:4, :]), reads=[bkB[4]], writes=[B_kttok])
            pA = [bk32(5).rearrange("p (a c) -> p a c", a=4), bk32(6).rearrange("p (a c) -> p a c", a=4)]
            for r in range(2):
                fns = [lambda e, t=t, r=r: e.matmul(out=pA[r][:, t, :], lhsT=kt_tok[64 * r:64 * r + 64, t, :], rhs=v_hg[64 * r:64 * r + 64, t, :], start=True, stop=True)
                       for t in range(4)]
                P.group("tensor", fns, reads=[B_kttok, B_vhg], writes=[bkB[5 + r]])
            Sc = Sseq[b % 2]
            Sp = Sseq[(b + 1) % 2]
            BSc = B_Sseq[b % 2]
            BSp = B_Sseq[(b + 1) % 2]
            for n in range(8):
                t, r = n // 2, n % 2
                prev_ap = Sp[:, 8, :] if n == 0 else Sc[:, n, :]
                rd = [bkB[5 + r], BSp if n == 0 else BSc]
                P.op("vector", lambda e, t=t, r=r, prev_ap=prev_ap: e.tensor_tensor(out=Ttmp, in0=pA[r][:, t, :], in1=prev_ap, op=ALU.add),
                     reads=rd, writes=[B_T])
                P.op("vector", lambda e, n=n: e.tensor_scalar(out=Sc[:, n + 1, :], in0=Ttmp, scalar1=eb[:, n * 64 + 63:n * 64 + 64], scalar2=None, op0=ALU.mult),
                     reads=[B_T, B_eb], writes=[BSc])
            P.op("gpsimd", lambda e: e.tensor_copy(out=S_bf[:, 0, :], in_=Sp[:, 8, :]), reads=[BSp], writes=[B_Sbf])
            P.op("gpsimd", lambda e: e.tensor_copy(out=S_bf[:, 1:8, :], in_=Sc[:, 1:8, :]), reads=[BSc], writes=[B_Sbf])

            psc = bk32(7).rearrange("p (a c) -> p a c", a=4)
            fns = [lambda e, t=t: e.matmul(out=psc[:, t, :], lhsT=kt_b[:, t * 128:(t + 1) * 128], rhs=qt_b[:, t * 128:(t + 1) * 128], start=True, stop=True) for t in range(4)]
            P.group("tensor", fns, reads=[B_kt, B_qt], writes=[bkB[7]])
            P.op("vector", lambda e: e.tensor_tensor(out=PT_b, in0=bk32(7), in1=trim, op=ALU.mult), reads=[bkB[7], B_trim], writes=[B_PT])
            po = bk32(7).rearrange("p (a c) -> p a c", a=4)
            fns = []
            for t in range(4):
                fns.append(lambda e, t=t: e.matmul(out=po[:, t, :], lhsT=PT_b[:, t * 128:(t + 1) * 128], rhs=v_hg[:, t, :], start=True, stop=False))
                for r in range(2):
                    n = 2 * t + r
                    fns.append(lambda e, t=t, r=r, n=n: e.matmul(out=po[64 * r:64 * r + 64, t, :], lhsT=qt_b[:, n * 64:(n + 1) * 64], rhs=S_bf[:, n, :],
                                                                 start=False, stop=(r == 1), skip_group_check=True))
            P.group("tensor", fns, reads=[B_PT, B_vhg, B_qt, B_Sbf], writes=[bkB[7]])
            P.op("scalar", lambda e: e.copy(out=o_bf, in_=po), reads=[bkB[7]], writes=[B_obf])
            poT = bk32(7).rearrange("p (a c) -> p a c", a=4)
            fns = [lambda e, t=t: e.matmul(out=poT[:, t, :], lhsT=o_bf[:, t, :], rhs=ident_b, start=True, stop=True) for t in range(4)]
            P.group("tensor", fns, reads=[B_obf, B_ident_b], writes=[bkB[7]])
            oT = oT_sb[b % 2]
            BoT = B_oT[b % 2]
            P.op("vector", lambda e: e.tensor_copy(out=oT, in_=poT), reads=[bkB[7]], writes=[BoT])
            return P.dma("sync", dsem(f"o{b % 2}"), hg_part[:, b * 512:(b + 1) * 512], oT, reads=[BoT])

        load_x(0)
        hg_toks = [a1_block(b) for b in range(NB)]

        B_hgall, B_daall = Buf(), Buf()
        P.custom("gpsimd", lambda e: e.collective_compute("AllGather", ALU.bypass, replica_groups=[list(range(NCORES))],
                                                          ins=[hg_part.ap()], outs=[hg_all.ap()]),
                 dsem("cc_hg"), None, writes=[B_hgall], extra=hg_toks[-2:])
        P.barrier([d for n_, d in sync_sems.items() if not n_.startswith("cc")])

        regA2 = Region(P_RES, regA.pos, False)
        sc1 = carve(regA2, [1, 8], F32)
        dl = carve(regA2, [1, 256], F32)
        dlp = carve(regA2, [1, 128], F32)
        ones_row = carve(regA2, [1, 128], F32)
        bc = carve(regA2, [128, 2], F32)
        sg08 = carve(regA2, [128, 128], F32)
        junk2 = carve(regA2, [128, 128], BF16)
        PTs = [carve(regA2, [128, 2, 512], BF16) for _ in range(3)]
        rs = carve(regA2, [128, 16], F32)
        at1 = carve(regA2, [128, 128], F32)
        ao = carve(regA2, [128, 4, 128], F32)
        aon = carve(regA2, [128, 4, 128], BF16)
        daT = [carve(regA2, [128, 4, 128], BF16) for _ in range(2)]
        B_sc1, B_dl, B_dlp, B_ones, B_bc, B_sg, B_junk2, B_rs, B_at1, B_ao, B_aon = [Buf() for _ in range(11)]
        B_PTs = [Buf() for _ in range(3)]
        B_daT = [Buf(), Buf()]
        P.dma("sync", dsem("c9"), dl, dld, writes=[B_dl])
        P.dma("sync", dsem("c10"), sg08, sgaind.partition_broadcast(128), writes=[B_sg])
        P.op("vector", lambda e: e.tensor_scalar_mul(out=sg08, in0=sg08, scalar1=1.0 - LAMBDA_INIT), reads=[B_sg], writes=[B_sg])
        P.op("gpsimd", lambda e: e.memset(ones_row, 1.0), writes=[B_ones])
        P.op("vector", lambda e: e.tensor_tensor(out=mx[:, 2:3], in0=mx[:, 0:1], in1=mx[:, 1:2], op=ALU.mult), reads=[B_mx], writes=[B_mx])
        P.op("scalar", lambda e: e.activation(out=mx[:, 2:3], in_=mx[:, 2:3], func=AF.Ln), reads=[B_mx], writes=[B_mx])
        P.op("scalar", lambda e: e.activation(out=mx[:, 2:3], in_=mx[:, 2:3], func=AF.Exp, scale=0.5), reads=[B_mx], writes=[B_mx])
        P.op("tensor", lambda e: e.transpose(out=bk32(7)[0:1, 0:128], in_=mx[:, 2:3], identity=ident_f), reads=[B_mx, B_ident_f], writes=[bkB[7]])
        P.op("vector", lambda e: e.reduce_max(out=sc1[:, 0:1], in_=bk32(7)[0:1, 0:128], axis=AX.X), reads=[bkB[7]], writes=[B_sc1])
        P.op("vector", lambda e: e.tensor_scalar_mul(out=sc1[:, 0:1], in0=sc1[:, 0:1], scalar1=-0.125), reads=[B_sc1], writes=[B_sc1])
        dl4 = dl.rearrange("p (a b c) -> p a b c", a=2, b=2)
        dlp2 = dlp.rearrange("p (a c) -> p a c", a=2)
        P.op("vector", lambda e: e.tensor_tensor(out=dlp2, in0=dl4[:, :, 0, :], in1=dl4[:, :, 1, :], op=ALU.mult), reads=[B_dl], writes=[B_dlp])
        P.op("vector", lambda e: e.reduce_sum(out=sc1[:, 2:4], in_=dlp2, axis=AX.X), reads=[B_dlp], writes=[B_sc1])
        P.op("scalar", lambda e: e.activation(out=sc1[:, 2:4], in_=sc1[:, 2:4], func=AF.Exp), reads=[B_sc1], writes=[B_sc1])
        P.op("vector", lambda e: e.tensor_sub(out=sc1[:, 1:2], in0=sc1[:, 2:3], in1=sc1[:, 3:4]), reads=[B_sc1], writes=[B_sc1])
        P.op("vector", lambda e: e.tensor_scalar_add(out=sc1[:, 1:2], in0=sc1[:, 1:2], scalar1=LAMBDA_INIT), reads=[B_sc1], writes=[B_sc1])
        P.op("tensor", lambda e: e.matmul(out=bk32(7)[:, 0:2], lhsT=ones_row, rhs=sc1[:, 0:2], start=True, stop=True), reads=[B_ones, B_sc1], writes=[bkB[7]])
        P.op("vector", lambda e: e.tensor_copy(out=bc, in_=bk32(7)[:, 0:2]), reads=[bkB[7]], writes=[B_bc])

        def acc(m, s_):
            idx = m * 4 + s_
            return bk32(4 + idx // 3)[:, (idx % 3) * VW:(idx % 3) * VW + VW], bkB[4 + idx // 3]

        def qk(qb, kt):
            pb = kt % 2
            for m in range(2):
                P.op("tensor", lambda e, m=m, pb=pb: e.matmul(out=bk32(2 * pb + m), lhsT=K_sb[64 * m:64 * m + 64, kt * 128:(kt + 1) * 128],
                                                              rhs=Q_sb[64 * m:64 * m + 64, qb * 512:(qb + 1) * 512], start=True, stop=True),
                     reads=[B_K[kt // 4], B_Q[qb]], writes=[bkB[2 * pb + m]])

        def expo(kt):
            pb = kt % 2
            x3 = kt % 3
            P.op("scalar", lambda e, pb=pb, x3=x3: e.activation(out=PTs[x3], in_=pair_t[pb][:, :, :], func=AF.Exp, bias=bc[:, 0:1], scale=0.125),
                 reads=[bkB[2 * pb], bkB[2 * pb + 1], B_bc], writes=[B_PTs[x3]])

        def pv_mm(kt):
            x3 = kt % 3
            fns = []
            for m in range(2):
                for s_ in range(4):
                    ap_, _ = acc(m, s_)
                    fns.append(lambda e, m=m, s_=s_, ap_=ap_: e.matmul(out=ap_, lhsT=PTs[x3][:, m, s_ * 128:(s_ + 1) * 128], rhs=V_sb[:, kt, :],
                                                                      start=False, stop=(kt == NT - 1), skip_group_check=True))
            P.group("tensor", fns, reads=[B_PTs[x3], B_V[kt // 4], B_Vones], writes=[bkB[4], bkB[5], bkB[6]])

        def a2_block(qb):
            for i in range(3):
                P.op("vector", lambda e, i=i: e.memset(bk32(4 + i), 0.0), writes=[bkB[4 + i]])
            for kt in range(NT + 1):
                if kt < NT:
                    qk(qb, kt)
                    expo(kt)
                if kt > 0:
                    pv_mm(kt - 1)
            for m in range(2):
                for s_ in range(4):
                    ap_, bb = acc(m, s_)
                    P.op("vector", lambda e, m=m, s_=s_, ap_=ap_: e.reciprocal(out=rs[:, m * 4 + s_:m * 4 + s_ + 1], in_=ap_[:, 128:129]), reads=[bb], writes=[B_rs])
            P.op("vector", lambda e: e.tensor_scalar(out=rs[:, 4:8], in0=rs[:, 4:8], scalar1=bc[:, 1:2], scalar2=None, op0=ALU.mult), reads=[B_rs, B_bc], writes=[B_rs])
            for s_ in range(4):
                a0, b0 = acc(0, s_)
                a1, b1 = acc(1, s_)
                P.op("vector", lambda e, s_=s_, a1=a1: e.tensor_scalar(out=at1, in0=a1[:, 0:128], scalar1=rs[:, 4 + s_:5 + s_], scalar2=None, op0=ALU.mult),
                     reads=[b1, B_rs], writes=[B_at1])
                P.op("vector", lambda e, s_=s_, a0=a0: e.scalar_tensor_tensor(out=ao[:, s_, :], in0=a0[:, 0:128], scalar=rs[:, s_:s_ + 1], in1=at1, op0=ALU.mult, op1=ALU.subtract),
                     reads=[b0, B_rs, B_at1], writes=[B_ao])
                P.op("scalar", lambda e, s_=s_: e.activation(out=junk2, in_=ao[:, s_, :], func=AF.Square, accum_out=rs[:, 8 + s_:9 + s_]),
                     reads=[B_ao], writes=[B_junk2, B_rs])
            P.op("vector", lambda e: e.tensor_scalar(out=rs[:, 12:16], in0=rs[:, 8:12], scalar1=1.0 / 128.0, scalar2=1e-5, op0=ALU.mult, op1=ALU.add), reads=[B_rs], writes=[B_rs])
            P.op("scalar", lambda e: e.activation(out=rs[:, 12:16], in_=rs[:, 12:16], func=AF.Ln), reads=[B_rs], writes=[B_rs])
            P.op("scalar", lambda e: e.activation(out=rs[:, 12:16], in_=rs[:, 12:16], func=AF.Exp, scale=-0.5), reads=[B_rs], writes=[B_rs])
            for s_ in range(4):
                P.op("vector", lambda e, s_=s_: e.scalar_tensor_tensor(out=aon[:, s_, :], in0=ao[:, s_, :], scalar=rs[:, 12 + s_:13 + s_], in1=sg08, op0=ALU.mult, op1=ALU.mult),
                     reads=[B_ao, B_rs, B_sg], writes=[B_aon])
            pdt = bk32(7).rearrange("p (a c) -> p a c", a=4)
            fns = [lambda e, s_=s_: e.matmul(out=pdt[:, s_, :], lhsT=aon[:, s_, :], rhs=ident_b, start=True, stop=True) for s_ in range(4)]
            P.group("tensor", fns, reads=[B_aon, B_ident_b], writes=[bkB[7]])
            dT = daT[qb % 2]
            P.op("vector", lambda e: e.tensor_copy(out=dT, in_=pdt), reads=[bkB[7]], writes=[B_daT[qb % 2]])
            return P.dma("sync", dsem(f"da{qb % 2}"), da_part[:, qb * 512:(qb + 1) * 512], dT, reads=[B_daT[qb % 2]])

        da_toks = [a2_block(qb) for qb in range(NQB)]

        P.custom("gpsimd", lambda e: e.collective_compute("AllGather", ALU.bypass, replica_groups=[list(range(NCORES))],
                                                          ins=[da_part.ap()], outs=[da_all.ap()]),
                 dsem("cc_da"), None, writes=[B_daall], extra=da_toks[-2:])

        if "hg" in debug:
            final_toks.append(P.dma("sync", dsem("dbg3"), dout("dbg_hg", [128, S], BF16), hg_part[:, :], extra=hg_toks[-2:]))
        if "da" in debug:
            final_toks.append(P.dma("sync", dsem("dbg4"), dout("dbg_da", [128, SH], BF16), da_part[:, :], extra=da_toks[-2:]))

        P.barrier([d for n_, d in sync_sems.items() if not n_.startswith("cc")])
        regC = Region(P_RES, ARENA_BYTES, True)
        regC1 = Region(P_RES, ARENA_BYTES, False)
        x1 = carve(regC, [128, NTC, 1024], F32)
        h2T = carve(regC, [128, 8, SC], BF16)
        gates = carve(regC, [128, NTC, 32], F32)
        lg = carve(regC, [128, NTC, 36], F32)
        g2b = carve(regC, [128, 1024], F32)
        gfb = carve(regC, [128, 1024], F32)
        brb = carve(regC, [128, 36], F32)
        wr_f = carve(regC, [128, 8, 36], F32)
        hgn = carve(regC, [128, 1], F32)
        B_x1 = [Buf() for _ in range(NTC)]
        B_h2T = [Buf() for _ in range(NTC)]
        B_gates = [Buf() for _ in range(NTC)]
        B_lg = [Buf() for _ in range(NTC)]
        B_g2b, B_gfb, B_brb, B_wr, B_hgn = [Buf() for _ in range(5)]
        P.dma("sync", dsem("k0"), g2b, g2d.partition_broadcast(128), writes=[B_g2b])
        P.dma("sync", dsem("k1"), gfb, gfd.partition_broadcast(128), writes=[B_gfb])
        P.dma("sync", dsem("k2"), brb, brd.partition_broadcast(128), writes=[B_brb])
        P.dma("sync", dsem("k3"), wr_f, wrd.rearrange("(k p) c -> p k c", p=128), writes=[B_wr])
        P.dma("sync", dsem("k4"), hgn, hgnd, writes=[B_hgn])

        wo_b = carve(regC1, [128, 8, 1024], BF16)
        wg_b = carve(regC1, [128, 8, 512], BF16)
        xnC = carve(regC1, [128, 4, 1024], BF16)
        xnTC = carve(regC1, [128, 8, 512], BF16)
        mixT = carve(regC1, [128, 8, 512], BF16)
        sgate = carve(regC1, [128, 512], F32)
        hgD = carve(regC1, [128, 4, 512], BF16)
        hgR = carve(regC1, [128, 4, 512], BF16)
        osum = carve(regC1, [128, 512], F32)
        sq = carve(regC1, [128, 512], BF16)
        rst = carve(regC1, [128, 512], F32)
        h2g = carve(regC1, [128, 1024], F32)
        h2Tf = carve(regC1, [128, 8, 128], F32)
        junkC = carve(regC1, [128, 1024], BF16)
        statC = [carve(regC1, [128, 8], F32) for _ in range(2)]
        stat2 = [carve(regC1, [128, 8], F32) for _ in range(2)]
        rt = carve(regC1, [128, 16], F32)
        rml = carve(regC1, [128, 32], F32)
        rml2 = carve(regC1, [128, 32], F32)
        roh1 = carve(regC1, [128, 32], F32)
        roh2 = carve(regC1, [128, 32], F32)
        rge = carve(regC1, [128, 4], F32)
        rmk = carve(regC1, [128, 4], F32)
        assert regC1.pos <= regC.pos, f"phase C1 does not fit: {regC1.pos} > {regC.pos}"
        (B_wo, B_wgb, B_xnC, B_xnTC, B_sgate, B_osum, B_sq, B_rst, B_h2g, B_h2Tf, B_junkC, B_rt, B_rml, B_rml2, B_roh1, B_roh2,
         B_rge, B_rmk) = [Buf() for _ in range(18)]
        B_mixT = [Buf() for _ in range(8)]
        B_statC, B_stat2 = [[Buf(), Buf()] for _ in range(2)]
        B_hgD, B_hgR = Buf(), Buf()
        P.dma("gpsimd", dsem("k5"), wo_b, wod.rearrange("(k p) c -> p k c", p=128), writes=[B_wo])
        P.dma("gpsimd", dsem("k6"), wg_b, wgd.rearrange("(k p) c -> p k c", p=128), writes=[B_wgb])
        for k in range(8):
            P.op("vector", lambda e, k=k: e.tensor_scalar(out=wg_b[:, k, :], in0=wg_b[:, k, :], scalar1=g1[:, k:k + 1], scalar2=None, op0=ALU.mult),
                 reads=[B_g1, B_wgb], writes=[B_wgb])

        dynreg = {}

        def dyn(e, tb):
            key = id(e)

            def mat(expr, lo, hi):
                return e.snap(e.to_reg(expr), donate=True, min_val=lo, max_val=hi)
            if key not in dynreg:
                pid = mat(e.partition_id(), 0, NCORES - 1)
                j = mat(pid // 4, 0, 1)
                jn = mat((7 - pid) // 4, 0, 1)
                colD = mat(jn * (pid * SC) + j * ((NCORES - 1 - pid) * SC), 0, SH - SC)
                colR = mat(jn * ((NCORES - 1 - pid) * SC) + j * (pid * SC), 0, S - SC)
                rowD = mat(j * 512, 0, 512)
                rowR = mat(jn * 512, 0, 512)
                dynreg[key] = dict(colD=colD, colR=colR, rowD=rowD, rowR=rowR, blk={})
            dct = dynreg[key]
            if tb not in dct["blk"]:
                cD = mat(dct["colD"] + tb * 512, 0, SH - 512)
                cR = mat(dct["colR"] + (SC - (tb + 1) * 512), 0, S - 512)
                dct["blk"][tb] = (cD, cR)
            cD, cR = dct["blk"][tb]
            return cD, cR, dct["rowD"], dct["rowR"]

        xov = xod.rearrange("(b t p) d -> b p t d", p=128, t=4)
        c4 = [0]

        def route_tile(tl):
            g4 = lg[:, tl, 0:4]
            le3 = lg[:, tl, 4:36].rearrange("p (g e) -> p g e", g=4)
            rml3 = rml.rearrange("p (g e) -> p g e", g=4)
            Bl = B_lg[tl]
            P.op("vector", lambda e: e.reduce_max(out=rt[:, 0:1], in_=g4, axis=AX.X), reads=[Bl], writes=[B_rt])
            P.op("vector", lambda e: e.tensor_scalar_mul(out=rt[:, 1:2], in0=rt[:, 0:1], scalar1=-1.0), reads=[B_rt], writes=[B_rt])
            P.op("scalar", lambda e: e.activation(out=rge, in_=g4, func=AF.Exp, bias=rt[:, 1:2], accum_out=rt[:, 2:3]), reads=[Bl, B_rt], writes=[B_rge, B_rt])
            P.op("vector", lambda e: e.reciprocal(out=rt[:, 3:4], in_=rt[:, 2:3]), reads=[B_rt], writes=[B_rt])
            P.op("vector", lambda e: e.tensor_scalar(out=rmk, in0=g4, scalar1=rt[:, 0:1], scalar2=None, op0=ALU.is_equal), reads=[Bl, B_rt], writes=[B_rmk])
            P.op("vector", lambda e: e.tensor_scalar(out=rmk, in0=rmk, scalar1=-1.0, scalar2=1e30, op0=ALU.add, op1=ALU.mult), reads=[B_rmk], writes=[B_rmk])
            for g in range(4):
                P.op("vector", lambda e, g=g: e.tensor_scalar(out=rml3[:, g, :], in0=le3[:, g, :], scalar1=rmk[:, g:g + 1], scalar2=None, op0=ALU.add),
                     reads=[Bl, B_rmk], writes=[B_rml])
            P.op("vector", lambda e: e.reduce_max(out=rt[:, 4:5], in_=rml, axis=AX.X), reads=[B_rml], writes=[B_rt])
            P.op("vector", lambda e: e.tensor_scalar(out=roh1, in0=rml, scalar1=rt[:, 4:5], scalar2=None, op0=ALU.is_equal), reads=[B_rml, B_rt], writes=[B_roh1])
            P.op("vector", lambda e: e.scalar_tensor_tensor(out=rml2, in0=roh1, scalar=-1e30, in1=rml, op0=ALU.mult, op1=ALU.add), reads=[B_roh1, B_rml], writes=[B_rml2])
            P.op("vector", lambda e: e.reduce_max(out=rt[:, 5:6], in_=rml2, axis=AX.X), reads=[B_rml2], writes=[B_rt])
            P.op("vector", lambda e: e.tensor_scalar(out=roh2, in0=rml2, scalar1=rt[:, 5:6], scalar2=None, op0=ALU.is_equal), reads=[B_rml2, B_rt], writes=[B_roh2])
            P.op("vector", lambda e: e.tensor_sub(out=rt[:, 6:7], in0=rt[:, 5:6], in1=rt[:, 4:5]), reads=[B_rt], writes=[B_rt])
            P.op("scalar", lambda e: e.activation(out=rt[:, 6:7], in_=rt[:, 6:7], func=AF.Exp), reads=[B_rt], writes=[B_rt])
            P.op("vector", lambda e: e.tensor_scalar_add(out=rt[:, 7:8], in0=rt[:, 6:7], scalar1=1.0), reads=[B_rt], writes=[B_rt])
            P.op("vector", lambda e: e.reciprocal(out=rt[:, 7:8], in_=rt[:, 7:8]), reads=[B_rt], writes=[B_rt])
            P.op("vector", lambda e: e.tensor_mul(out=rt[:, 8:9], in0=rt[:, 7:8], in1=rt[:, 6:7]), reads=[B_rt], writes=[B_rt])
            P.op("vector", lambda e: e.tensor_scalar(out=rt[:, 9:11], in0=rt[:, 7:9], scalar1=rt[:, 3:4], scalar2=None, op0=ALU.mult), reads=[B_rt], writes=[B_rt])
            P.op("vector", lambda e: e.tensor_scalar(out=roh1, in0=roh1, scalar1=rt[:, 9:10], scalar2=None, op0=ALU.mult), reads=[B_roh1, B_rt], writes=[B_roh1])
            P.op("vector", lambda e: e.scalar_tensor_tensor(out=gates[:, tl, :], in0=roh2, scalar=rt[:, 10:11], in1=roh1, op0=ALU.mult, op1=ALU.add),
                 reads=[B_roh2, B_roh1, B_rt], writes=[B_gates[tl]])

        def c1_block(tb):
            stt = statC[tb % 2]
            Bst = B_statC[tb % 2]
            tiles = [4 * tb + t for t in range(4)]
            P.dma("sync", dsem("xo"), x1[:, 4 * tb:4 * tb + 4, :], xov[tb], writes=[B_x1[tl] for tl in tiles])
            for t in range(4):
                P.op("scalar", lambda e, t=t: e.activation(out=junkC, in_=x1[:, 4 * tb + t, :], func=AF.Square, accum_out=stt[:, t:t + 1]),
                     reads=[B_x1[4 * tb + t]], writes=[B_junkC, Bst])
            stat_rstd(stt, Bst, 4, D, 1e-6)
            for t in range(4):
                P.op("gpsimd" if t % 2 == 0 else "vector",
                     lambda e, t=t: e.tensor_scalar(out=xnC[:, t, :], in0=x1[:, 4 * tb + t, :], scalar1=stt[:, 4 + t:5 + t], scalar2=None, op0=ALU.mult),
                     reads=[B_x1[4 * tb + t], Bst], writes=[B_xnC])
            for kk in range(4):
                bi = kk % 2
                pv = bk16(bi).rearrange("p (a c) -> p a c", a=2)
                fns = []
                for a in range(2):
                    k = 2 * kk + a
                    for t in range(4):
                        fns.append(lambda e, a=a, k=k, t=t, pv=pv: e.transpose(out=pv[:, a, t * 128:(t + 1) * 128], in_=xnC[:, t, k * 128:(k + 1) * 128], identity=ident_b))
                P.group("tensor", fns, reads=[B_xnC, B_ident_b], writes=[bkB[bi]])
                if kk % 2 == 0:
                    P.op("scalar", lambda e, kk=kk, pv=pv: e.copy(out=xnTC[:, 2 * kk:2 * kk + 2, :], in_=pv), reads=[bkB[bi]], writes=[B_xnTC])
                else:
                    P.op("vector", lambda e, kk=kk, pv=pv: e.tensor_copy(out=xnTC[:, 2 * kk:2 * kk + 2, :], in_=pv), reads=[bkB[bi]], writes=[B_xnTC])
            dq_eng = "sync" if tb < 2 else "gpsimd"
            P.dma(dq_eng, dsem("hd"), hgD,
                  lambda e: hg_all[bass.ds(dyn(e, tb)[2], 512), bass.ds(dyn(e, tb)[0], 512)].rearrange("(h p) c -> p h c", p=128),
                  reads=[B_hgall], writes=[B_hgD])
            P.dma(dq_eng, dsem("hr"), hgR,
                  lambda e: hg_all[bass.ds(dyn(e, tb)[3], 512), bass.ds(dyn(e, tb)[1], 512)].rearrange("(h p) c -> p h c", p=128),
                  reads=[B_hgall], writes=[B_hgR])
            P.dma(dq_eng, dsem("dd"), mixT[:, 4:8, :],
                  lambda e: da_all[bass.ds(dyn(e, tb)[2], 512), bass.ds(dyn(e, tb)[0], 512)].rearrange("(h p) c -> p h c", p=128),
                  reads=[B_daall], writes=B_mixT[4:8])
            for hh in range(4):
                fns = [lambda e, k=k, hh=hh: e.matmul(out=bk32(2), lhsT=wg_b[:, k, hh * 128:(hh + 1) * 128], rhs=xnTC[:, k, :], start=(k == 0), stop=(k == 7)) for k in range(8)]
                P.group("tensor", fns, reads=[B_wgb, B_xnTC], writes=[bkB[2]])
                P.op("scalar", lambda e: e.activation(out=sgate, in_=bk32(2), func=AF.Silu), reads=[bkB[2]], writes=[B_sgate])
                P.op("vector", lambda e, hh=hh: e.tensor_tensor(out=osum, in0=hgD[:, hh, :], in1=hgR[:, hh, ::-1], op=ALU.add), reads=[B_hgD, B_hgR], writes=[B_osum])
                P.op("gpsimd", lambda e: e.tensor_tensor(out=sq, in0=osum, in1=osum, op=ALU.mult), reads=[B_osum], writes=[B_sq])
                P.op("tensor", lambda e: e.matmul(out=bk32(3), lhsT=ones_b, rhs=sq, start=True, stop=True), reads=[B_onesb, B_sq], writes=[bkB[3]])
                P.op("vector", lambda e: e.tensor_scalar(out=rst, in0=bk32(3), scalar1=1.0 / 128.0, scalar2=1e-6, op0=ALU.mult, op1=ALU.add), reads=[bkB[3]], writes=[B_rst])
                P.op("scalar", lambda e: e.activation(out=rst, in_=rst, func=AF.Ln), reads=[B_rst], writes=[B_rst])
                P.op("scalar", lambda e: e.activation(out=rst, in_=rst, func=AF.Exp, scale=-0.5), reads=[B_rst], writes=[B_rst])
                P.op("vector", lambda e: e.scalar_tensor_tensor(out=osum, in0=osum, scalar=hgn[:, 0:1], in1=rst, op0=ALU.mult, op1=ALU.mult), reads=[B_osum, B_hgn, B_rst], writes=[B_osum])
                P.op("vector", lambda e, hh=hh: e.tensor_tensor(out=mixT[:, hh, :], in0=osum, in1=sgate, op=ALU.mult), reads=[B_osum, B_sgate], writes=[B_mixT[hh]])
            for t in range(4):
                for dh in range(2):
                    bank = 4 + (2 * t + dh) % 2
                    fns = [lambda e, f=f, t=t, dh=dh, bank=bank: e.matmul(out=bk32(bank), lhsT=mixT[:, f, t * 128:(t + 1) * 128], rhs=wo_b[:, f, dh * 512:(dh + 1) * 512],
                                                                          start=(f == 0), stop=(f == 7)) for f in range(8)]
                    P.group("tensor", fns, reads=B_mixT + [B_wo], writes=[bkB[bank]])
                    P.op("vector", lambda e, t=t, dh=dh, bank=bank: e.tensor_tensor(out=x1[:, 4 * tb + t, dh * 512:(dh + 1) * 512], in0=bk32(bank),
                                                                                     in1=x1[:, 4 * tb + t, dh * 512:(dh + 1) * 512], op=ALU.add),
                         reads=[bkB[bank], B_x1[4 * tb + t]], writes=[B_x1[4 * tb + t]])
            st2 = stat2[tb % 2]
            Bs2 = B_stat2[tb % 2]
            for t in range(4):
                P.op("scalar", lambda e, t=t: e.activation(out=junkC, in_=x1[:, 4 * tb + t, :], func=AF.Square, accum_out=st2[:, t:t + 1]),
                     reads=[B_x1[4 * tb + t]], writes=[B_junkC, Bs2])
            stat_rstd(st2, Bs2, 4, D, 1e-6)
            pT8 = pair_t[3][:, :, :].rearrange("p a (k c) -> p (a k) c", k=4)
            for t in range(4):
                tl = 4 * tb + t
                P.op("vector", lambda e, t=t, tl=tl: e.scalar_tensor_tensor(out=h2g, in0=x1[:, tl, :], scalar=st2[:, 4 + t:5 + t], in1=g2b, op0=ALU.mult, op1=ALU.mult),
                     reads=[B_x1[tl], Bs2, B_g2b], writes=[B_h2g])
                fns = [lambda e, k=k: e.transpose(out=pT8[:, k, :], in_=h2g[:, k * 128:(k + 1) * 128], identity=ident_f) for k in range(8)]
                P.group("tensor", fns, reads=[B_h2g, B_ident_f], writes=[bkB[6], bkB[7]])
                P.op("scalar", lambda e: e.copy(out=h2Tf, in_=pT8), reads=[bkB[6], bkB[7]], writes=[B_h2Tf])
                P.op("vector", lambda e, tl=tl: e.tensor_copy(out=h2T[:, :, tl * 128:(tl + 1) * 128], in_=pT8), reads=[bkB[6], bkB[7]], writes=[B_h2T[tl]])
                fns = [lambda e, k=k: e.matmul(out=bk32(2)[:, 0:36], lhsT=h2Tf[:, k, :], rhs=wr_f[:, k, :], start=(k == 0), stop=(k == 7)) for k in range(8)]
                P.group("tensor", fns, reads=[B_h2Tf, B_wr], writes=[bkB[2]])
                P.op("vector", lambda e, tl=tl: e.tensor_tensor(out=lg[:, tl, :], in0=bk32(2)[:, 0:36], in1=brb, op=ALU.add), reads=[bkB[2], B_brb], writes=[B_lg[tl]])
                route_tile(tl)

        for tb in range(NBC):
            c1_block(tb)

        if "x1" in debug:
            final_toks.append(P.dma("sync", dsem("dbg5"), dout("dbg_x1", [128, NTC, 1024], F32), x1, reads=B_x1))
            final_toks.append(P.dma("sync", dsem("dbg6"), dout("dbg_gates", [128, NTC, 32], F32), gates, reads=B_gates))
            final_toks.append(P.dma("sync", dsem("dbg7"), dout("dbg_h2T", [128, 8, SC], BF16), h2T, reads=B_h2T))

        P.barrier([d for n_, d in sync_sems.items() if not n_.startswith("cc")])
        regC3 = Region(P_RES, regC.pos, False)
        wge = [carve(regC3, [128, 8, 512], BF16) for _ in range(2)]
        wue = [carve(regC3, [128, 8, 512], BF16) for _ in range(2)]
        wde = [carve(regC3, [128, 4, 1024], BF16) for _ in range(2)]
        aT = [carve(regC3, [128, 4, 512], BF16) for _ in range(2)]
        sgs = [carve(regC3, [128, 512], F32) for _ in range(2)]
        fo = [carve(regC3, [128, 1024], F32) for _ in range(2)]
        junk3 = carve(regC3, [128, 1024], BF16)
        stf = carve(regC3, [128, 2 * NTC], F32)
        B_wge, B_wue, B_wde, B_aT, B_sgs, B_fo = [[Buf(), Buf()] for _ in range(6)]
        B_junk3, B_stf = Buf(), Buf()

        def load_expert(ex):
            sl = ex % 2
            P.dma("gpsimd", dsem(f"wg{sl}"), wge[sl], wgated[ex].rearrange("(k p) c -> p k c", p=128), writes=[B_wge[sl]])
            P.dma("gpsimd", dsem(f"wu{sl}"), wue[sl], wupd[ex].rearrange("(k p) c -> p k c", p=128), writes=[B_wue[sl]])
            P.dma("gpsimd", dsem(f"wd{sl}"), wde[sl], wdownd[ex].rearrange("(k p) c -> p k c", p=128), writes=[B_wde[sl]])

        cnt = [0]

        def expert(ex):
            sl = ex % 2
            if ex + 1 < N_EXPERTS:
                load_expert(ex + 1)
            for tb in range(NBC):
                asl = cnt[0] % 2
                cnt[0] += 1
                for jc in range(4):
                    pb = jc % 2
                    rd = [B_h2T[4 * tb + t] for t in range(4)]
                    fns = [lambda e, k=k, jc=jc, pb=pb, tb=tb: e.matmul(out=bk32(2 * pb), lhsT=wge[sl][:, k, jc * 128:(jc + 1) * 128], rhs=h2T[:, k, tb * 512:(tb + 1) * 512],
                                                                 start=(k == 0), stop=(k == 7)) for k in range(8)]
                    P.group("tensor", fns, reads=[B_wge[sl]] + rd, writes=[bkB[2 * pb]])
                    fns = [lambda e, k=k, jc=jc, pb=pb, tb=tb: e.matmul(out=bk32(2 * pb + 1), lhsT=wue[sl][:, k, jc * 128:(jc + 1) * 128], rhs=h2T[:, k, tb * 512:(tb + 1) * 512],
                                                                 start=(k == 0), stop=(k == 7)) for k in range(8)]
                    P.group("tensor", fns, reads=[B_wue[sl]] + rd, writes=[bkB[2 * pb + 1]])
                    P.op("scalar", lambda e, pb=pb: e.activation(out=sgs[pb], in_=bk32(2 * pb), func=AF.Silu), reads=[bkB[2 * pb]], writes=[B_sgs[pb]])
                    P.op("vector", lambda e, pb=pb, jc=jc, asl=asl: e.tensor_tensor(out=aT[asl][:, jc, :], in0=sgs[pb], in1=bk32(2 * pb + 1), op=ALU.mult),
                         reads=[B_sgs[pb], bkB[2 * pb + 1]], writes=[B_aT[asl]])
                for t in range(4):
                    tl = 4 * tb + t
                    for dh in range(2):
                        bank = 4 + (2 * t + dh) % 4
                        fns = [lambda e, jc=jc, t=t, dh=dh, bank=bank, asl=asl: e.matmul(out=bk32(bank), lhsT=aT[asl][:, jc, t * 128:(t + 1) * 128],
                                                                                       rhs=wde[sl][:, jc, dh * 512:(dh + 1) * 512], start=(jc == 0), stop=(jc == 3)) for jc in range(4)]
                        P.group("tensor", fns, reads=[B_aT[asl], B_wde[sl]], writes=[bkB[bank]])
                        P.op("vector", lambda e, tl=tl, dh=dh, bank=bank: e.scalar_tensor_tensor(out=x1[:, tl, dh * 512:(dh + 1) * 512], in0=bk32(bank), scalar=gates[:, tl, ex:ex + 1],
                                                                                                 in1=x1[:, tl, dh * 512:(dh + 1) * 512], op0=ALU.mult, op1=ALU.add),
                             reads=[bkB[bank], B_gates[tl], B_x1[tl]], writes=[B_x1[tl]])

        load_expert(0)
        for ex in range(N_EXPERTS):
            expert(ex)

        outv = outd.rearrange("(t p) d -> p t d", p=128)
        for tl in range(NTC):
            P.op("scalar", lambda e, tl=tl: e.activation(out=junk3, in_=x1[:, tl, :], func=AF.Square, accum_out=stf[:, tl:tl + 1]), reads=[B_x1[tl]], writes=[B_junk3, B_stf])
        stat_rstd(stf, B_stf, NTC, D, 1e-6)

        def fin_tile(tl):
            sl = tl % 2
            P.op("vector",
                 lambda e: e.scalar_tensor_tensor(out=fo[sl], in0=x1[:, tl, :], scalar=stf[:, NTC + tl:NTC + tl + 1], in1=gfb, op0=ALU.mult, op1=ALU.mult),
                 reads=[B_x1[tl], B_stf, B_gfb], writes=[B_fo[sl]])
            final_toks.append(P.dma("sync", dsem(f"out{sl}"), outv[:, tl, :], fo[sl], reads=[B_fo[sl]]))

        for tl in range(NTC):
            fin_tile(tl)

        P.wait_all("sync", final_toks)
        with nc.Block() as block:
            P.emit_all(block)
    return nc


def _consts():
    ident = np.eye(128, dtype=np.float32)
    blk1 = np.zeros((128, 128), np.float32)
    blk1[:64, :64] = 1.0
    blk1[64:, 64:] = 1.0
    ones = np.ones((128, 128), np.float32)
    s = np.arange(128)[:, None]
    t = np.arange(128)[None, :]
    tri = ((s <= t) & ((s // 64) == (t // 64))).astype(np.float32)
    trim = np.ascontiguousarray(np.tile(tri, (1, 4)))
    scm = np.ones((128, 512), np.float32)
    scm[:, ::64] = 0.0
    return ident, blk1, ones, trim, scm


def make_in_maps(inputs, S):
    f32 = lambda a: np.asarray(a, np.float32)
    x = f32(inputs["x"])[0]
    pos = np.asarray(inputs["positions"], np.int32)[0]
    w_in = f32(inputs["w_in"])[0]
    lbs = f32(inputs["hg_lower_bounds"])
    ident, blk1, ones, trim, scm = _consts()
    SC = S // NCORES
    g1 = np.ascontiguousarray(f32(inputs["norm1_gain"])[0].reshape(8, 128).T)
    perm = np.arange(128)
    for m in range(2):
        for d in range(8):
            perm[m * 64 + d] = m * 64 + d + 8
            perm[m * 64 + d + 8] = m * 64 + d
    shared = {
        "g1": g1, "ident": ident, "blk1": blk1, "ones": ones, "trim": trim, "scm": scm,
        "dl": np.ascontiguousarray(f32(inputs["diff_lambda"])[0].reshape(1, 256)),
        "sgain": np.ascontiguousarray(f32(inputs["diff_subln_gain"])[0].reshape(1, 128)),
        "wg": np.ascontiguousarray(w_in[:, 2048:2560]),
        "wo": np.ascontiguousarray(f32(inputs["w_out"])[0]),
        "hgn": np.ascontiguousarray(f32(inputs["hg_norm_gain"])[0].reshape(128, 1)),
        "g2": np.ascontiguousarray(f32(inputs["norm2_gain"])[0].reshape(1, D)),
        "gf": np.ascontiguousarray(f32(inputs["final_norm_gain"]).reshape(1, D)),
        "wr": np.ascontiguousarray(np.concatenate([f32(inputs["router_group_w"])[0], f32(inputs["router_expert_w"])[0]], axis=1)),
        "br": np.ascontiguousarray(np.concatenate([f32(inputs["router_group_b"])[0], f32(inputs["router_expert_b"])[0]]).reshape(1, 36)),
        "wgate": np.ascontiguousarray(f32(inputs["moe_w_gate"])[0]),
        "wup": np.ascontiguousarray(f32(inputs["moe_w_up"])[0]),
        "wdown": np.ascontiguousarray(f32(inputs["moe_w_down"])[0]),
    }
    in_maps = []
    for c in range(NCORES):
        h, j = c % 4, c // 4
        xs = x[::-1] if j else x
        ps = pos[::-1] if j else pos
        hq = w_in[:, h * 128:(h + 1) * 128]
        hf = w_in[:, 512 * (1 + j) + h * 128: 512 * (1 + j) + (h + 1) * 128]
        hi = w_in[:, 1536 + h * 128:1536 + (h + 1) * 128]
        dq = w_in[:, 2560 + h * 128:2560 + (h + 1) * 128]
        dk = w_in[:, 3072 + h * 128:3072 + (h + 1) * 128]
        dv = w_in[:, 3584 + h * 128:3584 + (h + 1) * 128]
        wa = np.concatenate([hq, hf, dk, dk[:, perm], dq, dq[:, perm], hi, dv], axis=1)
        xo = x[c * SC:(c + 1) * SC]
        if j:
            xo = xo[::-1]
        m = dict(shared)
        m.update({
            "xs": np.ascontiguousarray(xs),
            "pos": np.ascontiguousarray(ps.reshape(S // 128, 128)),
            "wa": np.ascontiguousarray(wa),
            "lbp": np.ascontiguousarray(lbs[j, :, h * 128:(h + 1) * 128].T),
            "xo": np.ascontiguousarray(xo),
        })
        in_maps.append(m)
    return in_maps


def assemble(results):
    outs = []
    for c in range(NCORES):
        o = np.asarray(results[c]["out"], np.float32)
        if c // 4:
            o = o[::-1]
        outs.append(o)
    return np.concatenate(outs, axis=0)[None]


def kernel(**inputs):
    S = int(np.asarray(inputs["x"]).shape[1])
    nc = build(S)
    in_maps = make_in_maps(inputs, S)
    res = run_bass_kernel_spmd(nc, in_maps, core_ids=list(range(NCORES)))
    return np.ascontiguousarray(assemble(res.results)).astype(np.float32)
```

```python
import math
from contextlib import ExitStack

import numpy as np
import concourse.bass as bass
import concourse.mybir as mybir
from concourse.bass_utils import run_bass_kernel_spmd

F32 = mybir.dt.float32
BF16 = mybir.dt.bfloat16
I32 = mybir.dt.int32
ALU = mybir.AluOpType
AF = mybir.ActivationFunctionType
AX = mybir.AxisListType

D = 1024
NCORES = 8
ROPE_THETA = 500000.0
N_EXPERTS = 32
D_EXPERT = 512
LAMBDA_INIT = 0.8 - 0.6 * math.exp(0.0)
TWO_PI = float(2.0 * np.pi)
PI = float(np.pi)
SEM_ROT = 20000


class Buf:
    __slots__ = ("name", "w", "rs", "excl")

    def __init__(self, name="", excl=False):
        self.name = name
        self.w = None
        self.rs = []
        self.excl = excl


class Prog:
    ENGS = ["tensor", "vector", "scalar", "gpsimd", "sync"]

    def __init__(self, nc, stack):
        self.nc = nc
        self.stack = stack
        self.ops = {e: [] for e in self.ENGS}
        self.cur = {}
        self.allsems = []
        self.nsem = 0
        for e in self.ENGS:
            self._new_sem(e)
        self.waited = {e: {} for e in self.ENGS}
        self.n_ops = 0

    def _new_sem(self, e):
        s = self.stack.enter_context(self.nc.semaphore(f"tl_{e}_{self.nsem}"))
        self.nsem += 1
        self.cur[e] = [s, 0]
        self.allsems.append((e, self.cur[e]))

    def _collect(self, eng, reads, writes, extra):
        deps = []
        for b in reads:
            if b.w is not None:
                deps.append(b.w)
            if b.excl:
                deps.extend(r for r in b.rs if r[0] != eng)
        for b in writes:
            if b.w is not None:
                deps.append(b.w)
            deps.extend(b.rs)
        deps.extend([d for d in extra if d is not None])
        waits = []
        wd = self.waited[eng]
        for (feng, sem, n) in deps:
            if feng == eng and eng == "tensor":
                continue
            key = id(sem)
            if wd.get(key, 0) >= n:
                continue
            wd[key] = n
            waits.append((sem, n))
        return waits

    @staticmethod
    def _update(tok, reads, writes):
        for b in reads:
            b.rs.append(tok)
        for b in writes:
            b.w = tok
            b.rs = []

    def op(self, eng, fn, reads=(), writes=(), extra=()):
        waits = self._collect(eng, reads, writes, extra)
        c = self.cur[eng]
        if c[1] >= SEM_ROT:
            self._new_sem(eng)
            c = self.cur[eng]
        c[1] += 1
        sem = c[0]
        tok = (eng, sem, c[1])
        self._update(tok, reads, writes)

        def emit(e, waits=waits, fn=fn, sem=sem):
            for s, v in waits:
                e.wait_ge(s, v)
            fn(e).then_inc(sem, 1)
        self.ops[eng].append(emit)
        self.n_ops += 1
        return tok

    def group(self, eng, fns, reads=(), writes=(), extra=()):
        n = len(fns)
        if n == 1:
            return self.op(eng, fns[0], reads, writes, extra)
        waits = self._collect(eng, reads, writes, extra)
        first = fns[0]

        def emit0(e, waits=waits, fn=first):
            for s, v in waits:
                e.wait_ge(s, v)
            fn(e)
        self.ops[eng].append(emit0)
        for fn in fns[1:-1]:
            self.ops[eng].append(lambda e, fn=fn: fn(e))
        self.n_ops += n - 1
        return self.op(eng, fns[-1], reads, writes, ())

    def new_dma_sem(self, name):
        s = self.stack.enter_context(self.nc.semaphore(name))
        return [s, 0]

    def dma(self, eng, ds, out, in_, reads=(), writes=(), extra=()):
        waits = self._collect(eng, reads, writes, extra)
        ds[1] += 16
        tok = ("dma", ds[0], ds[1])
        self._update(tok, reads, writes)

        def emit(e, waits=waits, out=out, in_=in_, s=ds[0]):
            for sm, v in waits:
                e.wait_ge(sm, v)
            o_ap = out(e) if callable(out) else out
            i_ap = in_(e) if callable(in_) else in_
            try:
                e.dma_start(out=o_ap, in_=i_ap).then_inc(s, 16)
            except Exception:
                print("DMA FAIL", eng, str(o_ap)[:300], "<<<<", str(i_ap)[:300])
                raise
        self.ops[eng].append(emit)
        self.n_ops += 1
        return tok

    def custom(self, eng, fn, ds, inc, reads=(), writes=(), extra=()):
        waits = self._collect(eng, reads, writes, extra)
        ds[1] += (1 if inc is None else inc)
        tok = ("cc", ds[0], ds[1])
        self._update(tok, reads, writes)

        def emit(e, waits=waits, fn=fn, s=ds[0], inc=inc):
            for sm, v in waits:
                e.wait_ge(sm, v)
            ins = fn(e)
            if inc is None:
                ins.then_inc(s)
            else:
                ins.then_inc(s, inc)
        self.ops[eng].append(emit)
        return tok

    def barrier(self, dma_sems):
        marks = [(f, c[0], c[1]) for (f, c) in self.allsems if c[1] > 0]
        marks += [("dma", d[0], d[1]) for d in dma_sems if d[1] > 0]
        for eng in self.ENGS:
            wd = self.waited[eng]
            waits = []
            for (f, sem, n) in marks:
                if f == eng:
                    continue
                if wd.get(id(sem), 0) >= n:
                    continue
                wd[id(sem)] = n
                waits.append((sem, n))

            def emit(e, waits=waits):
                for s, v in waits:
                    e.wait_ge(s, v)
            self.ops[eng].append(emit)

    def wait_all(self, eng, toks):
        waits = [(t[1], t[2]) for t in toks if t is not None]

        def emit(e, waits=waits):
            for s, v in waits:
                e.wait_ge(s, v)
        self.ops[eng].append(emit)

    def emit_all(self, block):
        for name in self.ENGS:
            lst = self.ops[name]

            def body(e, lst=lst):
                for f in lst:
                    f(e)
            getattr(block, name)(body)


ARENA_BYTES = 204 * 1024
P_RES = 8 * 1024


def _dsize(dt):
    return 4 if dt in (F32, I32) else 2


def build(S, debug=()):
    NB = S // 512
    NT = S // 128
    SH = S // 2
    NQB = SH // 512
    SC = S // NCORES
    NTC = SC // 128
    NBC = SC // 512
    VW = 130
    nc = bass.Bass("TRN2", target_bir_lowering=False)

    def din(name, shape, dt=F32):
        return nc.dram_tensor(name, shape, dt, kind="ExternalInput").ap()

    def dout(name, shape, dt=F32):
        return nc.dram_tensor(name, shape, dt, kind="ExternalOutput").ap()

    xs = din("xs", [S, D])
    posd = din("pos", [NT, 128], I32)
    wa = din("wa", [D, 1024])
    g1d = din("g1", [128, 8])
    lbpd = din("lbp", [128, 2])
    identd = din("ident", [128, 128])
    blk1d = din("blk1", [128, 128])
    onesd = din("ones", [128, 128])
    trimd = din("trim", [128, 512])
    scmd = din("scm", [128, 512])
    dld = din("dl", [1, 256])
    sgaind = din("sgain", [1, 128])
    xod = din("xo", [SC, D])
    wgd = din("wg", [D, 512])
    wod = din("wo", [D, D])
    hgnd = din("hgn", [128, 1])
    g2d = din("g2", [1, D])
    gfd = din("gf", [1, D])
    wrd = din("wr", [D, 36])
    brd = din("br", [1, 36])
    wgated = din("wgate", [N_EXPERTS, D, D_EXPERT])
    wupd = din("wup", [N_EXPERTS, D, D_EXPERT])
    wdownd = din("wdown", [N_EXPERTS, D_EXPERT, D])
    outd = dout("out", [SC, D])

    hg_part = nc.dram_tensor("hg_part", [128, S], BF16)
    da_part = nc.dram_tensor("da_part", [128, SH], BF16)
    hg_all = nc.dram_tensor("hg_all", [NCORES * 128, S], BF16)
    da_all = nc.dram_tensor("da_all", [NCORES * 128, SH], BF16)

    final_toks = []
    with ExitStack() as st:
        P = Prog(nc, st)
        arena = st.enter_context(nc.sbuf_tensor("arena", [128, ARENA_BYTES // 2], BF16))

        class Region:
            def __init__(self, lo, hi, down):
                self.lo, self.hi, self.down = lo, hi, down
                self.pos = hi if down else lo

        def carve(reg, shape, dt):
            npart = shape[0]
            nel = 1
            for d_ in shape[1:]:
                nel *= d_
            nbytes = (nel * _dsize(dt) + 63) // 64 * 64
            if reg.down:
                reg.pos -= nbytes
                start = reg.pos
                assert start >= reg.lo, "arena overflow (down)"
            else:
                start = reg.pos
                reg.pos += nbytes
                assert reg.pos <= reg.hi, "arena overflow (up)"
            ap = arena[0:npart, start // 2:(start + nbytes) // 2]
            if dt != BF16:
                ap = ap.bitcast(dt)
            ap = ap[:, 0:nel]
            if len(shape) == 3:
                ap = ap.rearrange("p (a b) -> p a b", a=shape[1])
            return ap

        regP = Region(0, P_RES, False)

        pair_t = [st.enter_context(nc.psum_tensor(f"bkp{i}", [128, 2, 512], F32)) for i in range(4)]
        bkB = [Buf(f"bk{i}", excl=True) for i in range(8)]

        def bk32(i):
            return pair_t[i // 2][:, i % 2, :]

        def bk16(i):
            return pair_t[i // 2][:, i % 2, :].bitcast(BF16)

        sync_sems = {}

        def dsem(name):
            if name not in sync_sems:
                sync_sems[name] = P.new_dma_sem("d_" + name)
            return sync_sems[name]

        def stat_rstd(stt, Bst, n, dim, eps):
            P.op("vector", lambda e: e.tensor_scalar(out=stt[:, n:2 * n], in0=stt[:, 0:n], scalar1=1.0 / dim, scalar2=eps, op0=ALU.mult, op1=ALU.add),
                 reads=[Bst], writes=[Bst])
            P.op("scalar", lambda e: e.activation(out=stt[:, n:2 * n], in_=stt[:, n:2 * n], func=AF.Ln), reads=[Bst], writes=[Bst])
            P.op("scalar", lambda e: e.activation(out=stt[:, n:2 * n], in_=stt[:, n:2 * n], func=AF.Exp, scale=-0.5), reads=[Bst], writes=[Bst])

        ident_f = carve(regP, [128, 128], F32)
        ident_b = carve(regP, [128, 128], BF16)
        blk1_b = carve(regP, [128, 128], BF16)
        ones_b = carve(regP, [128, 128], BF16)
        g1 = carve(regP, [128, 8], F32)
        B_ident_f, B_ident_b, B_blk1, B_onesb, B_g1 = [Buf() for _ in range(5)]
        P.dma("sync", dsem("c0"), ident_f, identd, writes=[B_ident_f])
        P.dma("gpsimd", dsem("c1"), ident_b, identd, writes=[B_ident_b])
        P.dma("gpsimd", dsem("c3"), blk1_b, blk1d, writes=[B_blk1])
        P.dma("gpsimd", dsem("c2"), ones_b, onesd, writes=[B_onesb])
        P.dma("sync", dsem("c6"), g1, g1d, writes=[B_g1])

        regA = Region(P_RES, ARENA_BYTES, True)
        regA1 = Region(P_RES, ARENA_BYTES, False)
        K_sb = carve(regA, [128, S], BF16)
        V_sb = carve(regA, [128, NT, VW], BF16)
        Q_sb = carve(regA, [128, SH], BF16)
        cos8 = carve(regA, [128, NT * 8], BF16)
        sin8 = carve(regA, [128, NT * 8], BF16)
        nsin8 = carve(regA, [128, NT * 8], BF16)
        mx = carve(regA, [128, 4], F32)
        lbp = carve(regA, [128, 2], F32)
        lbt = carve(regA, [128, 4], F32)
        Sseq = [carve(regA, [128, 9, 128], F32) for _ in range(2)]
        B_K = [Buf() for _ in range(NB)]
        B_V = [Buf() for _ in range(NB)]
        B_Q = [Buf() for _ in range(NQB)]
        B_Vones, B_lbp, B_lbt, B_mx, B_cos, B_sin = [Buf() for _ in range(6)]
        B_Sseq = [Buf(), Buf()]

        regA0 = Region(regA.hi - S * 2, regA.hi, False)
        posi = carve(regA0, [NT, 128], I32)
        posf = carve(regA0, [NT, 128], F32)
        posT = carve(regA0, [128, NT], F32)
        ang = carve(regA0, [128, NT * 8], F32)
        rr = carve(regA0, [128, NT * 8], F32)
        ki = carve(regA0, [128, NT * 8], I32)
        B_pos, B_posT, B_ang, B_rr, B_ki = [Buf() for _ in range(5)]

        P.dma("sync", dsem("c7"), lbp, lbpd, writes=[B_lbp])
        P.op("vector", lambda e: e.tensor_sub(out=lbt[:, 0:1], in0=lbp[:, 1:2], in1=lbp[:, 0:1]), reads=[B_lbp], writes=[B_lbt])
        P.op("scalar", lambda e: e.activation(out=lbt[:, 1:2], in_=lbt[:, 0:1], func=AF.Exp), reads=[B_lbt], writes=[B_lbt])
        P.op("vector", lambda e: e.tensor_scalar_add(out=lbt[:, 1:2], in0=lbt[:, 1:2], scalar1=1.0), reads=[B_lbt], writes=[B_lbt])
        P.op("vector", lambda e: e.reciprocal(out=lbt[:, 2:3], in_=lbt[:, 1:2]), reads=[B_lbt], writes=[B_lbt])
        P.op("vector", lambda e: e.tensor_scalar_mul(out=lbt[:, 3:4], in0=lbt[:, 2:3], scalar1=-1.0), reads=[B_lbt], writes=[B_lbt])
        oml = lbt[:, 2:3]
        noml = lbt[:, 3:4]
        P.op("gpsimd", lambda e: e.memset(V_sb[:, :, 128:130], 1.0), writes=[B_Vones])
        P.op("gpsimd", lambda e: e.memset(mx, 0.0), writes=[B_mx])
        P.op("gpsimd", lambda e: e.memset(Sseq[1][:, 8, :], 0.0), writes=[B_Sseq[1]])

        wa_b = carve(regA1, [128, 8, 1024], BF16)
        xbuf = [carve(regA1, [128, 2, 1024], F32) for _ in range(2)]
        xn = carve(regA1, [128, 2, 1024], BF16)
        xnT = carve(regA1, [128, 8, 512], BF16)
        junk = carve(regA1, [128, 1024], BF16)
        trim = carve(regA1, [128, 512], F32)
        scm = carve(regA1, [128, 512], F32)
        tabC = [carve(regA1, [128, 4, 128], BF16) for _ in range(2)]
        tabS = [carve(regA1, [128, 4, 128], BF16) for _ in range(2)]
        CS = [carve(regA1, [128, 2, 512], BF16) for _ in range(2)]
        stat = [carve(regA1, [128, 8], F32) for _ in range(2)]
        qT_f = carve(regA1, [128, 512], F32)
        e1 = carve(regA1, [128, 512], F32)
        sgn = carve(regA1, [128, 512], F32)
        lf = carve(regA1, [128, 512], F32)
        bcs = carve(regA1, [128, 512], F32)
        eb = carve(regA1, [128, 512], F32)
        enb = carve(regA1, [128, 512], F32)
        qt_b = carve(regA1, [128, 512], BF16)
        kt_b = carve(regA1, [128, 512], BF16)
        kt_tok = carve(regA1, [128, 4, 128], BF16)
        v_hg = carve(regA1, [128, 4, 128], BF16)
        S_bf = carve(regA1, [128, 8, 128], BF16)
        Ttmp = carve(regA1, [128, 128], F32)
        PT_b = carve(regA1, [128, 512], BF16)
        o_bf = carve(regA1, [128, 4, 128], BF16)
        oT_sb = [carve(regA1, [128, 4, 128], BF16) for _ in range(2)]
        t1, t2, sqk = e1, lf, PT_b
        (B_wa, B_xn, B_xnT, B_junk, B_trim, B_scm, B_qT, B_e1, B_sgn, B_lf, B_bcs, B_eb, B_enb, B_qt, B_kt, B_kttok, B_vhg,
         B_Sbf, B_T, B_PT, B_obf) = [Buf() for _ in range(21)]
        B_t1, B_t2, B_sqk = B_e1, B_lf, B_PT
        B_xbuf, B_tab, B_CS, B_stat, B_oT = [[Buf(), Buf()] for _ in range(5)]
        P.dma("sync", dsem("c4"), trim, trimd, writes=[B_trim])
        P.dma("sync", dsem("c5"), scm, scmd, writes=[B_scm])

        wav = wa.rearrange("(k p) c -> p k c", p=128)
        for r4 in range(4):
            sl = r4 % 2
            P.dma("sync", dsem(f"x{sl}"), xbuf[sl], wav[:, 2 * r4:2 * r4 + 2, :], writes=[B_xbuf[sl]])
            for kk in range(2):
                k = 2 * r4 + kk
                P.op("vector" if kk == 0 else "gpsimd",
                     lambda e, sl=sl, kk=kk, k=k: e.tensor_scalar(out=wa_b[:, k, :], in0=xbuf[sl][:, kk, :], scalar1=g1[:, k:k + 1], scalar2=None, op0=ALU.mult),
                     reads=[B_xbuf[sl], B_g1], writes=[B_wa])

        P.dma("sync", dsem("c8"), posi, posd, writes=[B_pos])
        P.op("vector", lambda e: e.tensor_copy(out=posf, in_=posi), reads=[B_pos], writes=[B_pos])
        P.op("tensor", lambda e: e.transpose(out=bk32(0)[:, 0:NT], in_=posf, identity=ident_f[0:NT, 0:NT]),
             reads=[B_pos, B_ident_f], writes=[bkB[0]])
        P.op("vector", lambda e: e.tensor_copy(out=posT, in_=bk32(0)[:, 0:NT]), reads=[bkB[0]], writes=[B_posT])
        ang3 = ang.rearrange("p (t i) -> p t i", i=8)
        for i in range(8):
            invf = float(np.float32(ROPE_THETA) ** np.float32(-(2.0 * i) / 16.0))
            P.op("vector", lambda e, i=i, invf=invf: e.tensor_scalar(out=ang3[:, :, i], in0=posT, scalar1=invf, scalar2=None, op0=ALU.mult),
                 reads=[B_posT], writes=[B_ang])

        def sin_of(dst, B_dst, shift):
            P.op("vector", lambda e: e.tensor_scalar(out=rr, in0=ang, scalar1=shift, scalar2=1.0 / TWO_PI, op0=ALU.add, op1=ALU.mult),
                 reads=[B_ang], writes=[B_rr])
            P.op("vector", lambda e: e.tensor_copy(out=ki, in_=rr), reads=[B_rr], writes=[B_ki])
            P.op("vector", lambda e: e.tensor_copy(out=rr, in_=ki), reads=[B_ki], writes=[B_rr])
            P.op("vector", lambda e: e.tensor_scalar(out=rr, in0=rr, scalar1=-TWO_PI, scalar2=shift, op0=ALU.mult, op1=ALU.add),
                 reads=[B_rr], writes=[B_rr])
            P.op("vector", lambda e: e.tensor_add(out=rr, in0=rr, in1=ang), reads=[B_rr, B_ang], writes=[B_rr])
            P.op("vector", lambda e: e.tensor_scalar(out=rr, in0=rr, scalar1=-PI, scalar2=PI, op0=ALU.max, op1=ALU.min),
                 reads=[B_rr], writes=[B_rr])
            P.op("scalar", lambda e: e.activation(out=dst, in_=rr, func=AF.Sin), reads=[B_rr], writes=[B_dst])

        sin_of(cos8, B_cos, PI / 2.0)
        sin_of(sin8, B_sin, 0.0)
        P.op("vector", lambda e: e.tensor_scalar_mul(out=nsin8, in0=sin8, scalar1=-1.0), reads=[B_sin], writes=[B_sin])
        cos3 = cos8.rearrange("p (t i) -> p t i", i=8)
        sin3 = sin8.rearrange("p (t i) -> p t i", i=8)
        nsin3 = nsin8.rearrange("p (t i) -> p t i", i=8)
        for i in range(2):
            P.op("gpsimd", lambda e, i=i: e.memset(tabC[i], 1.0), writes=[B_tab[i]])
            P.op("gpsimd", lambda e, i=i: e.memset(tabS[i], 0.0), writes=[B_tab[i]])
        P.barrier(sync_sems.values())

        xsv = xs.rearrange("(g t p) d -> g p t d", p=128, t=2)

        def load_x(g):
            P.dma("sync", dsem(f"x{g % 2}"), xbuf[g % 2], xsv[g], writes=[B_xbuf[g % 2]])

        qt2 = [qt_b, carve(regA1, [128, 512], BF16)]
        kt2 = [kt_b, carve(regA1, [128, 512], BF16)]
        kttok2 = [kt_tok, carve(regA1, [128, 4, 128], BF16)]
        vhg2 = [v_hg, carve(regA1, [128, 4, 128], BF16)]
        ebl2 = [carve(regA1, [128, 8], F32) for _ in range(2)]
        sqk2 = carve(regA1, [128, 512], BF16)
        B_sqk2 = Buf()
        B_qt2, B_kt2, B_kttok2, B_vhg2, B_ebl2 = [[Buf(), Buf()] for _ in range(5)]
        assert regA1.pos <= regA.pos, f"phase A1 does not fit: {regA1.pos} > {regA.pos}"

        def front_stages(b):
            pb_ = b % 2
            stt = stat[pb_]
            Bst = B_stat[pb_]
            qtb, ktb, kttok, vhg, ebl = qt2[pb_], kt2[pb_], kttok2[pb_], vhg2[pb_], ebl2[pb_]
            Bqt, Bkt, Bkttok, Bvhg, Bebl = B_qt2[pb_], B_kt2[pb_], B_kttok2[pb_], B_vhg2[pb_], B_ebl2[pb_]
            xT = xnT
            BxT = B_xnT
            tb = pb_

            def xpart(hb):
                g = 2 * b + hb
                if g + 1 < 2 * NB:
                    load_x(g + 1)
                xb = xbuf[g % 2]
                Bx = B_xbuf[g % 2]
                for tt in range(2):
                    t = 2 * hb + tt
                    P.op("scalar", lambda e, tt=tt, t=t: e.activation(out=junk, in_=xb[:, tt, :], func=AF.Square, accum_out=stt[:, t:t + 1]),
                         reads=[Bx], writes=[B_junk, Bst])
                P.op("vector", lambda e: e.tensor_scalar(out=stt[:, 4 + 2 * hb:6 + 2 * hb], in0=stt[:, 2 * hb:2 * hb + 2], scalar1=1.0 / D, scalar2=1e-6, op0=ALU.mult, op1=ALU.add),
                     reads=[Bst], writes=[Bst])
                P.op("scalar", lambda e: e.activation(out=stt[:, 4 + 2 * hb:6 + 2 * hb], in_=stt[:, 4 + 2 * hb:6 + 2 * hb], func=AF.Ln), reads=[Bst], writes=[Bst])
                P.op("scalar", lambda e: e.activation(out=stt[:, 4 + 2 * hb:6 + 2 * hb], in_=stt[:, 4 + 2 * hb:6 + 2 * hb], func=AF.Exp, scale=-0.5), reads=[Bst], writes=[Bst])
                for tt in range(2):
                    t = 2 * hb + tt
                    P.op("gpsimd" if tt == 0 else "vector",
                         lambda e, tt=tt, t=t: e.tensor_scalar(out=xn[:, tt, :], in0=xb[:, tt, :], scalar1=stt[:, 4 + t:5 + t], scalar2=None, op0=ALU.mult),
                         reads=[Bx, Bst], writes=[B_xn])
                for kk in range(4):
                    bi = kk % 2
                    pv = bk16(bi).rearrange("p (a c) -> p a c", a=2)
                    fns = []
                    for a_ in range(2):
                        k = 2 * kk + a_
                        for tt in range(2):
                            fns.append(lambda e, a_=a_, k=k, tt=tt, pv=pv: e.transpose(out=pv[:, a_, tt * 128:(tt + 1) * 128], in_=xn[:, tt, k * 128:(k + 1) * 128], identity=ident_b))
                    P.group("tensor", fns, reads=[B_xn, B_ident_b], writes=[bkB[bi]])
                    if kk % 2 == 0:
                        P.op("scalar", lambda e, kk=kk, pv=pv: e.copy(out=xnT[:, 2 * kk:2 * kk + 2, hb * 256:(hb + 1) * 256], in_=pv[:, :, 0:256]), reads=[bkB[bi]], writes=[B_xnT])
                    else:
                        P.op("vector", lambda e, kk=kk, pv=pv: e.tensor_copy(out=xnT[:, 2 * kk:2 * kk + 2, hb * 256:(hb + 1) * 256], in_=pv[:, :, 0:256]), reads=[bkB[bi]], writes=[B_xnT])

            def fm_group(g, bank):
                fns = []
                for k in range(8):
                    fns.append(lambda e, k=k: e.matmul(out=bk32(bank), lhsT=wa_b[:, k, g * 128:(g + 1) * 128], rhs=xT[:, k, :], start=(k == 0), stop=(k == 7)))
                P.group("tensor", fns, reads=[B_wa, BxT], writes=[bkB[bank]])

            def f2():
                for (src, c0) in ((cos3, 0), (cos3, 8), (cos3, 64), (cos3, 72)):
                    P.op("gpsimd", lambda e, src=src, c0=c0: e.tensor_copy(out=tabC[tb][:, :, c0:c0 + 8], in_=src[:, 4 * b:4 * b + 4, :]),
                         reads=[B_cos], writes=[B_tab[tb]])
                for (src, c0) in ((nsin3, 0), (sin3, 8), (nsin3, 64), (sin3, 72)):
                    P.op("gpsimd", lambda e, src=src, c0=c0: e.tensor_copy(out=tabS[tb][:, :, c0:c0 + 8], in_=src[:, 4 * b:4 * b + 4, :]),
                         reads=[B_sin], writes=[B_tab[tb]])
                pv = bk16(2).rearrange("p (a c) -> p a c", a=2)
                fns = []
                for a_, tab in ((0, tabC[tb]), (1, tabS[tb])):
                    for t in range(4):
                        fns.append(lambda e, a_=a_, tab=tab, t=t, pv=pv: e.transpose(out=pv[:, a_, t * 128:(t + 1) * 128], in_=tab[:, t, :], identity=ident_b))
                P.group("tensor", fns, reads=[B_tab[tb], B_ident_b], writes=[bkB[2]])
                P.op("vector", lambda e, pv=pv: e.tensor_copy(out=CS[tb], in_=pv), reads=[bkB[2]], writes=[B_CS[tb]])
                fm_group(0, 3)
                P.op("scalar", lambda e: e.copy(out=qT_f, in_=bk32(3)), reads=[bkB[3]], writes=[B_qT])
                fm_group(1, 2)
                P.op("scalar", lambda e: e.activation(out=e1, in_=bk32(2), func=AF.Exp), reads=[bkB[2]], writes=[B_e1])
                P.op("scalar", lambda e: e.activation(out=sgn, in_=e1, func=AF.Ln, bias=1.0), reads=[B_e1], writes=[B_sgn])
                P.op("scalar", lambda e: e.activation(out=sgn, in_=sgn, func=AF.Exp, scale=-1.0), reads=[B_sgn], writes=[B_sgn])
                P.op("scalar", lambda e: e.activation(out=lf, in_=sgn, func=AF.Ln, scale=noml, bias=1.0), reads=[B_sgn, B_lbt], writes=[B_lf])

            def f3():
                P.op("vector", lambda e: e.tensor_tensor_scan(out=bcs, data0=scm, data1=lf, initial=0.0, op0=ALU.mult, op1=ALU.add),
                     reads=[B_scm, B_lf], writes=[B_bcs])
                P.op("scalar", lambda e: e.activation(out=eb, in_=bcs, func=AF.Exp), reads=[B_bcs], writes=[B_eb])
                P.op("scalar", lambda e: e.activation(out=enb, in_=bcs, func=AF.Exp, scale=-1.0), reads=[B_bcs], writes=[B_enb])
                P.op("vector", lambda e: e.tensor_tensor(out=qtb, in0=qT_f, in1=eb, op=ALU.mult), reads=[B_qT, B_eb], writes=[Bqt])
                P.op("vector", lambda e: e.scalar_tensor_tensor(out=ktb, in0=sgn, scalar=oml, in1=enb, op0=ALU.mult, op1=ALU.mult),
                     reads=[B_sgn, B_enb, B_lbt], writes=[Bkt])
                eb3 = eb.rearrange("p (n c) -> p n c", c=64)
                P.op("vector", lambda e: e.tensor_copy(out=ebl, in_=eb3[:, :, 63]), reads=[B_eb], writes=[Bebl])

            def rot_group(g, dst_ap, B_dst, is_k):
                fm_group(g, 2)
                P.op("vector", lambda e: e.tensor_tensor(out=t1, in0=bk32(2), in1=CS[tb][:, 0, :], op=ALU.mult), reads=[bkB[2], B_CS[tb]], writes=[B_t1])
                fm_group(g + 1, 3)
                P.op("vector", lambda e: e.tensor_tensor(out=t2, in0=bk32(3), in1=CS[tb][:, 1, :], op=ALU.mult), reads=[bkB[3], B_CS[tb]], writes=[B_t2])
                P.op("gpsimd", lambda e: e.tensor_tensor(out=dst_ap, in0=t1, in1=t2, op=ALU.add), reads=[B_t1, B_t2], writes=[B_dst])
                P.op("gpsimd", lambda e: e.tensor_tensor(out=sqk2, in0=dst_ap, in1=dst_ap, op=ALU.mult), reads=[B_dst], writes=[B_sqk2])
                P.op("tensor", lambda e: e.matmul(out=bk32(2), lhsT=blk1_b, rhs=sqk2, start=True, stop=True), reads=[B_blk1, B_sqk2], writes=[bkB[2]])
                col = 0 if is_k else 1
                P.op("vector", lambda e: e.reduce_max(out=mx[:, 2:3], in_=bk32(2), axis=AX.X), reads=[bkB[2]], writes=[B_mx])
                P.op("vector", lambda e: e.tensor_max(out=mx[:, col:col + 1], in0=mx[:, col:col + 1], in1=mx[:, 2:3]), reads=[B_mx], writes=[B_mx])

            def f4():
                rot_group(2, K_sb[:, b * 512:(b + 1) * 512], B_K[b], True)

            def f5():
                if b < NQB:
                    rot_group(4, Q_sb[:, b * 512:(b + 1) * 512], B_Q[b], False)

            def f6():
                for rnd in range(2):
                    pvv = bk32(4).rearrange("p (a c) -> p a c", a=2)
                    fns = []
                    for a_ in range(2):
                        t = 2 * rnd + a_
                        for k in range(8):
                            fns.append(lambda e, a_=a_, t=t, k=k, pvv=pvv: e.matmul(out=pvv[:, a_, :], lhsT=xT[:, k, t * 128:(t + 1) * 128], rhs=wa_b[:, k, 768:1024], start=(k == 0), stop=(k == 7)))
                    P.group("tensor", fns, reads=[B_wa, BxT], writes=[bkB[4]])
                    P.op("scalar", lambda e, rnd=rnd, pvv=pvv: e.copy(out=vhg[:, 2 * rnd:2 * rnd + 2, :], in_=pvv[:, :, 0:128]), reads=[bkB[4]], writes=[Bvhg])
                    for a_ in range(2):
                        P.op("scalar", lambda e, rnd=rnd, pvv=pvv, a_=a_: e.copy(out=V_sb[:, 4 * b + 2 * rnd + a_, 0:128], in_=pvv[:, a_, 128:256]),
                             reads=[bkB[4]], writes=[B_V[b]])
                pk = bk16(4).rearrange("p (a c) -> p a c", a=8)
                fns = [lambda e, t=t, pk=pk: e.transpose(out=pk[:, t, :], in_=ktb[:, t * 128:(t + 1) * 128], identity=ident_b) for t in range(4)]
                P.group("tensor", fns, reads=[Bkt, B_ident_b], writes=[bkB[4]])
                P.op("scalar", lambda e, pk=pk: e.copy(out=kttok, in_=pk[:, 0:4, :]), reads=[bkB[4]], writes=[Bkttok])

            return [lambda: xpart(0), lambda: xpart(1), f2, f3, f4, f5, f6]

        def tail_stages(b):
            pb_ = b % 2
            qtb, ktb, kttok, vhg, ebl = qt2[pb_], kt2[pb_], kttok2[pb_], vhg2[pb_], ebl2[pb_]
            Bqt, Bkt, Bkttok, Bvhg, Bebl = B_qt2[pb_], B_kt2[pb_], B_kttok2[pb_], B_vhg2[pb_], B_ebl2[pb_]
            pA = [bk32(5).rearrange("p (a c) -> p a c", a=4), bk32(6).rearrange("p (a c) -> p a c", a=4)]
            Sc = Sseq[b % 2]
            Sp = Sseq[(b + 1) % 2]
            BSc = B_Sseq[b % 2]
            BSp = B_Sseq[(b + 1) % 2]
            res = {}

            def t0():
                for r in range(2):
                    fns = [lambda e, t=t, r=r: e.matmul(out=pA[r][:, t, :], lhsT=kttok[64 * r:64 * r + 64, t, :], rhs=vhg[64 * r:64 * r + 64, t, :], start=True, stop=True)
                           for t in range(4)]
                    P.group("tensor", fns, reads=[Bkttok, Bvhg], writes=[bkB[5 + r]])

            def rec(n0, n1):
                for n in range(n0, n1):
                    t, r = n // 2, n % 2
                    prev_ap = Sp[:, 8, :] if n == 0 else Sc[:, n, :]
                    rd = [bkB[5 + r], BSp if n == 0 else BSc]
                    P.op("vector", lambda e, t=t, r=r, prev_ap=prev_ap: e.tensor_tensor(out=Ttmp, in0=pA[r][:, t, :], in1=prev_ap, op=ALU.add),
                         reads=rd, writes=[B_T])
                    P.op("vector", lambda e, n=n: e.tensor_scalar(out=Sc[:, n + 1, :], in0=Ttmp, scalar1=ebl[:, n:n + 1], scalar2=None, op0=ALU.mult),
                         reads=[B_T, Bebl], writes=[BSc])

            def t2():
                rec(4, 8)
                P.op("gpsimd", lambda e: e.tensor_copy(out=S_bf[:, 0, :], in_=Sp[:, 8, :]), reads=[BSp], writes=[B_Sbf])
                P.op("gpsimd", lambda e: e.tensor_copy(out=S_bf[:, 1:8, :], in_=Sc[:, 1:8, :]), reads=[BSc], writes=[B_Sbf])

            def t3():
                psc = bk32(7).rearrange("p (a c) -> p a c", a=4)
                fns = [lambda e, t=t: e.matmul(out=psc[:, t, :], lhsT=ktb[:, t * 128:(t + 1) * 128], rhs=qtb[:, t * 128:(t + 1) * 128], start=True, stop=True) for t in range(4)]
                P.group("tensor", fns, reads=[Bkt, Bqt], writes=[bkB[7]])
                P.op("vector", lambda e: e.tensor_tensor(out=PT_b, in0=bk32(7), in1=trim, op=ALU.mult), reads=[bkB[7], B_trim], writes=[B_PT])

            def t4():
                po = bk32(7).rearrange("p (a c) -> p a c", a=4)
                fns = []
                for t in range(4):
                    fns.append(lambda e, t=t: e.matmul(out=po[:, t, :], lhsT=PT_b[:, t * 128:(t + 1) * 128], rhs=vhg[:, t, :], start=True, stop=False))
                    for r in range(2):
                        n = 2 * t + r
                        fns.append(lambda e, t=t, r=r, n=n: e.matmul(out=po[64 * r:64 * r + 64, t, :], lhsT=qtb[:, n * 64:(n + 1) * 64], rhs=S_bf[:, n, :],
                                                                     start=False, stop=(r == 1), skip_group_check=True))
                P.group("tensor", fns, reads=[B_PT, Bvhg, Bqt, B_Sbf], writes=[bkB[7]])
                P.op("scalar", lambda e: e.copy(out=o_bf, in_=po), reads=[bkB[7]], writes=[B_obf])

            def t5():
                poT = bk32(7).rearrange("p (a c) -> p a c", a=4)
                fns = [lambda e, t=t: e.matmul(out=poT[:, t, :], lhsT=o_bf[:, t, :], rhs=ident_b, start=True, stop=True) for t in range(4)]
                P.group("tensor", fns, reads=[B_obf, B_ident_b], writes=[bkB[7]])
                oT = oT_sb[b % 2]
                BoT = B_oT[b % 2]
                P.op("vector", lambda e: e.tensor_copy(out=oT, in_=poT), reads=[bkB[7]], writes=[BoT])
                res["tok"] = P.dma("sync", dsem(f"o{b % 2}"), hg_part[:, b * 512:(b + 1) * 512], oT, reads=[BoT])

            return [t0, lambda: rec(0, 4), t2, t3, t4, t5], res

        load_x(0)
        hg_res = []
        for b in range(NB + 1):
            fs = front_stages(b) if b < NB else []
            if b >= 1:
                ts, res = tail_stages(b - 1)
                hg_res.append(res)
            else:
                ts = []
            import os as _os
            if _os.environ.get("KNOIL"):
                for f_ in ts:
                    f_()
                for f_ in fs:
                    f_()
            else:
                for i in range(max(len(fs), len(ts))):
                    if i < len(fs):
                        fs[i]()
                    if i < len(ts):
                        ts[i]()
        hg_toks = [r["tok"] for r in hg_res]

        B_hgall, B_daall = Buf(), Buf()
        P.custom("gpsimd", lambda e: e.collective_compute("AllGather", ALU.bypass, replica_groups=[list(range(NCORES))],
                                                          ins=[hg_part.ap()], outs=[hg_all.ap()]),
                 dsem("cc_hg"), None, writes=[B_hgall], extra=hg_toks[-2:])
        P.barrier([d for n_, d in sync_sems.items() if not n_.startswith("cc")])

        regA2 = Region(P_RES, regA.pos, False)
        sc1 = carve(regA2, [1, 8], F32)
        dl = carve(regA2, [1, 256], F32)
        dlp = carve(regA2, [1, 128], F32)
        ones_row = carve(regA2, [1, 128], F32)
        bc = carve(regA2, [128, 2], F32)
        sg08 = carve(regA2, [128, 128], F32)
        junk2 = carve(regA2, [128, 128], BF16)
        PTs = [carve(regA2, [128, 2, 512], BF16) for _ in range(3)]
        rs = carve(regA2, [128, 16], F32)
        at1 = carve(regA2, [128, 128], F32)
        ao = carve(regA2, [128, 4, 128], F32)
        aon = carve(regA2, [128, 4, 128], BF16)
        daT = [carve(regA2, [128, 4, 128], BF16) for _ in range(2)]
        B_sc1, B_dl, B_dlp, B_ones, B_bc, B_sg, B_junk2, B_rs, B_at1, B_ao, B_aon = [Buf() for _ in range(11)]
        B_PTs = [Buf() for _ in range(3)]
        B_daT = [Buf(), Buf()]
        P.dma("sync", dsem("c9"), dl, dld, writes=[B_dl])
        P.dma("sync", dsem("c10"), sg08, sgaind.partition_broadcast(128), writes=[B_sg])
        P.op("vector", lambda e: e.tensor_scalar_mul(out=sg08, in0=sg08, scalar1=1.0 - LAMBDA_INIT), reads=[B_sg], writes=[B_sg])
        P.op("gpsimd", lambda e: e.memset(ones_row, 1.0), writes=[B_ones])
        P.op("vector", lambda e: e.tensor_tensor(out=mx[:, 2:3], in0=mx[:, 0:1], in1=mx[:, 1:2], op=ALU.mult), reads=[B_mx], writes=[B_mx])
        P.op("scalar", lambda e: e.activation(out=mx[:, 2:3], in_=mx[:, 2:3], func=AF.Ln), reads=[B_mx], writes=[B_mx])
        P.op("scalar", lambda e: e.activation(out=mx[:, 2:3], in_=mx[:, 2:3], func=AF.Exp, scale=0.5), reads=[B_mx], writes=[B_mx])
        P.op("tensor", lambda e: e.transpose(out=bk32(7)[0:1, 0:128], in_=mx[:, 2:3], identity=ident_f), reads=[B_mx, B_ident_f], writes=[bkB[7]])
        P.op("vector", lambda e: e.reduce_max(out=sc1[:, 0:1], in_=bk32(7)[0:1, 0:128], axis=AX.X), reads=[bkB[7]], writes=[B_sc1])
        P.op("vector", lambda e: e.tensor_scalar_mul(out=sc1[:, 0:1], in0=sc1[:, 0:1], scalar1=-0.125), reads=[B_sc1], writes=[B_sc1])
        dl4 = dl.rearrange("p (a b c) -> p a b c", a=2, b=2)
        dlp2 = dlp.rearrange("p (a c) -> p a c", a=2)
        P.op("vector", lambda e: e.tensor_tensor(out=dlp2, in0=dl4[:, :, 0, :], in1=dl4[:, :, 1, :], op=ALU.mult), reads=[B_dl], writes=[B_dlp])
        P.op("vector", lambda e: e.reduce_sum(out=sc1[:, 2:4], in_=dlp2, axis=AX.X), reads=[B_dlp], writes=[B_sc1])
        P.op("scalar", lambda e: e.activation(out=sc1[:, 2:4], in_=sc1[:, 2:4], func=AF.Exp), reads=[B_sc1], writes=[B_sc1])
        P.op("vector", lambda e: e.tensor_sub(out=sc1[:, 1:2], in0=sc1[:, 2:3], in1=sc1[:, 3:4]), reads=[B_sc1], writes=[B_sc1])
        P.op("vector", lambda e: e.tensor_scalar_add(out=sc1[:, 1:2], in0=sc1[:, 1:2], scalar1=LAMBDA_INIT), reads=[B_sc1], writes=[B_sc1])
        P.op("tensor", lambda e: e.matmul(out=bk32(7)[:, 0:2], lhsT=ones_row, rhs=sc1[:, 0:2], start=True, stop=True), reads=[B_ones, B_sc1], writes=[bkB[7]])
        P.op("vector", lambda e: e.tensor_copy(out=bc, in_=bk32(7)[:, 0:2]), reads=[bkB[7]], writes=[B_bc])

        def acc(m, s_):
            idx = m * 4 + s_
            return bk32(4 + idx // 3)[:, (idx % 3) * VW:(idx % 3) * VW + VW], bkB[4 + idx // 3]

        def qk(qb, kt):
            pb = kt % 2
            for m in range(2):
                P.op("tensor", lambda e, m=m, pb=pb: e.matmul(out=bk32(2 * pb + m), lhsT=K_sb[64 * m:64 * m + 64, kt * 128:(kt + 1) * 128],
                                                              rhs=Q_sb[64 * m:64 * m + 64, qb * 512:(qb + 1) * 512], start=True, stop=True),
                     reads=[B_K[kt // 4], B_Q[qb]], writes=[bkB[2 * pb + m]])

        def expo(kt):
            pb = kt % 2
            x3 = kt % 3
            P.op("scalar", lambda e, pb=pb, x3=x3: e.activation(out=PTs[x3], in_=pair_t[pb][:, :, :], func=AF.Exp, bias=bc[:, 0:1], scale=0.125),
                 reads=[bkB[2 * pb], bkB[2 * pb + 1], B_bc], writes=[B_PTs[x3]])

        def pv_mm(kt):
            x3 = kt % 3
            fns = []
            for m in range(2):
                for s_ in range(4):
                    ap_, _ = acc(m, s_)
                    fns.append(lambda e, m=m, s_=s_, ap_=ap_: e.matmul(out=ap_, lhsT=PTs[x3][:, m, s_ * 128:(s_ + 1) * 128], rhs=V_sb[:, kt, :],
                                                                      start=False, stop=(kt == NT - 1), skip_group_check=True))
            P.group("tensor", fns, reads=[B_PTs[x3], B_V[kt // 4], B_Vones], writes=[bkB[4], bkB[5], bkB[6]])

        def a2_block(qb):
            for i in range(3):
                P.op("vector", lambda e, i=i: e.memset(bk32(4 + i), 0.0), writes=[bkB[4 + i]])
            for kt in range(NT + 1):
                if kt < NT:
                    qk(qb, kt)
                    expo(kt)
                if kt > 0:
                    pv_mm(kt - 1)
            for m in range(2):
                for s_ in range(4):
                    ap_, bb = acc(m, s_)
                    P.op("vector", lambda e, m=m, s_=s_, ap_=ap_: e.reciprocal(out=rs[:, m * 4 + s_:m * 4 + s_ + 1], in_=ap_[:, 128:129]), reads=[bb], writes=[B_rs])
            P.op("vector", lambda e: e.tensor_scalar(out=rs[:, 4:8], in0=rs[:, 4:8], scalar1=bc[:, 1:2], scalar2=None, op0=ALU.mult), reads=[B_rs, B_bc], writes=[B_rs])
            for s_ in range(4):
                a0, b0 = acc(0, s_)
                a1, b1 = acc(1, s_)
                P.op("vector", lambda e, s_=s_, a1=a1: e.tensor_scalar(out=at1, in0=a1[:, 0:128], scalar1=rs[:, 4 + s_:5 + s_], scalar2=None, op0=ALU.mult),
                     reads=[b1, B_rs], writes=[B_at1])
                P.op("vector", lambda e, s_=s_, a0=a0: e.scalar_tensor_tensor(out=ao[:, s_, :], in0=a0[:, 0:128], scalar=rs[:, s_:s_ + 1], in1=at1, op0=ALU.mult, op1=ALU.subtract),
                     reads=[b0, B_rs, B_at1], writes=[B_ao])
                P.op("scalar", lambda e, s_=s_: e.activation(out=junk2, in_=ao[:, s_, :], func=AF.Square, accum_out=rs[:, 8 + s_:9 + s_]),
                     reads=[B_ao], writes=[B_junk2, B_rs])
            P.op("vector", lambda e: e.tensor_scalar(out=rs[:, 12:16], in0=rs[:, 8:12], scalar1=1.0 / 128.0, scalar2=1e-5, op0=ALU.mult, op1=ALU.add), reads=[B_rs], writes=[B_rs])
            P.op("scalar", lambda e: e.activation(out=rs[:, 12:16], in_=rs[:, 12:16], func=AF.Ln), reads=[B_rs], writes=[B_rs])
            P.op("scalar", lambda e: e.activation(out=rs[:, 12:16], in_=rs[:, 12:16], func=AF.Exp, scale=-0.5), reads=[B_rs], writes=[B_rs])
            for s_ in range(4):
                P.op("vector", lambda e, s_=s_: e.scalar_tensor_tensor(out=aon[:, s_, :], in0=ao[:, s_, :], scalar=rs[:, 12 + s_:13 + s_], in1=sg08, op0=ALU.mult, op1=ALU.mult),
                     reads=[B_ao, B_rs, B_sg], writes=[B_aon])
            pdt = bk32(7).rearrange("p (a c) -> p a c", a=4)
            fns = [lambda e, s_=s_: e.matmul(out=pdt[:, s_, :], lhsT=aon[:, s_, :], rhs=ident_b, start=True, stop=True) for s_ in range(4)]
            P.group("tensor", fns, reads=[B_aon, B_ident_b], writes=[bkB[7]])
            dT = daT[qb % 2]
            P.op("vector", lambda e: e.tensor_copy(out=dT, in_=pdt), reads=[bkB[7]], writes=[B_daT[qb % 2]])
            return P.dma("sync", dsem(f"da{qb % 2}"), da_part[:, qb * 512:(qb + 1) * 512], dT, reads=[B_daT[qb % 2]])

        da_toks = [a2_block(qb) for qb in range(NQB)]

        P.custom("gpsimd", lambda e: e.collective_compute("AllGather", ALU.bypass, replica_groups=[list(range(NCORES))],
                                                          ins=[da_part.ap()], outs=[da_all.ap()]),
                 dsem("cc_da"), None, writes=[B_daall], extra=da_toks[-2:])

        if "hg" in debug:
            final_toks.append(P.dma("sync", dsem("dbg3"), dout("dbg_hg", [128, S], BF16), hg_part[:, :], extra=hg_toks[-2:]))
        if "da" in debug:
            final_toks.append(P.dma("sync", dsem("dbg4"), dout("dbg_da", [128, SH], BF16), da_part[:, :], extra=da_toks[-2:]))

        P.barrier([d for n_, d in sync_sems.items() if not n_.startswith("cc")])
        regC = Region(P_RES, ARENA_BYTES, True)
        regC1 = Region(P_RES, ARENA_BYTES, False)
        x1 = carve(regC, [128, NTC, 1024], F32)
        h2T = carve(regC, [128, 8, SC], BF16)
        gates = carve(regC, [128, NTC, 32], F32)
        lg = carve(regC, [128, NTC, 36], F32)
        g2b = carve(regC, [128, 1024], F32)
        gfb = carve(regC, [128, 1024], F32)
        brb = carve(regC, [128, 36], F32)
        wr_f = carve(regC, [128, 8, 36], F32)
        hgn = carve(regC, [128, 1], F32)
        B_x1 = [Buf() for _ in range(NTC)]
        B_h2T = [Buf() for _ in range(NTC)]
        B_gates = [Buf() for _ in range(NTC)]
        B_lg = [Buf() for _ in range(NTC)]
        B_g2b, B_gfb, B_brb, B_wr, B_hgn = [Buf() for _ in range(5)]
        P.dma("sync", dsem("k0"), g2b, g2d.partition_broadcast(128), writes=[B_g2b])
        P.dma("sync", dsem("k1"), gfb, gfd.partition_broadcast(128), writes=[B_gfb])
        P.dma("sync", dsem("k2"), brb, brd.partition_broadcast(128), writes=[B_brb])
        P.dma("sync", dsem("k3"), wr_f, wrd.rearrange("(k p) c -> p k c", p=128), writes=[B_wr])
        P.dma("sync", dsem("k4"), hgn, hgnd, writes=[B_hgn])

        wo_b = carve(regC1, [128, 8, 1024], BF16)
        wg_b = carve(regC1, [128, 8, 512], BF16)
        xnC = carve(regC1, [128, 4, 1024], BF16)
        xnTC = carve(regC1, [128, 8, 512], BF16)
        mixT = carve(regC1, [128, 8, 512], BF16)
        sgate = carve(regC1, [128, 512], F32)
        hgD = carve(regC1, [128, 4, 512], BF16)
        hgR = carve(regC1, [128, 4, 512], BF16)
        osum = carve(regC1, [128, 512], F32)
        sq = carve(regC1, [128, 512], BF16)
        rst = carve(regC1, [128, 512], F32)
        h2g = carve(regC1, [128, 1024], F32)
        h2Tf = carve(regC1, [128, 8, 128], F32)
        junkC = carve(regC1, [128, 1024], BF16)
        statC = [carve(regC1, [128, 8], F32) for _ in range(2)]
        stat2 = [carve(regC1, [128, 8], F32) for _ in range(2)]
        rt = carve(regC1, [128, 16], F32)
        rml = carve(regC1, [128, 32], F32)
        rml2 = carve(regC1, [128, 32], F32)
        roh1 = carve(regC1, [128, 32], F32)
        roh2 = carve(regC1, [128, 32], F32)
        rge = carve(regC1, [128, 4], F32)
        rmk = carve(regC1, [128, 4], F32)
        assert regC1.pos <= regC.pos, f"phase C1 does not fit: {regC1.pos} > {regC.pos}"
        (B_wo, B_wgb, B_xnC, B_xnTC, B_sgate, B_osum, B_sq, B_rst, B_h2g, B_h2Tf, B_junkC, B_rt, B_rml, B_rml2, B_roh1, B_roh2,
         B_rge, B_rmk) = [Buf() for _ in range(18)]
        B_mixT = [Buf() for _ in range(8)]
        B_statC, B_stat2 = [[Buf(), Buf()] for _ in range(2)]
        B_hgD, B_hgR = Buf(), Buf()
        P.dma("gpsimd", dsem("k5"), wo_b, wod.rearrange("(k p) c -> p k c", p=128), writes=[B_wo])
        P.dma("gpsimd", dsem("k6"), wg_b, wgd.rearrange("(k p) c -> p k c", p=128), writes=[B_wgb])
        for k in range(8):
            P.op("vector", lambda e, k=k: e.tensor_scalar(out=wg_b[:, k, :], in0=wg_b[:, k, :], scalar1=g1[:, k:k + 1], scalar2=None, op0=ALU.mult),
                 reads=[B_g1, B_wgb], writes=[B_wgb])

        dynreg = {}

        def dyn(e, tb):
            key = id(e)

            def mat(expr, lo, hi):
                return e.snap(e.to_reg(expr), donate=True, min_val=lo, max_val=hi)
            if key not in dynreg:
                pid = mat(e.partition_id(), 0, NCORES - 1)
                j = mat(pid // 4, 0, 1)
                jn = mat((7 - pid) // 4, 0, 1)
                colD = mat(jn * (pid * SC) + j * ((NCORES - 1 - pid) * SC), 0, SH - SC)
                colR = mat(jn * ((NCORES - 1 - pid) * SC) + j * (pid * SC), 0, S - SC)
                rowD = mat(j * 512, 0, 512)
                rowR = mat(jn * 512, 0, 512)
                dynreg[key] = dict(colD=colD, colR=colR, rowD=rowD, rowR=rowR, blk={})
            dct = dynreg[key]
            if tb not in dct["blk"]:
                cD = mat(dct["colD"] + tb * 512, 0, SH - 512)
                cR = mat(dct["colR"] + (SC - (tb + 1) * 512), 0, S - 512)
                dct["blk"][tb] = (cD, cR)
            cD, cR = dct["blk"][tb]
            return cD, cR, dct["rowD"], dct["rowR"]

        xov = xod.rearrange("(b t p) d -> b p t d", p=128, t=4)
        c4 = [0]

        def route_tile(tl):
            g4 = lg[:, tl, 0:4]
            le3 = lg[:, tl, 4:36].rearrange("p (g e) -> p g e", g=4)
            rml3 = rml.rearrange("p (g e) -> p g e", g=4)
            Bl = B_lg[tl]
            P.op("vector", lambda e: e.reduce_max(out=rt[:, 0:1], in_=g4, axis=AX.X), reads=[Bl], writes=[B_rt])
            P.op("vector", lambda e: e.tensor_scalar_mul(out=rt[:, 1:2], in0=rt[:, 0:1], scalar1=-1.0), reads=[B_rt], writes=[B_rt])
            P.op("scalar", lambda e: e.activation(out=rge, in_=g4, func=AF.Exp, bias=rt[:, 1:2], accum_out=rt[:, 2:3]), reads=[Bl, B_rt], writes=[B_rge, B_rt])
            P.op("vector", lambda e: e.reciprocal(out=rt[:, 3:4], in_=rt[:, 2:3]), reads=[B_rt], writes=[B_rt])
            P.op("vector", lambda e: e.tensor_scalar(out=rmk, in0=g4, scalar1=rt[:, 0:1], scalar2=None, op0=ALU.is_equal), reads=[Bl, B_rt], writes=[B_rmk])
            P.op("vector", lambda e: e.tensor_scalar(out=rmk, in0=rmk, scalar1=-1.0, scalar2=1e30, op0=ALU.add, op1=ALU.mult), reads=[B_rmk], writes=[B_rmk])
            for g in range(4):
                P.op("vector", lambda e, g=g: e.tensor_scalar(out=rml3[:, g, :], in0=le3[:, g, :], scalar1=rmk[:, g:g + 1], scalar2=None, op0=ALU.add),
                     reads=[Bl, B_rmk], writes=[B_rml])
            P.op("vector", lambda e: e.reduce_max(out=rt[:, 4:5], in_=rml, axis=AX.X), reads=[B_rml], writes=[B_rt])
            P.op("vector", lambda e: e.tensor_scalar(out=roh1, in0=rml, scalar1=rt[:, 4:5], scalar2=None, op0=ALU.is_equal), reads=[B_rml, B_rt], writes=[B_roh1])
            P.op("vector", lambda e: e.scalar_tensor_tensor(out=rml2, in0=roh1, scalar=-1e30, in1=rml, op0=ALU.mult, op1=ALU.add), reads=[B_roh1, B_rml], writes=[B_rml2])
            P.op("vector", lambda e: e.reduce_max(out=rt[:, 5:6], in_=rml2, axis=AX.X), reads=[B_rml2], writes=[B_rt])
            P.op("vector", lambda e: e.tensor_scalar(out=roh2, in0=rml2, scalar1=rt[:, 5:6], scalar2=None, op0=ALU.is_equal), reads=[B_rml2, B_rt], writes=[B_roh2])
            P.op("vector", lambda e: e.tensor_sub(out=rt[:, 6:7], in0=rt[:, 5:6], in1=rt[:, 4:5]), reads=[B_rt], writes=[B_rt])
            P.op("scalar", lambda e: e.activation(out=rt[:, 6:7], in_=rt[:, 6:7], func=AF.Exp), reads=[B_rt], writes=[B_rt])
            P.op("vector", lambda e: e.tensor_scalar_add(out=rt[:, 7:8], in0=rt[:, 6:7], scalar1=1.0), reads=[B_rt], writes=[B_rt])
            P.op("vector", lambda e: e.reciprocal(out=rt[:, 7:8], in_=rt[:, 7:8]), reads=[B_rt], writes=[B_rt])
            P.op("vector", lambda e: e.tensor_mul(out=rt[:, 8:9], in0=rt[:, 7:8], in1=rt[:, 6:7]), reads=[B_rt], writes=[B_rt])
            P.op("vector", lambda e: e.tensor_scalar(out=rt[:, 9:11], in0=rt[:, 7:9], scalar1=rt[:, 3:4], scalar2=None, op0=ALU.mult), reads=[B_rt], writes=[B_rt])
            P.op("vector", lambda e: e.tensor_scalar(out=roh1, in0=roh1, scalar1=rt[:, 9:10], scalar2=None, op0=ALU.mult), reads=[B_roh1, B_rt], writes=[B_roh1])
            P.op("vector", lambda e: e.scalar_tensor_tensor(out=gates[:, tl, :], in0=roh2, scalar=rt[:, 10:11], in1=roh1, op0=ALU.mult, op1=ALU.add),
                 reads=[B_roh2, B_roh1, B_rt], writes=[B_gates[tl]])

        def c1_block(tb):
            stt = statC[tb % 2]
            Bst = B_statC[tb % 2]
            tiles = [4 * tb + t for t in range(4)]
            P.dma("sync", dsem("xo"), x1[:, 4 * tb:4 * tb + 4, :], xov[tb], writes=[B_x1[tl] for tl in tiles])
            for t in range(4):
                P.op("scalar", lambda e, t=t: e.activation(out=junkC, in_=x1[:, 4 * tb + t, :], func=AF.Square, accum_out=stt[:, t:t + 1]),
                     reads=[B_x1[4 * tb + t]], writes=[B_junkC, Bst])
            stat_rstd(stt, Bst, 4, D, 1e-6)
            for t in range(4):
                P.op("gpsimd" if t % 2 == 0 else "vector",
                     lambda e, t=t: e.tensor_scalar(out=xnC[:, t, :], in0=x1[:, 4 * tb + t, :], scalar1=stt[:, 4 + t:5 + t], scalar2=None, op0=ALU.mult),
                     reads=[B_x1[4 * tb + t], Bst], writes=[B_xnC])
            for kk in range(4):
                bi = kk % 2
                pv = bk16(bi).rearrange("p (a c) -> p a c", a=2)
                fns = []
                for a in range(2):
                    k = 2 * kk + a
                    for t in range(4):
                        fns.append(lambda e, a=a, k=k, t=t, pv=pv: e.transpose(out=pv[:, a, t * 128:(t + 1) * 128], in_=xnC[:, t, k * 128:(k + 1) * 128], identity=ident_b))
                P.group("tensor", fns, reads=[B_xnC, B_ident_b], writes=[bkB[bi]])
                if kk % 2 == 0:
                    P.op("scalar", lambda e, kk=kk, pv=pv: e.copy(out=xnTC[:, 2 * kk:2 * kk + 2, :], in_=pv), reads=[bkB[bi]], writes=[B_xnTC])
                else:
                    P.op("vector", lambda e, kk=kk, pv=pv: e.tensor_copy(out=xnTC[:, 2 * kk:2 * kk + 2, :], in_=pv), reads=[bkB[bi]], writes=[B_xnTC])
            dq_eng = "sync" if tb < 2 else "gpsimd"
            P.dma(dq_eng, dsem("hd"), hgD,
                  lambda e: hg_all[bass.ds(dyn(e, tb)[2], 512), bass.ds(dyn(e, tb)[0], 512)].rearrange("(h p) c -> p h c", p=128),
                  reads=[B_hgall], writes=[B_hgD])
            P.dma(dq_eng, dsem("hr"), hgR,
                  lambda e: hg_all[bass.ds(dyn(e, tb)[3], 512), bass.ds(dyn(e, tb)[1], 512)].rearrange("(h p) c -> p h c", p=128),
                  reads=[B_hgall], writes=[B_hgR])
            P.dma(dq_eng, dsem("dd"), mixT[:, 4:8, :],
                  lambda e: da_all[bass.ds(dyn(e, tb)[2], 512), bass.ds(dyn(e, tb)[0], 512)].rearrange("(h p) c -> p h c", p=128),
                  reads=[B_daall], writes=B_mixT[4:8])
            for hh in range(4):
                fns = [lambda e, k=k, hh=hh: e.matmul(out=bk32(2), lhsT=wg_b[:, k, hh * 128:(hh + 1) * 128], rhs=xnTC[:, k, :], start=(k == 0), stop=(k == 7)) for k in range(8)]
                P.group("tensor", fns, reads=[B_wgb, B_xnTC], writes=[bkB[2]])
                P.op("scalar", lambda e: e.activation(out=sgate, in_=bk32(2), func=AF.Silu), reads=[bkB[2]], writes=[B_sgate])
                P.op("vector", lambda e, hh=hh: e.tensor_tensor(out=osum, in0=hgD[:, hh, :], in1=hgR[:, hh, ::-1], op=ALU.add), reads=[B_hgD, B_hgR], writes=[B_osum])
                P.op("gpsimd", lambda e: e.tensor_tensor(out=sq, in0=osum, in1=osum, op=ALU.mult), reads=[B_osum], writes=[B_sq])
                P.op("tensor", lambda e: e.matmul(out=bk32(3), lhsT=ones_b, rhs=sq, start=True, stop=True), reads=[B_onesb, B_sq], writes=[bkB[3]])
                P.op("vector", lambda e: e.tensor_scalar(out=rst, in0=bk32(3), scalar1=1.0 / 128.0, scalar2=1e-6, op0=ALU.mult, op1=ALU.add), reads=[bkB[3]], writes=[B_rst])
                P.op("scalar", lambda e: e.activation(out=rst, in_=rst, func=AF.Ln), reads=[B_rst], writes=[B_rst])
                P.op("scalar", lambda e: e.activation(out=rst, in_=rst, func=AF.Exp, scale=-0.5), reads=[B_rst], writes=[B_rst])
                P.op("vector", lambda e: e.scalar_tensor_tensor(out=osum, in0=osum, scalar=hgn[:, 0:1], in1=rst, op0=ALU.mult, op1=ALU.mult), reads=[B_osum, B_hgn, B_rst], writes=[B_osum])
                P.op("vector", lambda e, hh=hh: e.tensor_tensor(out=mixT[:, hh, :], in0=osum, in1=sgate, op=ALU.mult), reads=[B_osum, B_sgate], writes=[B_mixT[hh]])
            for t in range(4):
                for dh in range(2):
                    bank = 4 + (2 * t + dh) % 2
                    fns = [lambda e, f=f, t=t, dh=dh, bank=bank: e.matmul(out=bk32(bank), lhsT=mixT[:, f, t * 128:(t + 1) * 128], rhs=wo_b[:, f, dh * 512:(dh + 1) * 512],
                                                                          start=(f == 0), stop=(f == 7)) for f in range(8)]
                    P.group("tensor", fns, reads=B_mixT + [B_wo], writes=[bkB[bank]])
                    P.op("vector", lambda e, t=t, dh=dh, bank=bank: e.tensor_tensor(out=x1[:, 4 * tb + t, dh * 512:(dh + 1) * 512], in0=bk32(bank),
                                                                                     in1=x1[:, 4 * tb + t, dh * 512:(dh + 1) * 512], op=ALU.add),
                         reads=[bkB[bank], B_x1[4 * tb + t]], writes=[B_x1[4 * tb + t]])
            st2 = stat2[tb % 2]
            Bs2 = B_stat2[tb % 2]
            for t in range(4):
                P.op("scalar", lambda e, t=t: e.activation(out=junkC, in_=x1[:, 4 * tb + t, :], func=AF.Square, accum_out=st2[:, t:t + 1]),
                     reads=[B_x1[4 * tb + t]], writes=[B_junkC, Bs2])
            stat_rstd(st2, Bs2, 4, D, 1e-6)
            pT8 = pair_t[3][:, :, :].rearrange("p a (k c) -> p (a k) c", k=4)
            for t in range(4):
                tl = 4 * tb + t
                P.op("vector", lambda e, t=t, tl=tl: e.scalar_tensor_tensor(out=h2g, in0=x1[:, tl, :], scalar=st2[:, 4 + t:5 + t], in1=g2b, op0=ALU.mult, op1=ALU.mult),
                     reads=[B_x1[tl], Bs2, B_g2b], writes=[B_h2g])
                fns = [lambda e, k=k: e.transpose(out=pT8[:, k, :], in_=h2g[:, k * 128:(k + 1) * 128], identity=ident_f) for k in range(8)]
                P.group("tensor", fns, reads=[B_h2g, B_ident_f], writes=[bkB[6], bkB[7]])
                P.op("scalar", lambda e: e.copy(out=h2Tf, in_=pT8), reads=[bkB[6], bkB[7]], writes=[B_h2Tf])
                P.op("vector", lambda e, tl=tl: e.tensor_copy(out=h2T[:, :, tl * 128:(tl + 1) * 128], in_=pT8), reads=[bkB[6], bkB[7]], writes=[B_h2T[tl]])
                fns = [lambda e, k=k: e.matmul(out=bk32(2)[:, 0:36], lhsT=h2Tf[:, k, :], rhs=wr_f[:, k, :], start=(k == 0), stop=(k == 7)) for k in range(8)]
                P.group("tensor", fns, reads=[B_h2Tf, B_wr], writes=[bkB[2]])
                P.op("vector", lambda e, tl=tl: e.tensor_tensor(out=lg[:, tl, :], in0=bk32(2)[:, 0:36], in1=brb, op=ALU.add), reads=[bkB[2], B_brb], writes=[B_lg[tl]])
                route_tile(tl)

        for tb in range(NBC):
            c1_block(tb)

        if "x1" in debug:
            final_toks.append(P.dma("sync", dsem("dbg5"), dout("dbg_x1", [128, NTC, 1024], F32), x1, reads=B_x1))
            final_toks.append(P.dma("sync", dsem("dbg6"), dout("dbg_gates", [128, NTC, 32], F32), gates, reads=B_gates))
            final_toks.append(P.dma("sync", dsem("dbg7"), dout("dbg_h2T", [128, 8, SC], BF16), h2T, reads=B_h2T))

        P.barrier([d for n_, d in sync_sems.items() if not n_.startswith("cc")])
        regC3 = Region(P_RES, regC.pos, False)
        wge = [carve(regC3, [128, 8, 512], BF16) for _ in range(2)]
        wue = [carve(regC3, [128, 8, 512], BF16) for _ in range(2)]
        wde = [carve(regC3, [128, 4, 1024], BF16) for _ in range(2)]
        aT = [carve(regC3, [128, 4, 512], BF16) for _ in range(2)]
        sgs = [carve(regC3, [128, 512], F32) for _ in range(2)]
        fo = [carve(regC3, [128, 1024], F32) for _ in range(2)]
        junk3 = carve(regC3, [128, 1024], BF16)
        stf = carve(regC3, [128, 2 * NTC], F32)
        B_wge, B_wue, B_wde, B_aT, B_sgs, B_fo = [[Buf(), Buf()] for _ in range(6)]
        B_junk3, B_stf = Buf(), Buf()

        def load_expert(ex):
            sl = ex % 2
            P.dma("gpsimd", dsem(f"wg{sl}"), wge[sl], wgated[ex].rearrange("(k p) c -> p k c", p=128), writes=[B_wge[sl]])
            P.dma("gpsimd", dsem(f"wu{sl}"), wue[sl], wupd[ex].rearrange("(k p) c -> p k c", p=128), writes=[B_wue[sl]])
            P.dma("gpsimd", dsem(f"wd{sl}"), wde[sl], wdownd[ex].rearrange("(k p) c -> p k c", p=128), writes=[B_wde[sl]])

        cnt = [0]

        def expert(ex):
            sl = ex % 2
            if ex + 1 < N_EXPERTS:
                load_expert(ex + 1)
            for tb in range(NBC):
                asl = cnt[0] % 2
                cnt[0] += 1
                for jc in range(4):
                    pb = jc % 2
                    rd = [B_h2T[4 * tb + t] for t in range(4)]
                    fns = [lambda e, k=k, jc=jc, pb=pb, tb=tb: e.matmul(out=bk32(2 * pb), lhsT=wge[sl][:, k, jc * 128:(jc + 1) * 128], rhs=h2T[:, k, tb * 512:(tb + 1) * 512],
                                                                 start=(k == 0), stop=(k == 7)) for k in range(8)]
                    P.group("tensor", fns, reads=[B_wge[sl]] + rd, writes=[bkB[2 * pb]])
                    fns = [lambda e, k=k, jc=jc, pb=pb, tb=tb: e.matmul(out=bk32(2 * pb + 1), lhsT=wue[sl][:, k, jc * 128:(jc + 1) * 128], rhs=h2T[:, k, tb * 512:(tb + 1) * 512],
                                                                 start=(k == 0), stop=(k == 7)) for k in range(8)]
                    P.group("tensor", fns, reads=[B_wue[sl]] + rd, writes=[bkB[2 * pb + 1]])
                    P.op("scalar", lambda e, pb=pb: e.activation(out=sgs[pb], in_=bk32(2 * pb), func=AF.Silu), reads=[bkB[2 * pb]], writes=[B_sgs[pb]])
                    P.op("vector", lambda e, pb=pb, jc=jc, asl=asl: e.tensor_tensor(out=aT[asl][:, jc, :], in0=sgs[pb], in1=bk32(2 * pb + 1), op=ALU.mult),
                         reads=[B_sgs[pb], bkB[2 * pb + 1]], writes=[B_aT[asl]])
                for t in range(4):
                    tl = 4 * tb + t
                    for dh in range(2):
                        bank = 4 + (2 * t + dh) % 4
                        fns = [lambda e, jc=jc, t=t, dh=dh, bank=bank, asl=asl: e.matmul(out=bk32(bank), lhsT=aT[asl][:, jc, t * 128:(t + 1) * 128],
                                                                                       rhs=wde[sl][:, jc, dh * 512:(dh + 1) * 512], start=(jc == 0), stop=(jc == 3)) for jc in range(4)]
                        P.group("tensor", fns, reads=[B_aT[asl], B_wde[sl]], writes=[bkB[bank]])
                        P.op("vector", lambda e, tl=tl, dh=dh, bank=bank: e.scalar_tensor_tensor(out=x1[:, tl, dh * 512:(dh + 1) * 512], in0=bk32(bank), scalar=gates[:, tl, ex:ex + 1],
                                                                                                 in1=x1[:, tl, dh * 512:(dh + 1) * 512], op0=ALU.mult, op1=ALU.add),
                             reads=[bkB[bank], B_gates[tl], B_x1[tl]], writes=[B_x1[tl]])

        load_expert(0)
        for ex in range(N_EXPERTS):
            expert(ex)

        outv = outd.rearrange("(t p) d -> p t d", p=128)
        for tl in range(NTC):
            P.op("scalar", lambda e, tl=tl: e.activation(out=junk3, in_=x1[:, tl, :], func=AF.Square, accum_out=stf[:, tl:tl + 1]), reads=[B_x1[tl]], writes=[B_junk3, B_stf])
        stat_rstd(stf, B_stf, NTC, D, 1e-6)

        def fin_tile(tl):
            sl = tl % 2
            P.op("vector",
                 lambda e: e.scalar_tensor_tensor(out=fo[sl], in0=x1[:, tl, :], scalar=stf[:, NTC + tl:NTC + tl + 1], in1=gfb, op0=ALU.mult, op1=ALU.mult),
                 reads=[B_x1[tl], B_stf, B_gfb], writes=[B_fo[sl]])
            final_toks.append(P.dma("sync", dsem(f"out{sl}"), outv[:, tl, :], fo[sl], reads=[B_fo[sl]]))

        for tl in range(NTC):
            fin_tile(tl)

        P.wait_all("sync", final_toks)
        with nc.Block() as block:
            P.emit_all(block)
    return nc


def _consts():
    ident = np.eye(128, dtype=np.float32)
    blk1 = np.zeros((128, 128), np.float32)
    blk1[:64, :64] = 1.0
    blk1[64:, 64:] = 1.0
    ones = np.ones((128, 128), np.float32)
    s = np.arange(128)[:, None]
    t = np.arange(128)[None, :]
    tri = ((s <= t) & ((s // 64) == (t // 64))).astype(np.float32)
    trim = np.ascontiguousarray(np.tile(tri, (1, 4)))
    scm = np.ones((128, 512), np.float32)
    scm[:, ::64] = 0.0
    return ident, blk1, ones, trim, scm


def make_in_maps(inputs, S):
    f32 = lambda a: np.asarray(a, np.float32)
    x = f32(inputs["x"])[0]
    pos = np.asarray(inputs["positions"], np.int32)[0]
    w_in = f32(inputs["w_in"])[0]
    lbs = f32(inputs["hg_lower_bounds"])
    ident, blk1, ones, trim, scm = _consts()
    SC = S // NCORES
    g1 = np.ascontiguousarray(f32(inputs["norm1_gain"])[0].reshape(8, 128).T)
    perm = np.arange(128)
    for m in range(2):
        for d in range(8):
            perm[m * 64 + d] = m * 64 + d + 8
            perm[m * 64 + d + 8] = m * 64 + d
    shared = {
        "g1": g1, "ident": ident, "blk1": blk1, "ones": ones, "trim": trim, "scm": scm,
        "dl": np.ascontiguousarray(f32(inputs["diff_lambda"])[0].reshape(1, 256)),
        "sgain": np.ascontiguousarray(f32(inputs["diff_subln_gain"])[0].reshape(1, 128)),
        "wg": np.ascontiguousarray(w_in[:, 2048:2560]),
        "wo": np.ascontiguousarray(f32(inputs["w_out"])[0]),
        "hgn": np.ascontiguousarray(f32(inputs["hg_norm_gain"])[0].reshape(128, 1)),
        "g2": np.ascontiguousarray(f32(inputs["norm2_gain"])[0].reshape(1, D)),
        "gf": np.ascontiguousarray(f32(inputs["final_norm_gain"]).reshape(1, D)),
        "wr": np.ascontiguousarray(np.concatenate([f32(inputs["router_group_w"])[0], f32(inputs["router_expert_w"])[0]], axis=1)),
        "br": np.ascontiguousarray(np.concatenate([f32(inputs["router_group_b"])[0], f32(inputs["router_expert_b"])[0]]).reshape(1, 36)),
        "wgate": np.ascontiguousarray(f32(inputs["moe_w_gate"])[0]),
        "wup": np.ascontiguousarray(f32(inputs["moe_w_up"])[0]),
        "wdown": np.ascontiguousarray(f32(inputs["moe_w_down"])[0]),
    }
    in_maps = []
    for c in range(NCORES):
        h, j = c % 4, c // 4
        xs = x[::-1] if j else x
        ps = pos[::-1] if j else pos
        hq = w_in[:, h * 128:(h + 1) * 128]
        hf = w_in[:, 512 * (1 + j) + h * 128: 512 * (1 + j) + (h + 1) * 128]
        hi = w_in[:, 1536 + h * 128:1536 + (h + 1) * 128]
        dq = w_in[:, 2560 + h * 128:2560 + (h + 1) * 128]
        dk = w_in[:, 3072 + h * 128:3072 + (h + 1) * 128]
        dv = w_in[:, 3584 + h * 128:3584 + (h + 1) * 128]
        wa = np.concatenate([hq, hf, dk, dk[:, perm], dq, dq[:, perm], hi, dv], axis=1)
        xo = x[c * SC:(c + 1) * SC]
        if j:
            xo = xo[::-1]
        m = dict(shared)
        m.update({
            "xs": np.ascontiguousarray(xs),
            "pos": np.ascontiguousarray(ps.reshape(S // 128, 128)),
            "wa": np.ascontiguousarray(wa),
            "lbp": np.ascontiguousarray(lbs[j, :, h * 128:(h + 1) * 128].T),
            "xo": np.ascontiguousarray(xo),
        })
        in_maps.append(m)
    return in_maps


def assemble(results):
    outs = []
    for c in range(NCORES):
        o = np.asarray(results[c]["out"], np.float32)
        if c // 4:
            o = o[::-1]
        outs.append(o)
    return np.concatenate(outs, axis=0)[None]


def kernel(**inputs):
    S = int(np.asarray(inputs["x"]).shape[1])
    nc = build(S)
    in_maps = make_in_maps(inputs, S)
    res = run_bass_kernel_spmd(nc, in_maps, core_ids=list(range(NCORES)))
    return np.ascontiguousarray(assemble(res.results)).astype(np.float32)
```

```python
import math
from contextlib import ExitStack

import numpy as np
import concourse.bass as bass
import concourse.mybir as mybir
from concourse.bass_utils import run_bass_kernel_spmd

F32 = mybir.dt.float32
BF16 = mybir.dt.bfloat16
I32 = mybir.dt.int32
ALU = mybir.AluOpType
AF = mybir.ActivationFunctionType
AX = mybir.AxisListType

D = 1024
NCORES = 8
ROPE_THETA = 500000.0
N_EXPERTS = 32
D_EXPERT = 512
LAMBDA_INIT = 0.8 - 0.6 * math.exp(0.0)
TWO_PI = float(2.0 * np.pi)
PI = float(np.pi)
SEM_ROT = 20000


class Buf:
    __slots__ = ("name", "w", "rs", "excl")

    def __init__(self, name="", excl=False):
        self.name = name
        self.w = None
        self.rs = []
        self.excl = excl


class Prog:
    ENGS = ["tensor", "vector", "scalar", "gpsimd", "sync"]

    def __init__(self, nc, stack):
        self.nc = nc
        self.stack = stack
        self.ops = {e: [] for e in self.ENGS}
        self.cur = {}
        self.allsems = []
        self.nsem = 0
        for e in self.ENGS:
            self._new_sem(e)
        self.waited = {e: {} for e in self.ENGS}
        self.n_ops = 0

    def _new_sem(self, e):
        s = self.stack.enter_context(self.nc.semaphore(f"tl_{e}_{self.nsem}"))
        self.nsem += 1
        self.cur[e] = [s, 0]
        self.allsems.append((e, self.cur[e]))

    def _collect(self, eng, reads, writes, extra):
        deps = []
        for b in reads:
            if b.w is not None:
                deps.append(b.w)
            if b.excl:
                deps.extend(r for r in b.rs if r[0] != eng)
        for b in writes:
            if b.w is not None:
                deps.append(b.w)
            deps.extend(b.rs)
        deps.extend([d for d in extra if d is not None])
        waits = []
        wd = self.waited[eng]
        for (feng, sem, n) in deps:
            if feng == eng and eng == "tensor":
                continue
            key = id(sem)
            if wd.get(key, 0) >= n:
                continue
            wd[key] = n
            waits.append((sem, n))
        return waits

    @staticmethod
    def _update(tok, reads, writes):
        for b in reads:
            b.rs.append(tok)
        for b in writes:
            b.w = tok
            b.rs = []

    def op(self, eng, fn, reads=(), writes=(), extra=()):
        waits = self._collect(eng, reads, writes, extra)
        c = self.cur[eng]
        if c[1] >= SEM_ROT:
            self._new_sem(eng)
            c = self.cur[eng]
        c[1] += 1
        sem = c[0]
        tok = (eng, sem, c[1])
        self._update(tok, reads, writes)

        def emit(e, waits=waits, fn=fn, sem=sem):
            for s, v in waits:
                e.wait_ge(s, v)
            fn(e).then_inc(sem, 1)
        self.ops[eng].append(emit)
        self.n_ops += 1
        return tok

    def group(self, eng, fns, reads=(), writes=(), extra=()):
        n = len(fns)
        if n == 1:
            return self.op(eng, fns[0], reads, writes, extra)
        waits = self._collect(eng, reads, writes, extra)
        first = fns[0]

        def emit0(e, waits=waits, fn=first):
            for s, v in waits:
                e.wait_ge(s, v)
            fn(e)
        self.ops[eng].append(emit0)
        for fn in fns[1:-1]:
            self.ops[eng].append(lambda e, fn=fn: fn(e))
        self.n_ops += n - 1
        return self.op(eng, fns[-1], reads, writes, ())

    def new_dma_sem(self, name):
        s = self.stack.enter_context(self.nc.semaphore(name))
        return [s, 0]

    def dma(self, eng, ds, out, in_, reads=(), writes=(), extra=()):
        waits = self._collect(eng, reads, writes, extra)
        ds[1] += 16
        tok = ("dma", ds[0], ds[1])
        self._update(tok, reads, writes)

        def emit(e, waits=waits, out=out, in_=in_, s=ds[0]):
            for sm, v in waits:
                e.wait_ge(sm, v)
            o_ap = out(e) if callable(out) else out
            i_ap = in_(e) if callable(in_) else in_
            try:
                e.dma_start(out=o_ap, in_=i_ap).then_inc(s, 16)
            except Exception:
                print("DMA FAIL", eng, str(o_ap)[:300], "<<<<", str(i_ap)[:300])
                raise
        self.ops[eng].append(emit)
        self.n_ops += 1
        return tok

    def custom(self, eng, fn, ds, inc, reads=(), writes=(), extra=()):
        waits = self._collect(eng, reads, writes, extra)
        ds[1] += (1 if inc is None else inc)
        tok = ("cc", ds[0], ds[1])
        self._update(tok, reads, writes)

        def emit(e, waits=waits, fn=fn, s=ds[0], inc=inc):
            for sm, v in waits:
                e.wait_ge(sm, v)
            ins = fn(e)
            if inc is None:
                ins.then_inc(s)
            else:
                ins.then_inc(s, inc)
        self.ops[eng].append(emit)
        return tok

    def barrier(self, dma_sems):
        marks = [(f, c[0], c[1]) for (f, c) in self.allsems if c[1] > 0]
        marks += [("dma", d[0], d[1]) for d in dma_sems if d[1] > 0]
        for eng in self.ENGS:
            wd = self.waited[eng]
            waits = []
            for (f, sem, n) in marks:
                if f == eng:
                    continue
                if wd.get(id(sem), 0) >= n:
                    continue
                wd[id(sem)] = n
                waits.append((sem, n))

            def emit(e, waits=waits):
                for s, v in waits:
                    e.wait_ge(s, v)
            self.ops[eng].append(emit)

    def wait_all(self, eng, toks):
        waits = [(t[1], t[2]) for t in toks if t is not None]

        def emit(e, waits=waits):
            for s, v in waits:
                e.wait_ge(s, v)
        self.ops[eng].append(emit)

    def emit_all(self, block):
        for name in self.ENGS:
            lst = self.ops[name]

            def body(e, lst=lst):
                for f in lst:
                    f(e)
            getattr(block, name)(body)


ARENA_BYTES = 204 * 1024
P_RES = 8 * 1024


def _dsize(dt):
    return 4 if dt in (F32, I32) else 2


def build(S, debug=()):
    NB = S // 512
    NT = S // 128
    SH = S // 2
    NQB = SH // 512
    SC = S // NCORES
    NTC = SC // 128
    NBC = SC // 512
    VW = 130
    nc = bass.Bass("TRN2", target_bir_lowering=False)

    def din(name, shape, dt=F32):
        return nc.dram_tensor(name, shape, dt, kind="ExternalInput").ap()

    def dout(name, shape, dt=F32):
        return nc.dram_tensor(name, shape, dt, kind="ExternalOutput").ap()

    xs = din("xs", [S, D])
    posd = din("pos", [NT, 128], I32)
    wa = din("wa", [D, 1024])
    g1d = din("g1", [128, 8])
    lbpd = din("lbp", [128, 2])
    identd = din("ident", [128, 128])
    blk1d = din("blk1", [128, 128])
    onesd = din("ones", [128, 128])
    trimd = din("trim", [128, 512])
    scmd = din("scm", [128, 512])
    dld = din("dl", [1, 256])
    sgaind = din("sgain", [1, 128])
    xod = din("xo", [SC, D])
    wgd = din("wg", [D, 512])
    wod = din("wo", [D, D])
    hgnd = din("hgn", [128, 1])
    g2d = din("g2", [1, D])
    gfd = din("gf", [1, D])
    wrd = din("wr", [D, 36])
    brd = din("br", [1, 36])
    wgated = din("wgate", [N_EXPERTS, D, D_EXPERT])
    wupd = din("wup", [N_EXPERTS, D, D_EXPERT])
    wdownd = din("wdown", [N_EXPERTS, D_EXPERT, D])
    outd = dout("out", [SC, D])

    hg_part = nc.dram_tensor("hg_part", [128, S], BF16)
    da_part = nc.dram_tensor("da_part", [128, SH], BF16)
    hg_all = nc.dram_tensor("hg_all", [NCORES * 128, S], BF16)
    da_all = nc.dram_tensor("da_all", [NCORES * 128, SH], BF16)

    final_toks = []
    with ExitStack() as st:
        P = Prog(nc, st)
        arena = st.enter_context(nc.sbuf_tensor("arena", [128, ARENA_BYTES // 2], BF16))

        class Region:
            def __init__(self, lo, hi, down):
                self.lo, self.hi, self.down = lo, hi, down
                self.pos = hi if down else lo

        def carve(reg, shape, dt):
            npart = shape[0]
            nel = 1
            for d_ in shape[1:]:
                nel *= d_
            nbytes = (nel * _dsize(dt) + 63) // 64 * 64
            if reg.down:
                reg.pos -= nbytes
                start = reg.pos
                assert start >= reg.lo, "arena overflow (down)"
            else:
                start = reg.pos
                reg.pos += nbytes
                assert reg.pos <= reg.hi, "arena overflow (up)"
            ap = arena[0:npart, start // 2:(start + nbytes) // 2]
            if dt != BF16:
                ap = ap.bitcast(dt)
            ap = ap[:, 0:nel]
            if len(shape) == 3:
                ap = ap.rearrange("p (a b) -> p a b", a=shape[1])
            return ap

        regP = Region(0, P_RES, False)

        pair_t = [st.enter_context(nc.psum_tensor(f"bkp{i}", [128, 2, 512], F32)) for i in range(4)]
        bkB = [Buf(f"bk{i}", excl=True) for i in range(8)]

        def bk32(i):
            return pair_t[i // 2][:, i % 2, :]

        def bk16(i):
            return pair_t[i // 2][:, i % 2, :].bitcast(BF16)

        sync_sems = {}

        def dsem(name):
            if name not in sync_sems:
                sync_sems[name] = P.new_dma_sem("d_" + name)
            return sync_sems[name]

        def stat_rstd(stt, Bst, n, dim, eps):
            P.op("vector", lambda e: e.tensor_scalar(out=stt[:, n:2 * n], in0=stt[:, 0:n], scalar1=1.0 / dim, scalar2=eps, op0=ALU.mult, op1=ALU.add),
                 reads=[Bst], writes=[Bst])
            P.op("scalar", lambda e: e.activation(out=stt[:, n:2 * n], in_=stt[:, n:2 * n], func=AF.Ln), reads=[Bst], writes=[Bst])
            P.op("scalar", lambda e: e.activation(out=stt[:, n:2 * n], in_=stt[:, n:2 * n], func=AF.Exp, scale=-0.5), reads=[Bst], writes=[Bst])

        ident_f = carve(regP, [128, 128], F32)
        ident_b = carve(regP, [128, 128], BF16)
        blk1_b = carve(regP, [128, 128], BF16)
        ones_b = carve(regP, [128, 128], BF16)
        g1 = carve(regP, [128, 8], F32)
        B_ident_f, B_ident_b, B_blk1, B_onesb, B_g1 = [Buf() for _ in range(5)]
        P.dma("sync", dsem("c0"), ident_f, identd, writes=[B_ident_f])
        P.dma("gpsimd", dsem("c1"), ident_b, identd, writes=[B_ident_b])
        P.dma("gpsimd", dsem("c3"), blk1_b, blk1d, writes=[B_blk1])
        P.dma("gpsimd", dsem("c2"), ones_b, onesd, writes=[B_onesb])
        P.dma("sync", dsem("c6"), g1, g1d, writes=[B_g1])

        regA = Region(P_RES, ARENA_BYTES, True)
        regA1 = Region(P_RES, ARENA_BYTES, False)
        K_sb = carve(regA, [128, S], BF16)
        V_sb = carve(regA, [128, NT, VW], BF16)
        Q_sb = carve(regA, [128, SH], BF16)
        cos8 = carve(regA, [128, NT * 8], BF16)
        sin8 = carve(regA, [128, NT * 8], BF16)
        nsin8 = carve(regA, [128, NT * 8], BF16)
        mx = carve(regA, [128, 4], F32)
        lbp = carve(regA, [128, 2], F32)
        lbt = carve(regA, [128, 4], F32)
        Sseq = [carve(regA, [128, 9, 128], F32) for _ in range(2)]
        B_K = [Buf() for _ in range(NB)]
        B_V = [Buf() for _ in range(NB)]
        B_Q = [Buf() for _ in range(NQB)]
        B_Vones, B_lbp, B_lbt, B_mx, B_cos, B_sin = [Buf() for _ in range(6)]
        B_Sseq = [Buf(), Buf()]

        regA0 = Region(regA.hi - S * 2, regA.hi, False)
        posi = carve(regA0, [NT, 128], I32)
        posf = carve(regA0, [NT, 128], F32)
        posT = carve(regA0, [128, NT], F32)
        ang = carve(regA0, [128, NT * 8], F32)
        rr = carve(regA0, [128, NT * 8], F32)
        ki = carve(regA0, [128, NT * 8], I32)
        B_pos, B_posT, B_ang, B_rr, B_ki = [Buf() for _ in range(5)]

        P.dma("sync", dsem("c7"), lbp, lbpd, writes=[B_lbp])
        P.op("vector", lambda e: e.tensor_sub(out=lbt[:, 0:1], in0=lbp[:, 1:2], in1=lbp[:, 0:1]), reads=[B_lbp], writes=[B_lbt])
        P.op("scalar", lambda e: e.activation(out=lbt[:, 1:2], in_=lbt[:, 0:1], func=AF.Exp), reads=[B_lbt], writes=[B_lbt])
        P.op("vector", lambda e: e.tensor_scalar_add(out=lbt[:, 1:2], in0=lbt[:, 1:2], scalar1=1.0), reads=[B_lbt], writes=[B_lbt])
        P.op("vector", lambda e: e.reciprocal(out=lbt[:, 2:3], in_=lbt[:, 1:2]), reads=[B_lbt], writes=[B_lbt])
        P.op("vector", lambda e: e.tensor_scalar_mul(out=lbt[:, 3:4], in0=lbt[:, 2:3], scalar1=-1.0), reads=[B_lbt], writes=[B_lbt])
        oml = lbt[:, 2:3]
        noml = lbt[:, 3:4]
        P.op("gpsimd", lambda e: e.memset(V_sb[:, :, 128:130], 1.0), writes=[B_Vones])
        P.op("gpsimd", lambda e: e.memset(mx, 0.0), writes=[B_mx])
        P.op("gpsimd", lambda e: e.memset(Sseq[1][:, 8, :], 0.0), writes=[B_Sseq[1]])

        wa_b = carve(regA1, [128, 8, 1024], BF16)
        xbuf = [carve(regA1, [128, 2, 1024], F32) for _ in range(2)]
        xn = carve(regA1, [128, 2, 1024], BF16)
        xnT = carve(regA1, [128, 8, 512], BF16)
        junk = carve(regA1, [128, 1024], BF16)
        trim = carve(regA1, [128, 512], F32)
        scm = carve(regA1, [128, 512], F32)
        tabC = [carve(regA1, [128, 4, 128], BF16) for _ in range(2)]
        tabS = [carve(regA1, [128, 4, 128], BF16) for _ in range(2)]
        CS = [carve(regA1, [128, 2, 512], BF16) for _ in range(2)]
        stat = [carve(regA1, [128, 8], F32) for _ in range(2)]
        qT_f = carve(regA1, [128, 512], F32)
        e1 = carve(regA1, [128, 512], F32)
        sgn = carve(regA1, [128, 512], F32)
        lf = carve(regA1, [128, 512], F32)
        bcs = carve(regA1, [128, 512], F32)
        eb = carve(regA1, [128, 512], F32)
        enb = carve(regA1, [128, 512], F32)
        qt_b = carve(regA1, [128, 512], BF16)
        kt_b = carve(regA1, [128, 512], BF16)
        kt_tok = carve(regA1, [128, 4, 128], BF16)
        v_hg = carve(regA1, [128, 4, 128], BF16)
        S_bf = carve(regA1, [128, 8, 128], BF16)
        Ttmp = carve(regA1, [128, 128], F32)
        PT_b = carve(regA1, [128, 512], BF16)
        o_bf = carve(regA1, [128, 4, 128], BF16)
        oT_sb = [carve(regA1, [128, 4, 128], BF16) for _ in range(2)]
        t1, t2, sqk = e1, lf, PT_b
        (B_wa, B_xn, B_xnT, B_junk, B_trim, B_scm, B_qT, B_e1, B_sgn, B_lf, B_bcs, B_eb, B_enb, B_qt, B_kt, B_kttok, B_vhg,
         B_Sbf, B_T, B_PT, B_obf) = [Buf() for _ in range(21)]
        B_t1, B_t2, B_sqk = B_e1, B_lf, B_PT
        B_xbuf, B_tab, B_CS, B_stat, B_oT = [[Buf(), Buf()] for _ in range(5)]
        P.dma("sync", dsem("c4"), trim, trimd, writes=[B_trim])
        P.dma("sync", dsem("c5"), scm, scmd, writes=[B_scm])

        wav = wa.rearrange("(k p) c -> p k c", p=128)
        for r4 in range(4):
            sl = r4 % 2
            P.dma("sync", dsem(f"x{sl}"), xbuf[sl], wav[:, 2 * r4:2 * r4 + 2, :], writes=[B_xbuf[sl]])
            for kk in range(2):
                k = 2 * r4 + kk
                P.op("vector",
                     lambda e, sl=sl, kk=kk, k=k: e.tensor_scalar(out=wa_b[:, k, :], in0=xbuf[sl][:, kk, :], scalar1=g1[:, k:k + 1], scalar2=None, op0=ALU.mult),
                     reads=[B_xbuf[sl], B_g1], writes=[B_wa])

        P.dma("sync", dsem("c8"), posi, posd, writes=[B_pos])
        P.op("vector", lambda e: e.tensor_copy(out=posf, in_=posi), reads=[B_pos], writes=[B_pos])
        P.op("tensor", lambda e: e.transpose(out=bk32(0)[:, 0:NT], in_=posf, identity=ident_f[0:NT, 0:NT]),
             reads=[B_pos, B_ident_f], writes=[bkB[0]])
        P.op("vector", lambda e: e.tensor_copy(out=posT, in_=bk32(0)[:, 0:NT]), reads=[bkB[0]], writes=[B_posT])
        ang3 = ang.rearrange("p (t i) -> p t i", i=8)
        for i in range(8):
            invf = float(np.float32(ROPE_THETA) ** np.float32(-(2.0 * i) / 16.0))
            P.op("vector", lambda e, i=i, invf=invf: e.tensor_scalar(out=ang3[:, :, i], in0=posT, scalar1=invf, scalar2=None, op0=ALU.mult),
                 reads=[B_posT], writes=[B_ang])

        def sin_of(dst, B_dst, shift):
            P.op("vector", lambda e: e.tensor_scalar(out=rr, in0=ang, scalar1=shift, scalar2=1.0 / TWO_PI, op0=ALU.add, op1=ALU.mult),
                 reads=[B_ang], writes=[B_rr])
            P.op("vector", lambda e: e.tensor_copy(out=ki, in_=rr), reads=[B_rr], writes=[B_ki])
            P.op("vector", lambda e: e.tensor_copy(out=rr, in_=ki), reads=[B_ki], writes=[B_rr])
            P.op("vector", lambda e: e.tensor_scalar(out=rr, in0=rr, scalar1=-TWO_PI, scalar2=shift, op0=ALU.mult, op1=ALU.add),
                 reads=[B_rr], writes=[B_rr])
            P.op("vector", lambda e: e.tensor_add(out=rr, in0=rr, in1=ang), reads=[B_rr, B_ang], writes=[B_rr])
            P.op("vector", lambda e: e.tensor_scalar(out=rr, in0=rr, scalar1=-PI, scalar2=PI, op0=ALU.max, op1=ALU.min),
                 reads=[B_rr], writes=[B_rr])
            P.op("scalar", lambda e: e.activation(out=dst, in_=rr, func=AF.Sin), reads=[B_rr], writes=[B_dst])

        sin_of(cos8, B_cos, PI / 2.0)
        sin_of(sin8, B_sin, 0.0)
        P.op("vector", lambda e: e.tensor_scalar_mul(out=nsin8, in0=sin8, scalar1=-1.0), reads=[B_sin], writes=[B_sin])
        cos3 = cos8.rearrange("p (t i) -> p t i", i=8)
        sin3 = sin8.rearrange("p (t i) -> p t i", i=8)
        nsin3 = nsin8.rearrange("p (t i) -> p t i", i=8)
        for i in range(2):
            P.op("gpsimd", lambda e, i=i: e.memset(tabC[i], 1.0), writes=[B_tab[i]])
            P.op("gpsimd", lambda e, i=i: e.memset(tabS[i], 0.0), writes=[B_tab[i]])
        P.barrier(sync_sems.values())

        xsv = xs.rearrange("(g t p) d -> g p t d", p=128, t=2)

        def load_x(g):
            P.dma("sync", dsem(f"x{g % 2}"), xbuf[g % 2], xsv[g], writes=[B_xbuf[g % 2]])

        qt2 = [qt_b, carve(regA1, [128, 512], BF16)]
        kt2 = [kt_b, carve(regA1, [128, 512], BF16)]
        kttok2 = [kt_tok, carve(regA1, [128, 4, 128], BF16)]
        vhg2 = [v_hg, carve(regA1, [128, 4, 128], BF16)]
        ebl2 = [carve(regA1, [128, 8], F32) for _ in range(2)]
        sqk2 = carve(regA1, [128, 512], BF16)
        B_sqk2 = Buf()
        B_qt2, B_kt2, B_kttok2, B_vhg2, B_ebl2 = [[Buf(), Buf()] for _ in range(5)]
        assert regA1.pos <= regA.pos, f"phase A1 does not fit: {regA1.pos} > {regA.pos}"

        def front_stages(b):
            pb_ = b % 2
            stt = stat[pb_]
            Bst = B_stat[pb_]
            qtb, ktb, kttok, vhg, ebl = qt2[pb_], kt2[pb_], kttok2[pb_], vhg2[pb_], ebl2[pb_]
            Bqt, Bkt, Bkttok, Bvhg, Bebl = B_qt2[pb_], B_kt2[pb_], B_kttok2[pb_], B_vhg2[pb_], B_ebl2[pb_]
            xT = xnT
            BxT = B_xnT
            tb = pb_

            def xpart(hb):
                g = 2 * b + hb
                if g + 1 < 2 * NB:
                    load_x(g + 1)
                xb = xbuf[g % 2]
                Bx = B_xbuf[g % 2]
                for tt in range(2):
                    t = 2 * hb + tt
                    P.op("scalar", lambda e, tt=tt, t=t: e.activation(out=junk, in_=xb[:, tt, :], func=AF.Square, accum_out=stt[:, t:t + 1]),
                         reads=[Bx], writes=[B_junk, Bst])
                P.op("vector", lambda e: e.tensor_scalar(out=stt[:, 4 + 2 * hb:6 + 2 * hb], in0=stt[:, 2 * hb:2 * hb + 2], scalar1=1.0 / D, scalar2=1e-6, op0=ALU.mult, op1=ALU.add),
                     reads=[Bst], writes=[Bst])
                P.op("scalar", lambda e: e.activation(out=stt[:, 4 + 2 * hb:6 + 2 * hb], in_=stt[:, 4 + 2 * hb:6 + 2 * hb], func=AF.Ln), reads=[Bst], writes=[Bst])
                P.op("scalar", lambda e: e.activation(out=stt[:, 4 + 2 * hb:6 + 2 * hb], in_=stt[:, 4 + 2 * hb:6 + 2 * hb], func=AF.Exp, scale=-0.5), reads=[Bst], writes=[Bst])
                for tt in range(2):
                    t = 2 * hb + tt
                    if tt == 0:
                        P.op("scalar", lambda e, tt=tt, t=t: e.activation(out=xn[:, tt, :], in_=xb[:, tt, :], func=AF.Copy, scale=stt[:, 4 + t:5 + t]),
                             reads=[Bx, Bst], writes=[B_xn])
                    else:
                        P.op("vector", lambda e, tt=tt, t=t: e.tensor_scalar(out=xn[:, tt, :], in0=xb[:, tt, :], scalar1=stt[:, 4 + t:5 + t], scalar2=None, op0=ALU.mult),
                             reads=[Bx, Bst], writes=[B_xn])
                for kk in range(4):
                    bi = kk % 2
                    pv = bk16(bi).rearrange("p (a c) -> p a c", a=2)
                    fns = []
                    for a_ in range(2):
                        k = 2 * kk + a_
                        for tt in range(2):
                            fns.append(lambda e, a_=a_, k=k, tt=tt, pv=pv: e.transpose(out=pv[:, a_, tt * 128:(tt + 1) * 128], in_=xn[:, tt, k * 128:(k + 1) * 128], identity=ident_b))
                    P.group("tensor", fns, reads=[B_xn, B_ident_b], writes=[bkB[bi]])
                    if kk % 2 == 0:
                        P.op("scalar", lambda e, kk=kk, pv=pv: e.copy(out=xnT[:, 2 * kk:2 * kk + 2, hb * 256:(hb + 1) * 256], in_=pv[:, :, 0:256]), reads=[bkB[bi]], writes=[B_xnT])
                    else:
                        P.op("vector", lambda e, kk=kk, pv=pv: e.tensor_copy(out=xnT[:, 2 * kk:2 * kk + 2, hb * 256:(hb + 1) * 256], in_=pv[:, :, 0:256]), reads=[bkB[bi]], writes=[B_xnT])

            def fm_group(g, bank):
                fns = []
                for k in range(8):
                    fns.append(lambda e, k=k: e.matmul(out=bk32(bank), lhsT=wa_b[:, k, g * 128:(g + 1) * 128], rhs=xT[:, k, :], start=(k == 0), stop=(k == 7)))
                P.group("tensor", fns, reads=[B_wa, BxT], writes=[bkB[bank]])

            def f2():
                for (src, c0) in ((cos3, 0), (cos3, 8), (cos3, 64), (cos3, 72)):
                    P.op("gpsimd", lambda e, src=src, c0=c0: e.tensor_copy(out=tabC[tb][:, :, c0:c0 + 8], in_=src[:, 4 * b:4 * b + 4, :]),
                         reads=[B_cos], writes=[B_tab[tb]])
                for (src, c0) in ((nsin3, 0), (sin3, 8), (nsin3, 64), (sin3, 72)):
                    P.op("gpsimd", lambda e, src=src, c0=c0: e.tensor_copy(out=tabS[tb][:, :, c0:c0 + 8], in_=src[:, 4 * b:4 * b + 4, :]),
                         reads=[B_sin], writes=[B_tab[tb]])
                pv = bk16(2).rearrange("p (a c) -> p a c", a=2)
                fns = []
                for a_, tab in ((0, tabC[tb]), (1, tabS[tb])):
                    for t in range(4):
                        fns.append(lambda e, a_=a_, tab=tab, t=t, pv=pv: e.transpose(out=pv[:, a_, t * 128:(t + 1) * 128], in_=tab[:, t, :], identity=ident_b))
                P.group("tensor", fns, reads=[B_tab[tb], B_ident_b], writes=[bkB[2]])
                P.op("vector", lambda e, pv=pv: e.tensor_copy(out=CS[tb], in_=pv), reads=[bkB[2]], writes=[B_CS[tb]])
                fm_group(0, 3)
                P.op("scalar", lambda e: e.copy(out=qT_f, in_=bk32(3)), reads=[bkB[3]], writes=[B_qT])
                fm_group(1, 2)
                P.op("scalar", lambda e: e.activation(out=e1, in_=bk32(2), func=AF.Exp), reads=[bkB[2]], writes=[B_e1])
                P.op("scalar", lambda e: e.activation(out=sgn, in_=e1, func=AF.Ln, bias=1.0), reads=[B_e1], writes=[B_sgn])
                P.op("scalar", lambda e: e.activation(out=sgn, in_=sgn, func=AF.Exp, scale=-1.0), reads=[B_sgn], writes=[B_sgn])
                P.op("scalar", lambda e: e.activation(out=lf, in_=sgn, func=AF.Ln, scale=noml, bias=1.0), reads=[B_sgn, B_lbt], writes=[B_lf])

            def f3():
                P.op("vector", lambda e: e.tensor_tensor_scan(out=bcs, data0=scm, data1=lf, initial=0.0, op0=ALU.mult, op1=ALU.add),
                     reads=[B_scm, B_lf], writes=[B_bcs])
                P.op("scalar", lambda e: e.activation(out=eb, in_=bcs, func=AF.Exp), reads=[B_bcs], writes=[B_eb])
                P.op("scalar", lambda e: e.activation(out=enb, in_=bcs, func=AF.Exp, scale=-1.0), reads=[B_bcs], writes=[B_enb])
                P.op("vector", lambda e: e.tensor_tensor(out=qtb, in0=qT_f, in1=eb, op=ALU.mult), reads=[B_qT, B_eb], writes=[Bqt])
                P.op("vector", lambda e: e.scalar_tensor_tensor(out=ktb, in0=sgn, scalar=oml, in1=enb, op0=ALU.mult, op1=ALU.mult),
                     reads=[B_sgn, B_enb, B_lbt], writes=[Bkt])
                eb3 = eb.rearrange("p (n c) -> p n c", c=64)
                P.op("vector", lambda e: e.tensor_copy(out=ebl, in_=eb3[:, :, 63]), reads=[B_eb], writes=[Bebl])

            def rot_group(g, dst_ap, B_dst, is_k):
                fm_group(g, 2)
                P.op("vector", lambda e: e.tensor_tensor(out=t1, in0=bk32(2), in1=CS[tb][:, 0, :], op=ALU.mult), reads=[bkB[2], B_CS[tb]], writes=[B_t1])
                fm_group(g + 1, 3)
                P.op("vector", lambda e: e.tensor_tensor(out=t2, in0=bk32(3), in1=CS[tb][:, 1, :], op=ALU.mult), reads=[bkB[3], B_CS[tb]], writes=[B_t2])
                P.op("vector", lambda e: e.tensor_tensor(out=dst_ap, in0=t1, in1=t2, op=ALU.add), reads=[B_t1, B_t2], writes=[B_dst])
                P.op("vector", lambda e: e.tensor_tensor(out=sqk2, in0=dst_ap, in1=dst_ap, op=ALU.mult), reads=[B_dst], writes=[B_sqk2])
                P.op("tensor", lambda e: e.matmul(out=bk32(2), lhsT=blk1_b, rhs=sqk2, start=True, stop=True), reads=[B_blk1, B_sqk2], writes=[bkB[2]])
                col = 0 if is_k else 1
                P.op("vector", lambda e: e.reduce_max(out=mx[:, 2:3], in_=bk32(2), axis=AX.X), reads=[bkB[2]], writes=[B_mx])
                P.op("vector", lambda e: e.tensor_max(out=mx[:, col:col + 1], in0=mx[:, col:col + 1], in1=mx[:, 2:3]), reads=[B_mx], writes=[B_mx])

            def f4():
                rot_group(2, K_sb[:, b * 512:(b + 1) * 512], B_K[b], True)

            def f5():
                if b < NQB:
                    rot_group(4, Q_sb[:, b * 512:(b + 1) * 512], B_Q[b], False)

            def f6():
                for rnd in range(2):
                    pvv = bk32(4).rearrange("p (a c) -> p a c", a=2)
                    fns = []
                    for a_ in range(2):
                        t = 2 * rnd + a_
                        for k in range(8):
                            fns.append(lambda e, a_=a_, t=t, k=k, pvv=pvv: e.matmul(out=pvv[:, a_, :], lhsT=xT[:, k, t * 128:(t + 1) * 128], rhs=wa_b[:, k, 768:1024], start=(k == 0), stop=(k == 7)))
                    P.group("tensor", fns, reads=[B_wa, BxT], writes=[bkB[4]])
                    P.op("scalar", lambda e, rnd=rnd, pvv=pvv: e.copy(out=vhg[:, 2 * rnd:2 * rnd + 2, :], in_=pvv[:, :, 0:128]), reads=[bkB[4]], writes=[Bvhg])
                    for a_ in range(2):
                        P.op("scalar", lambda e, rnd=rnd, pvv=pvv, a_=a_: e.copy(out=V_sb[:, 4 * b + 2 * rnd + a_, 0:128], in_=pvv[:, a_, 128:256]),
                             reads=[bkB[4]], writes=[B_V[b]])
                pk = bk16(4).rearrange("p (a c) -> p a c", a=8)
                fns = [lambda e, t=t, pk=pk: e.transpose(out=pk[:, t, :], in_=ktb[:, t * 128:(t + 1) * 128], identity=ident_b) for t in range(4)]
                P.group("tensor", fns, reads=[Bkt, B_ident_b], writes=[bkB[4]])
                P.op("scalar", lambda e, pk=pk: e.copy(out=kttok, in_=pk[:, 0:4, :]), reads=[bkB[4]], writes=[Bkttok])

            return [lambda: xpart(0), lambda: xpart(1), f2, f3, f4, f5, f6]

        def tail_stages(b):
            pb_ = b % 2
            qtb, ktb, kttok, vhg, ebl = qt2[pb_], kt2[pb_], kttok2[pb_], vhg2[pb_], ebl2[pb_]
            Bqt, Bkt, Bkttok, Bvhg, Bebl = B_qt2[pb_], B_kt2[pb_], B_kttok2[pb_], B_vhg2[pb_], B_ebl2[pb_]
            pA = [bk32(5).rearrange("p (a c) -> p a c", a=4), bk32(6).rearrange("p (a c) -> p a c", a=4)]
            Sc = Sseq[b % 2]
            Sp = Sseq[(b + 1) % 2]
            BSc = B_Sseq[b % 2]
            BSp = B_Sseq[(b + 1) % 2]
            res = {}

            def t0():
                for r in range(2):
                    fns = [lambda e, t=t, r=r: e.matmul(out=pA[r][:, t, :], lhsT=kttok[64 * r:64 * r + 64, t, :], rhs=vhg[64 * r:64 * r + 64, t, :], start=True, stop=True)
                           for t in range(4)]
                    P.group("tensor", fns, reads=[Bkttok, Bvhg], writes=[bkB[5 + r]])

            def rec(n0, n1):
                for n in range(n0, n1):
                    t, r = n // 2, n % 2
                    prev_ap = Sp[:, 8, :] if n == 0 else Sc[:, n, :]
                    rd = [bkB[5 + r], BSp if n == 0 else BSc]
                    P.op("vector", lambda e, t=t, r=r, prev_ap=prev_ap: e.tensor_tensor(out=Ttmp, in0=pA[r][:, t, :], in1=prev_ap, op=ALU.add),
                         reads=rd, writes=[B_T])
                    P.op("vector", lambda e, n=n: e.tensor_scalar(out=Sc[:, n + 1, :], in0=Ttmp, scalar1=ebl[:, n:n + 1], scalar2=None, op0=ALU.mult),
                         reads=[B_T, Bebl], writes=[BSc])

            def t2():
                rec(4, 8)
                P.op("scalar", lambda e: e.copy(out=S_bf[:, 0, :], in_=Sp[:, 8, :]), reads=[BSp], writes=[B_Sbf])
                P.op("scalar", lambda e: e.copy(out=S_bf[:, 1:8, :], in_=Sc[:, 1:8, :]), reads=[BSc], writes=[B_Sbf])

            def t3():
                psc = bk32(7).rearrange("p (a c) -> p a c", a=4)
                fns = [lambda e, t=t: e.matmul(out=psc[:, t, :], lhsT=ktb[:, t * 128:(t + 1) * 128], rhs=qtb[:, t * 128:(t + 1) * 128], start=True, stop=True) for t in range(4)]
                P.group("tensor", fns, reads=[Bkt, Bqt], writes=[bkB[7]])
                P.op("vector", lambda e: e.tensor_tensor(out=PT_b, in0=bk32(7), in1=trim, op=ALU.mult), reads=[bkB[7], B_trim], writes=[B_PT])

            def t4():
                po = bk32(7).rearrange("p (a c) -> p a c", a=4)
                fns = []
                for t in range(4):
                    fns.append(lambda e, t=t: e.matmul(out=po[:, t, :], lhsT=PT_b[:, t * 128:(t + 1) * 128], rhs=vhg[:, t, :], start=True, stop=False))
                    for r in range(2):
                        n = 2 * t + r
                        fns.append(lambda e, t=t, r=r, n=n: e.matmul(out=po[64 * r:64 * r + 64, t, :], lhsT=qtb[:, n * 64:(n + 1) * 64], rhs=S_bf[:, n, :],
                                                                     start=False, stop=(r == 1), skip_group_check=True))
                P.group("tensor", fns, reads=[B_PT, Bvhg, Bqt, B_Sbf], writes=[bkB[7]])
                P.op("scalar", lambda e: e.copy(out=o_bf, in_=po), reads=[bkB[7]], writes=[B_obf])

            def t5():
                poT = bk32(7).rearrange("p (a c) -> p a c", a=4)
                fns = [lambda e, t=t: e.matmul(out=poT[:, t, :], lhsT=o_bf[:, t, :], rhs=ident_b, start=True, stop=True) for t in range(4)]
                P.group("tensor", fns, reads=[B_obf, B_ident_b], writes=[bkB[7]])
                oT = oT_sb[b % 2]
                BoT = B_oT[b % 2]
                P.op("vector", lambda e: e.tensor_copy(out=oT, in_=poT), reads=[bkB[7]], writes=[BoT])
                res["tok"] = P.dma("sync", dsem(f"o{b % 2}"), hg_part[:, b * 512:(b + 1) * 512], oT, reads=[BoT])

            return [t0, lambda: rec(0, 4), t2, t3, t4, t5], res

        load_x(0)
        hg_res = []
        for b in range(NB + 1):
            fs = front_stages(b) if b < NB else []
            if b >= 1:
                ts, res = tail_stages(b - 1)
                hg_res.append(res)
            else:
                ts = []
            import os as _os
            if _os.environ.get("KNOIL"):
                for f_ in ts:
                    f_()
                for f_ in fs:
                    f_()
            else:
                for i in range(max(len(fs), len(ts))):
                    if i < len(fs):
                        fs[i]()
                    if i < len(ts):
                        ts[i]()
        hg_toks = [r["tok"] for r in hg_res]

        B_hgall, B_daall = Buf(), Buf()
        P.custom("gpsimd", lambda e: e.collective_compute("AllGather", ALU.bypass, replica_groups=[list(range(NCORES))],
                                                          ins=[hg_part.ap()], outs=[hg_all.ap()]),
                 dsem("cc_hg"), None, writes=[B_hgall], extra=hg_toks[-2:])
        P.barrier([d for n_, d in sync_sems.items() if not n_.startswith("cc")])

        regA2 = Region(P_RES, regA.pos, False)
        sc1 = carve(regA2, [1, 8], F32)
        dl = carve(regA2, [1, 256], F32)
        dlp = carve(regA2, [1, 128], F32)
        ones_row = carve(regA2, [1, 128], F32)
        bc = carve(regA2, [128, 2], F32)
        sg08 = carve(regA2, [128, 128], F32)
        junk2 = carve(regA2, [128, 128], BF16)
        PTs = [carve(regA2, [128, 2, 512], BF16) for _ in range(3)]
        rs = carve(regA2, [128, 16], F32)
        at1 = carve(regA2, [128, 128], F32)
        ao = carve(regA2, [128, 4, 128], F32)
        aon = carve(regA2, [128, 4, 128], BF16)
        daT = [carve(regA2, [128, 4, 128], BF16) for _ in range(2)]
        B_sc1, B_dl, B_dlp, B_ones, B_bc, B_sg, B_junk2, B_rs, B_at1, B_ao, B_aon = [Buf() for _ in range(11)]
        B_PTs = [Buf() for _ in range(3)]
        B_daT = [Buf(), Buf()]
        P.dma("sync", dsem("c9"), dl, dld, writes=[B_dl])
        P.dma("sync", dsem("c10"), sg08, sgaind.partition_broadcast(128), writes=[B_sg])
        P.op("vector", lambda e: e.tensor_scalar_mul(out=sg08, in0=sg08, scalar1=1.0 - LAMBDA_INIT), reads=[B_sg], writes=[B_sg])
        P.op("gpsimd", lambda e: e.memset(ones_row, 1.0), writes=[B_ones])
        P.op("vector", lambda e: e.tensor_tensor(out=mx[:, 2:3], in0=mx[:, 0:1], in1=mx[:, 1:2], op=ALU.mult), reads=[B_mx], writes=[B_mx])
        P.op("scalar", lambda e: e.activation(out=mx[:, 2:3], in_=mx[:, 2:3], func=AF.Ln), reads=[B_mx], writes=[B_mx])
        P.op("scalar", lambda e: e.activation(out=mx[:, 2:3], in_=mx[:, 2:3], func=AF.Exp, scale=0.5), reads=[B_mx], writes=[B_mx])
        P.op("tensor", lambda e: e.transpose(out=bk32(7)[0:1, 0:128], in_=mx[:, 2:3], identity=ident_f), reads=[B_mx, B_ident_f], writes=[bkB[7]])
        P.op("vector", lambda e: e.reduce_max(out=sc1[:, 0:1], in_=bk32(7)[0:1, 0:128], axis=AX.X), reads=[bkB[7]], writes=[B_sc1])
        P.op("vector", lambda e: e.tensor_scalar_mul(out=sc1[:, 0:1], in0=sc1[:, 0:1], scalar1=-0.125), reads=[B_sc1], writes=[B_sc1])
        dl4 = dl.rearrange("p (a b c) -> p a b c", a=2, b=2)
        dlp2 = dlp.rearrange("p (a c) -> p a c", a=2)
        P.op("vector", lambda e: e.tensor_tensor(out=dlp2, in0=dl4[:, :, 0, :], in1=dl4[:, :, 1, :], op=ALU.mult), reads=[B_dl], writes=[B_dlp])
        P.op("vector", lambda e: e.reduce_sum(out=sc1[:, 2:4], in_=dlp2, axis=AX.X), reads=[B_dlp], writes=[B_sc1])
        P.op("scalar", lambda e: e.activation(out=sc1[:, 2:4], in_=sc1[:, 2:4], func=AF.Exp), reads=[B_sc1], writes=[B_sc1])
        P.op("vector", lambda e: e.tensor_sub(out=sc1[:, 1:2], in0=sc1[:, 2:3], in1=sc1[:, 3:4]), reads=[B_sc1], writes=[B_sc1])
        P.op("vector", lambda e: e.tensor_scalar_add(out=sc1[:, 1:2], in0=sc1[:, 1:2], scalar1=LAMBDA_INIT), reads=[B_sc1], writes=[B_sc1])
        P.op("tensor", lambda e: e.matmul(out=bk32(7)[:, 0:2], lhsT=ones_row, rhs=sc1[:, 0:2], start=True, stop=True), reads=[B_ones, B_sc1], writes=[bkB[7]])
        P.op("vector", lambda e: e.tensor_copy(out=bc, in_=bk32(7)[:, 0:2]), reads=[bkB[7]], writes=[B_bc])

        def acc(m, s_):
            idx = m * 4 + s_
            return bk32(4 + idx // 3)[:, (idx % 3) * VW:(idx % 3) * VW + VW], bkB[4 + idx // 3]

        def qk(qb, kt):
            pb = kt % 2
            for m in range(2):
                P.op("tensor", lambda e, m=m, pb=pb: e.matmul(out=bk32(2 * pb + m), lhsT=K_sb[64 * m:64 * m + 64, kt * 128:(kt + 1) * 128],
                                                              rhs=Q_sb[64 * m:64 * m + 64, qb * 512:(qb + 1) * 512], start=True, stop=True),
                     reads=[B_K[kt // 4], B_Q[qb]], writes=[bkB[2 * pb + m]])

        def expo(kt):
            pb = kt % 2
            x3 = kt % 3
            P.op("scalar", lambda e, pb=pb, x3=x3: e.activation(out=PTs[x3], in_=pair_t[pb][:, :, :], func=AF.Exp, bias=bc[:, 0:1], scale=0.125),
                 reads=[bkB[2 * pb], bkB[2 * pb + 1], B_bc], writes=[B_PTs[x3]])

        def pv_mm(kt):
            x3 = kt % 3
            fns = []
            for m in range(2):
                for s_ in range(4):
                    ap_, _ = acc(m, s_)
                    fns.append(lambda e, m=m, s_=s_, ap_=ap_: e.matmul(out=ap_, lhsT=PTs[x3][:, m, s_ * 128:(s_ + 1) * 128], rhs=V_sb[:, kt, :],
                                                                      start=False, stop=(kt == NT - 1), skip_group_check=True))
            P.group("tensor", fns, reads=[B_PTs[x3], B_V[kt // 4], B_Vones], writes=[bkB[4], bkB[5], bkB[6]])

        def a2_block(qb):
            for i in range(3):
                P.op("vector", lambda e, i=i: e.memset(bk32(4 + i), 0.0), writes=[bkB[4 + i]])
            for kt in range(NT + 1):
                if kt < NT:
                    qk(qb, kt)
                    expo(kt)
                if kt > 0:
                    pv_mm(kt - 1)
            for m in range(2):
                for s_ in range(4):
                    ap_, bb = acc(m, s_)
                    P.op("vector", lambda e, m=m, s_=s_, ap_=ap_: e.reciprocal(out=rs[:, m * 4 + s_:m * 4 + s_ + 1], in_=ap_[:, 128:129]), reads=[bb], writes=[B_rs])
            P.op("vector", lambda e: e.tensor_scalar(out=rs[:, 4:8], in0=rs[:, 4:8], scalar1=bc[:, 1:2], scalar2=None, op0=ALU.mult), reads=[B_rs, B_bc], writes=[B_rs])
            for s_ in range(4):
                a0, b0 = acc(0, s_)
                a1, b1 = acc(1, s_)
                P.op("vector", lambda e, s_=s_, a1=a1: e.tensor_scalar(out=at1, in0=a1[:, 0:128], scalar1=rs[:, 4 + s_:5 + s_], scalar2=None, op0=ALU.mult),
                     reads=[b1, B_rs], writes=[B_at1])
                P.op("vector", lambda e, s_=s_, a0=a0: e.scalar_tensor_tensor(out=ao[:, s_, :], in0=a0[:, 0:128], scalar=rs[:, s_:s_ + 1], in1=at1, op0=ALU.mult, op1=ALU.subtract),
                     reads=[b0, B_rs, B_at1], writes=[B_ao])
                P.op("scalar", lambda e, s_=s_: e.activation(out=junk2, in_=ao[:, s_, :], func=AF.Square, accum_out=rs[:, 8 + s_:9 + s_]),
                     reads=[B_ao], writes=[B_junk2, B_rs])
            P.op("vector", lambda e: e.tensor_scalar(out=rs[:, 12:16], in0=rs[:, 8:12], scalar1=1.0 / 128.0, scalar2=1e-5, op0=ALU.mult, op1=ALU.add), reads=[B_rs], writes=[B_rs])
            P.op("scalar", lambda e: e.activation(out=rs[:, 12:16], in_=rs[:, 12:16], func=AF.Ln), reads=[B_rs], writes=[B_rs])
            P.op("scalar", lambda e: e.activation(out=rs[:, 12:16], in_=rs[:, 12:16], func=AF.Exp, scale=-0.5), reads=[B_rs], writes=[B_rs])
            for s_ in range(4):
                P.op("vector", lambda e, s_=s_: e.scalar_tensor_tensor(out=aon[:, s_, :], in0=ao[:, s_, :], scalar=rs[:, 12 + s_:13 + s_], in1=sg08, op0=ALU.mult, op1=ALU.mult),
                     reads=[B_ao, B_rs, B_sg], writes=[B_aon])
            pdt = bk32(7).rearrange("p (a c) -> p a c", a=4)
            fns = [lambda e, s_=s_: e.matmul(out=pdt[:, s_, :], lhsT=aon[:, s_, :], rhs=ident_b, start=True, stop=True) for s_ in range(4)]
            P.group("tensor", fns, reads=[B_aon, B_ident_b], writes=[bkB[7]])
            dT = daT[qb % 2]
            P.op("vector", lambda e: e.tensor_copy(out=dT, in_=pdt), reads=[bkB[7]], writes=[B_daT[qb % 2]])
            return P.dma("sync", dsem(f"da{qb % 2}"), da_part[:, qb * 512:(qb + 1) * 512], dT, reads=[B_daT[qb % 2]])

        da_toks = [a2_block(qb) for qb in range(NQB)]

        P.custom("gpsimd", lambda e: e.collective_compute("AllGather", ALU.bypass, replica_groups=[list(range(NCORES))],
                                                          ins=[da_part.ap()], outs=[da_all.ap()]),
                 dsem("cc_da"), None, writes=[B_daall], extra=da_toks[-2:])

        if "hg" in debug:
            final_toks.append(P.dma("sync", dsem("dbg3"), dout("dbg_hg", [128, S], BF16), hg_part[:, :], extra=hg_toks[-2:]))
        if "da" in debug:
            final_toks.append(P.dma("sync", dsem("dbg4"), dout("dbg_da", [128, SH], BF16), da_part[:, :], extra=da_toks[-2:]))

        P.barrier([d for n_, d in sync_sems.items() if not n_.startswith("cc")])
        regC = Region(P_RES, ARENA_BYTES, True)
        regC1 = Region(P_RES, ARENA_BYTES, False)
        x1 = carve(regC, [128, NTC, 1024], F32)
        h2T = carve(regC, [128, 8, SC], BF16)
        gates = carve(regC, [128, NTC, 32], F32)
        lg = carve(regC, [128, NTC, 36], F32)
        g2b = carve(regC, [128, 1024], F32)
        gfb = carve(regC, [128, 1024], F32)
        brb = carve(regC, [128, 36], F32)
        wr_f = carve(regC, [128, 8, 36], F32)
        hgn = carve(regC, [128, 1], F32)
        B_x1 = [Buf() for _ in range(NTC)]
        B_h2T = [Buf() for _ in range(NTC)]
        B_gates = [Buf() for _ in range(NTC)]
        B_lg = [Buf() for _ in range(NTC)]
        B_g2b, B_gfb, B_brb, B_wr, B_hgn = [Buf() for _ in range(5)]
        P.dma("sync", dsem("k0"), g2b, g2d.partition_broadcast(128), writes=[B_g2b])
        P.dma("sync", dsem("k1"), gfb, gfd.partition_broadcast(128), writes=[B_gfb])
        P.dma("sync", dsem("k2"), brb, brd.partition_broadcast(128), writes=[B_brb])
        P.dma("sync", dsem("k3"), wr_f, wrd.rearrange("(k p) c -> p k c", p=128), writes=[B_wr])
        P.dma("sync", dsem("k4"), hgn, hgnd, writes=[B_hgn])

        wo_b = carve(regC1, [128, 8, 1024], BF16)
        wg_b = carve(regC1, [128, 8, 512], BF16)
        xnC = carve(regC1, [128, 4, 1024], BF16)
        xnTC = carve(regC1, [128, 8, 512], BF16)
        mixT = carve(regC1, [128, 8, 512], BF16)
        sgate = carve(regC1, [128, 512], F32)
        hgD = carve(regC1, [128, 4, 512], BF16)
        hgR = carve(regC1, [128, 4, 512], BF16)
        osum = carve(regC1, [128, 512], F32)
        sq = carve(regC1, [128, 512], BF16)
        rst = carve(regC1, [128, 512], F32)
        h2g = carve(regC1, [128, 1024], F32)
        h2Tf = carve(regC1, [128, 8, 128], F32)
        junkC = carve(regC1, [128, 1024], BF16)
        statC = [carve(regC1, [128, 8], F32) for _ in range(2)]
        stat2 = [carve(regC1, [128, 8], F32) for _ in range(2)]
        rt = carve(regC1, [128, 16], F32)
        rml = carve(regC1, [128, 32], F32)
        rml2 = carve(regC1, [128, 32], F32)
        roh1 = carve(regC1, [128, 32], F32)
        roh2 = carve(regC1, [128, 32], F32)
        rge = carve(regC1, [128, 4], F32)
        rmk = carve(regC1, [128, 4], F32)
        assert regC1.pos <= regC.pos, f"phase C1 does not fit: {regC1.pos} > {regC.pos}"
        (B_wo, B_wgb, B_xnC, B_xnTC, B_sgate, B_osum, B_sq, B_rst, B_h2g, B_h2Tf, B_junkC, B_rt, B_rml, B_rml2, B_roh1, B_roh2,
         B_rge, B_rmk) = [Buf() for _ in range(18)]
        B_mixT = [Buf() for _ in range(8)]
        B_statC, B_stat2 = [[Buf(), Buf()] for _ in range(2)]
        B_hgD, B_hgR = Buf(), Buf()
        P.dma("gpsimd", dsem("k5"), wo_b, wod.rearrange("(k p) c -> p k c", p=128), writes=[B_wo])
        P.dma("gpsimd", dsem("k6"), wg_b, wgd.rearrange("(k p) c -> p k c", p=128), writes=[B_wgb])
        for k in range(8):
            P.op("vector", lambda e, k=k: e.tensor_scalar(out=wg_b[:, k, :], in0=wg_b[:, k, :], scalar1=g1[:, k:k + 1], scalar2=None, op0=ALU.mult),
                 reads=[B_g1, B_wgb], writes=[B_wgb])

        dynreg = {}

        def dyn(e, tb):
            key = id(e)

            def mat(expr, lo, hi):
                return e.snap(e.to_reg(expr), donate=True, min_val=lo, max_val=hi)
            if key not in dynreg:
                pid = mat(e.partition_id(), 0, NCORES - 1)
                j = mat(pid // 4, 0, 1)
                jn = mat((7 - pid) // 4, 0, 1)
                colD = mat(jn * (pid * SC) + j * ((NCORES - 1 - pid) * SC), 0, SH - SC)
                colR = mat(jn * ((NCORES - 1 - pid) * SC) + j * (pid * SC), 0, S - SC)
                rowD = mat(j * 512, 0, 512)
                rowR = mat(jn * 512, 0, 512)
                dynreg[key] = dict(colD=colD, colR=colR, rowD=rowD, rowR=rowR, blk={})
            dct = dynreg[key]
            if tb not in dct["blk"]:
                cD = mat(dct["colD"] + tb * 512, 0, SH - 512)
                cR = mat(dct["colR"] + (SC - (tb + 1) * 512), 0, S - 512)
                dct["blk"][tb] = (cD, cR)
            cD, cR = dct["blk"][tb]
            return cD, cR, dct["rowD"], dct["rowR"]

        xov = xod.rearrange("(b t p) d -> b p t d", p=128, t=4)
        c4 = [0]

        def route_tile(tl):
            g4 = lg[:, tl, 0:4]
            le3 = lg[:, tl, 4:36].rearrange("p (g e) -> p g e", g=4)
            rml3 = rml.rearrange("p (g e) -> p g e", g=4)
            Bl = B_lg[tl]
            P.op("vector", lambda e: e.reduce_max(out=rt[:, 0:1], in_=g4, axis=AX.X), reads=[Bl], writes=[B_rt])
            P.op("vector", lambda e: e.tensor_scalar_mul(out=rt[:, 1:2], in0=rt[:, 0:1], scalar1=-1.0), reads=[B_rt], writes=[B_rt])
            P.op("scalar", lambda e: e.activation(out=rge, in_=g4, func=AF.Exp, bias=rt[:, 1:2], accum_out=rt[:, 2:3]), reads=[Bl, B_rt], writes=[B_rge, B_rt])
            P.op("vector", lambda e: e.reciprocal(out=rt[:, 3:4], in_=rt[:, 2:3]), reads=[B_rt], writes=[B_rt])
            P.op("vector", lambda e: e.tensor_scalar(out=rmk, in0=g4, scalar1=rt[:, 0:1], scalar2=None, op0=ALU.is_equal), reads=[Bl, B_rt], writes=[B_rmk])
            P.op("vector", lambda e: e.tensor_scalar(out=rmk, in0=rmk, scalar1=-1.0, scalar2=1e30, op0=ALU.add, op1=ALU.mult), reads=[B_rmk], writes=[B_rmk])
            for g in range(4):
                P.op("vector", lambda e, g=g: e.tensor_scalar(out=rml3[:, g, :], in0=le3[:, g, :], scalar1=rmk[:, g:g + 1], scalar2=None, op0=ALU.add),
                     reads=[Bl, B_rmk], writes=[B_rml])
            P.op("vector", lambda e: e.reduce_max(out=rt[:, 4:5], in_=rml, axis=AX.X), reads=[B_rml], writes=[B_rt])
            P.op("vector", lambda e: e.tensor_scalar(out=roh1, in0=rml, scalar1=rt[:, 4:5], scalar2=None, op0=ALU.is_equal), reads=[B_rml, B_rt], writes=[B_roh1])
            P.op("vector", lambda e: e.scalar_tensor_tensor(out=rml2, in0=roh1, scalar=-1e30, in1=rml, op0=ALU.mult, op1=ALU.add), reads=[B_roh1, B_rml], writes=[B_rml2])
            P.op("vector", lambda e: e.reduce_max(out=rt[:, 5:6], in_=rml2, axis=AX.X), reads=[B_rml2], writes=[B_rt])
            P.op("vector", lambda e: e.tensor_scalar(out=roh2, in0=rml2, scalar1=rt[:, 5:6], scalar2=None, op0=ALU.is_equal), reads=[B_rml2, B_rt], writes=[B_roh2])
            P.op("vector", lambda e: e.tensor_sub(out=rt[:, 6:7], in0=rt[:, 5:6], in1=rt[:, 4:5]), reads=[B_rt], writes=[B_rt])
            P.op("scalar", lambda e: e.activation(out=rt[:, 6:7], in_=rt[:, 6:7], func=AF.Exp), reads=[B_rt], writes=[B_rt])
            P.op("vector", lambda e: e.tensor_scalar_add(out=rt[:, 7:8], in0=rt[:, 6:7], scalar1=1.0), reads=[B_rt], writes=[B_rt])
            P.op("vector", lambda e: e.reciprocal(out=rt[:, 7:8], in_=rt[:, 7:8]), reads=[B_rt], writes=[B_rt])
            P.op("vector", lambda e: e.tensor_mul(out=rt[:, 8:9], in0=rt[:, 7:8], in1=rt[:, 6:7]), reads=[B_rt], writes=[B_rt])
            P.op("vector", lambda e: e.tensor_scalar(out=rt[:, 9:11], in0=rt[:, 7:9], scalar1=rt[:, 3:4], scalar2=None, op0=ALU.mult), reads=[B_rt], writes=[B_rt])
            P.op("vector", lambda e: e.tensor_scalar(out=roh1, in0=roh1, scalar1=rt[:, 9:10], scalar2=None, op0=ALU.mult), reads=[B_roh1, B_rt], writes=[B_roh1])
            P.op("vector", lambda e: e.scalar_tensor_tensor(out=gates[:, tl, :], in0=roh2, scalar=rt[:, 10:11], in1=roh1, op0=ALU.mult, op1=ALU.add),
                 reads=[B_roh2, B_roh1, B_rt], writes=[B_gates[tl]])

        def c1_block(tb):
            stt = statC[tb % 2]
            Bst = B_statC[tb % 2]
            tiles = [4 * tb + t for t in range(4)]
            P.dma("sync", dsem("xo"), x1[:, 4 * tb:4 * tb + 4, :], xov[tb], writes=[B_x1[tl] for tl in tiles])
            for t in range(4):
                P.op("scalar", lambda e, t=t: e.activation(out=junkC, in_=x1[:, 4 * tb + t, :], func=AF.Square, accum_out=stt[:, t:t + 1]),
                     reads=[B_x1[4 * tb + t]], writes=[B_junkC, Bst])
            stat_rstd(stt, Bst, 4, D, 1e-6)
            for t in range(4):
                if t % 2 == 0:
                    P.op("scalar", lambda e, t=t: e.activation(out=xnC[:, t, :], in_=x1[:, 4 * tb + t, :], func=AF.Copy, scale=stt[:, 4 + t:5 + t]),
                         reads=[B_x1[4 * tb + t], Bst], writes=[B_xnC])
                else:
                    P.op("vector", lambda e, t=t: e.tensor_scalar(out=xnC[:, t, :], in0=x1[:, 4 * tb + t, :], scalar1=stt[:, 4 + t:5 + t], scalar2=None, op0=ALU.mult),
                         reads=[B_x1[4 * tb + t], Bst], writes=[B_xnC])
            for kk in range(4):
                bi = kk % 2
                pv = bk16(bi).rearrange("p (a c) -> p a c", a=2)
                fns = []
                for a in range(2):
                    k = 2 * kk + a
                    for t in range(4):
                        fns.append(lambda e, a=a, k=k, t=t, pv=pv: e.transpose(out=pv[:, a, t * 128:(t + 1) * 128], in_=xnC[:, t, k * 128:(k + 1) * 128], identity=ident_b))
                P.group("tensor", fns, reads=[B_xnC, B_ident_b], writes=[bkB[bi]])
                if kk % 2 == 0:
                    P.op("scalar", lambda e, kk=kk, pv=pv: e.copy(out=xnTC[:, 2 * kk:2 * kk + 2, :], in_=pv), reads=[bkB[bi]], writes=[B_xnTC])
                else:
                    P.op("vector", lambda e, kk=kk, pv=pv: e.tensor_copy(out=xnTC[:, 2 * kk:2 * kk + 2, :], in_=pv), reads=[bkB[bi]], writes=[B_xnTC])
            dq_eng = "sync" if tb < 2 else "gpsimd"
            P.dma(dq_eng, dsem("hd"), hgD,
                  lambda e: hg_all[bass.ds(dyn(e, tb)[2], 512), bass.ds(dyn(e, tb)[0], 512)].rearrange("(h p) c -> p h c", p=128),
                  reads=[B_hgall], writes=[B_hgD])
            P.dma(dq_eng, dsem("hr"), hgR,
                  lambda e: hg_all[bass.ds(dyn(e, tb)[3], 512), bass.ds(dyn(e, tb)[1], 512)].rearrange("(h p) c -> p h c", p=128),
                  reads=[B_hgall], writes=[B_hgR])
            P.dma(dq_eng, dsem("dd"), mixT[:, 4:8, :],
                  lambda e: da_all[bass.ds(dyn(e, tb)[2], 512), bass.ds(dyn(e, tb)[0], 512)].rearrange("(h p) c -> p h c", p=128),
                  reads=[B_daall], writes=B_mixT[4:8])
            for hh in range(4):
                fns = [lambda e, k=k, hh=hh: e.matmul(out=bk32(2), lhsT=wg_b[:, k, hh * 128:(hh + 1) * 128], rhs=xnTC[:, k, :], start=(k == 0), stop=(k == 7)) for k in range(8)]
                P.group("tensor", fns, reads=[B_wgb, B_xnTC], writes=[bkB[2]])
                P.op("scalar", lambda e: e.activation(out=sgate, in_=bk32(2), func=AF.Silu), reads=[bkB[2]], writes=[B_sgate])
                P.op("vector", lambda e, hh=hh: e.tensor_tensor(out=osum, in0=hgD[:, hh, :], in1=hgR[:, hh, ::-1], op=ALU.add), reads=[B_hgD, B_hgR], writes=[B_osum])
                P.op("vector", lambda e: e.tensor_tensor(out=sq, in0=osum, in1=osum, op=ALU.mult), reads=[B_osum], writes=[B_sq])
                P.op("tensor", lambda e: e.matmul(out=bk32(3), lhsT=ones_b, rhs=sq, start=True, stop=True), reads=[B_onesb, B_sq], writes=[bkB[3]])
                P.op("vector", lambda e: e.tensor_scalar(out=rst, in0=bk32(3), scalar1=1.0 / 128.0, scalar2=1e-6, op0=ALU.mult, op1=ALU.add), reads=[bkB[3]], writes=[B_rst])
                P.op("scalar", lambda e: e.activation(out=rst, in_=rst, func=AF.Ln), reads=[B_rst], writes=[B_rst])
                P.op("scalar", lambda e: e.activation(out=rst, in_=rst, func=AF.Exp, scale=-0.5), reads=[B_rst], writes=[B_rst])
                P.op("vector", lambda e: e.scalar_tensor_tensor(out=osum, in0=osum, scalar=hgn[:, 0:1], in1=rst, op0=ALU.mult, op1=ALU.mult), reads=[B_osum, B_hgn, B_rst], writes=[B_osum])
                P.op("vector", lambda e, hh=hh: e.tensor_tensor(out=mixT[:, hh, :], in0=osum, in1=sgate, op=ALU.mult), reads=[B_osum, B_sgate], writes=[B_mixT[hh]])
            for t in range(4):
                for dh in range(2):
                    bank = 4 + (2 * t + dh) % 2
                    fns = [lambda e, f=f, t=t, dh=dh, bank=bank: e.matmul(out=bk32(bank), lhsT=mixT[:, f, t * 128:(t + 1) * 128], rhs=wo_b[:, f, dh * 512:(dh + 1) * 512],
                                                                          start=(f == 0), stop=(f == 7)) for f in range(8)]
                    P.group("tensor", fns, reads=B_mixT + [B_wo], writes=[bkB[bank]])
                    P.op("vector", lambda e, t=t, dh=dh, bank=bank: e.tensor_tensor(out=x1[:, 4 * tb + t, dh * 512:(dh + 1) * 512], in0=bk32(bank),
                                                                                     in1=x1[:, 4 * tb + t, dh * 512:(dh + 1) * 512], op=ALU.add),
                         reads=[bkB[bank], B_x1[4 * tb + t]], writes=[B_x1[4 * tb + t]])
            st2 = stat2[tb % 2]
            Bs2 = B_stat2[tb % 2]
            for t in range(4):
                P.op("scalar", lambda e, t=t: e.activation(out=junkC, in_=x1[:, 4 * tb + t, :], func=AF.Square, accum_out=st2[:, t:t + 1]),
                     reads=[B_x1[4 * tb + t]], writes=[B_junkC, Bs2])
            stat_rstd(st2, Bs2, 4, D, 1e-6)
            pT8 = pair_t[3][:, :, :].rearrange("p a (k c) -> p (a k) c", k=4)
            for t in range(4):
                tl = 4 * tb + t
                P.op("vector", lambda e, t=t, tl=tl: e.scalar_tensor_tensor(out=h2g, in0=x1[:, tl, :], scalar=st2[:, 4 + t:5 + t], in1=g2b, op0=ALU.mult, op1=ALU.mult),
                     reads=[B_x1[tl], Bs2, B_g2b], writes=[B_h2g])
                fns = [lambda e, k=k: e.transpose(out=pT8[:, k, :], in_=h2g[:, k * 128:(k + 1) * 128], identity=ident_f) for k in range(8)]
                P.group("tensor", fns, reads=[B_h2g, B_ident_f], writes=[bkB[6], bkB[7]])
                P.op("scalar", lambda e: e.copy(out=h2Tf, in_=pT8), reads=[bkB[6], bkB[7]], writes=[B_h2Tf])
                P.op("vector", lambda e, tl=tl: e.tensor_copy(out=h2T[:, :, tl * 128:(tl + 1) * 128], in_=pT8), reads=[bkB[6], bkB[7]], writes=[B_h2T[tl]])
                fns = [lambda e, k=k: e.matmul(out=bk32(2)[:, 0:36], lhsT=h2Tf[:, k, :], rhs=wr_f[:, k, :], start=(k == 0), stop=(k == 7)) for k in range(8)]
                P.group("tensor", fns, reads=[B_h2Tf, B_wr], writes=[bkB[2]])
                P.op("vector", lambda e, tl=tl: e.tensor_tensor(out=lg[:, tl, :], in0=bk32(2)[:, 0:36], in1=brb, op=ALU.add), reads=[bkB[2], B_brb], writes=[B_lg[tl]])
                route_tile(tl)

        for tb in range(NBC):
            c1_block(tb)

        if "x1" in debug:
            final_toks.append(P.dma("sync", dsem("dbg5"), dout("dbg_x1", [128, NTC, 1024], F32), x1, reads=B_x1))
            final_toks.append(P.dma("sync", dsem("dbg6"), dout("dbg_gates", [128, NTC, 32], F32), gates, reads=B_gates))
            final_toks.append(P.dma("sync", dsem("dbg7"), dout("dbg_h2T", [128, 8, SC], BF16), h2T, reads=B_h2T))

        P.barrier([d for n_, d in sync_sems.items() if not n_.startswith("cc")])
        regC3 = Region(P_RES, regC.pos, False)
        wge = [carve(regC3, [128, 8, 512], BF16) for _ in range(2)]
        wue = [carve(regC3, [128, 8, 512], BF16) for _ in range(2)]
        wde = [carve(regC3, [128, 4, 1024], BF16) for _ in range(2)]
        aT = [carve(regC3, [128, 4, 512], BF16) for _ in range(2)]
        sgs = [carve(regC3, [128, 512], F32) for _ in range(2)]
        fo = [carve(regC3, [128, 1024], F32) for _ in range(2)]
        junk3 = carve(regC3, [128, 1024], BF16)
        stf = carve(regC3, [128, 2 * NTC], F32)
        B_wge, B_wue, B_wde, B_aT, B_sgs, B_fo = [[Buf(), Buf()] for _ in range(6)]
        B_junk3, B_stf = Buf(), Buf()

        def load_expert(ex):
            sl = ex % 2
            P.dma("gpsimd", dsem(f"wg{sl}"), wge[sl], wgated[ex].rearrange("(k p) c -> p k c", p=128), writes=[B_wge[sl]])
            P.dma("gpsimd", dsem(f"wu{sl}"), wue[sl], wupd[ex].rearrange("(k p) c -> p k c", p=128), writes=[B_wue[sl]])
            P.dma("gpsimd", dsem(f"wd{sl}"), wde[sl], wdownd[ex].rearrange("(k p) c -> p k c", p=128), writes=[B_wde[sl]])

        cnt = [0]

        def expert(ex):
            sl = ex % 2
            if ex + 1 < N_EXPERTS:
                load_expert(ex + 1)
            for tb in range(NBC):
                asl = cnt[0] % 2
                cnt[0] += 1
                for jc in range(4):
                    pb = jc % 2
                    rd = [B_h2T[4 * tb + t] for t in range(4)]
                    fns = [lambda e, k=k, jc=jc, pb=pb, tb=tb: e.matmul(out=bk32(2 * pb), lhsT=wge[sl][:, k, jc * 128:(jc + 1) * 128], rhs=h2T[:, k, tb * 512:(tb + 1) * 512],
                                                                 start=(k == 0), stop=(k == 7)) for k in range(8)]
                    P.group("tensor", fns, reads=[B_wge[sl]] + rd, writes=[bkB[2 * pb]])
                    fns = [lambda e, k=k, jc=jc, pb=pb, tb=tb: e.matmul(out=bk32(2 * pb + 1), lhsT=wue[sl][:, k, jc * 128:(jc + 1) * 128], rhs=h2T[:, k, tb * 512:(tb + 1) * 512],
                                                                 start=(k == 0), stop=(k == 7)) for k in range(8)]
                    P.group("tensor", fns, reads=[B_wue[sl]] + rd, writes=[bkB[2 * pb + 1]])
                    P.op("scalar", lambda e, pb=pb: e.activation(out=sgs[pb], in_=bk32(2 * pb), func=AF.Silu), reads=[bkB[2 * pb]], writes=[B_sgs[pb]])
                    P.op("vector", lambda e, pb=pb, jc=jc, asl=asl: e.tensor_tensor(out=aT[asl][:, jc, :], in0=sgs[pb], in1=bk32(2 * pb + 1), op=ALU.mult),
                         reads=[B_sgs[pb], bkB[2 * pb + 1]], writes=[B_aT[asl]])
                for t in range(4):
                    tl = 4 * tb + t
                    for dh in range(2):
                        bank = 4 + (2 * t + dh) % 4
                        fns = [lambda e, jc=jc, t=t, dh=dh, bank=bank, asl=asl: e.matmul(out=bk32(bank), lhsT=aT[asl][:, jc, t * 128:(t + 1) * 128],
                                                                                       rhs=wde[sl][:, jc, dh * 512:(dh + 1) * 512], start=(jc == 0), stop=(jc == 3)) for jc in range(4)]
                        P.group("tensor", fns, reads=[B_aT[asl], B_wde[sl]], writes=[bkB[bank]])
                        P.op("vector", lambda e, tl=tl, dh=dh, bank=bank: e.scalar_tensor_tensor(out=x1[:, tl, dh * 512:(dh + 1) * 512], in0=bk32(bank), scalar=gates[:, tl, ex:ex + 1],
                                                                                                 in1=x1[:, tl, dh * 512:(dh + 1) * 512], op0=ALU.mult, op1=ALU.add),
                             reads=[bkB[bank], B_gates[tl], B_x1[tl]], writes=[B_x1[tl]])

        load_expert(0)
        for ex in range(N_EXPERTS):
            expert(ex)

        outv = outd.rearrange("(t p) d -> p t d", p=128)
        for tl in range(NTC):
            P.op("scalar", lambda e, tl=tl: e.activation(out=junk3, in_=x1[:, tl, :], func=AF.Square, accum_out=stf[:, tl:tl + 1]), reads=[B_x1[tl]], writes=[B_junk3, B_stf])
        stat_rstd(stf, B_stf, NTC, D, 1e-6)

        def fin_tile(tl):
            sl = tl % 2
            P.op("vector",
                 lambda e: e.scalar_tensor_tensor(out=fo[sl], in0=x1[:, tl, :], scalar=stf[:, NTC + tl:NTC + tl + 1], in1=gfb, op0=ALU.mult, op1=ALU.mult),
                 reads=[B_x1[tl], B_stf, B_gfb], writes=[B_fo[sl]])
            final_toks.append(P.dma("sync", dsem(f"out{sl}"), outv[:, tl, :], fo[sl], reads=[B_fo[sl]]))

        for tl in range(NTC):
            fin_tile(tl)

        P.wait_all("sync", final_toks)
        with nc.Block() as block:
            P.emit_all(block)
    return nc


def _consts():
    ident = np.eye(128, dtype=np.float32)
    blk1 = np.zeros((128, 128), np.float32)
    blk1[:64, :64] = 1.0
    blk1[64:, 64:] = 1.0
    ones = np.ones((128, 128), np.float32)
    s = np.arange(128)[:, None]
    t = np.arange(128)[None, :]
    tri = ((s <= t) & ((s // 64) == (t // 64))).astype(np.float32)
    trim = np.ascontiguousarray(np.tile(tri, (1, 4)))
    scm = np.ones((128, 512), np.float32)
    scm[:, ::64] = 0.0
    return ident, blk1, ones, trim, scm


def make_in_maps(inputs, S):
    f32 = lambda a: np.asarray(a, np.float32)
    x = f32(inputs["x"])[0]
    pos = np.asarray(inputs["positions"], np.int32)[0]
    w_in = f32(inputs["w_in"])[0]
    lbs = f32(inputs["hg_lower_bounds"])
    ident, blk1, ones, trim, scm = _consts()
    SC = S // NCORES
    g1 = np.ascontiguousarray(f32(inputs["norm1_gain"])[0].reshape(8, 128).T)
    perm = np.arange(128)
    for m in range(2):
        for d in range(8):
            perm[m * 64 + d] = m * 64 + d + 8
            perm[m * 64 + d + 8] = m * 64 + d
    shared = {
        "g1": g1, "ident": ident, "blk1": blk1, "ones": ones, "trim": trim, "scm": scm,
        "dl": np.ascontiguousarray(f32(inputs["diff_lambda"])[0].reshape(1, 256)),
        "sgain": np.ascontiguousarray(f32(inputs["diff_subln_gain"])[0].reshape(1, 128)),
        "wg": np.ascontiguousarray(w_in[:, 2048:2560]),
        "wo": np.ascontiguousarray(f32(inputs["w_out"])[0]),
        "hgn": np.ascontiguousarray(f32(inputs["hg_norm_gain"])[0].reshape(128, 1)),
        "g2": np.ascontiguousarray(f32(inputs["norm2_gain"])[0].reshape(1, D)),
        "gf": np.ascontiguousarray(f32(inputs["final_norm_gain"]).reshape(1, D)),
        "wr": np.ascontiguousarray(np.concatenate([f32(inputs["router_group_w"])[0], f32(inputs["router_expert_w"])[0]], axis=1)),
        "br": np.ascontiguousarray(np.concatenate([f32(inputs["router_group_b"])[0], f32(inputs["router_expert_b"])[0]]).reshape(1, 36)),
        "wgate": np.ascontiguousarray(f32(inputs["moe_w_gate"])[0]),
        "wup": np.ascontiguousarray(f32(inputs["moe_w_up"])[0]),
        "wdown": np.ascontiguousarray(f32(inputs["moe_w_down"])[0]),
    }
    in_maps = []
    for c in range(NCORES):
        h, j = c % 4, c // 4
        xs = x[::-1] if j else x
        ps = pos[::-1] if j else pos
        hq = w_in[:, h * 128:(h + 1) * 128]
        hf = w_in[:, 512 * (1 + j) + h * 128: 512 * (1 + j) + (h + 1) * 128]
        hi = w_in[:, 1536 + h * 128:1536 + (h + 1) * 128]
        dq = w_in[:, 2560 + h * 128:2560 + (h + 1) * 128]
        dk = w_in[:, 3072 + h * 128:3072 + (h + 1) * 128]
        dv = w_in[:, 3584 + h * 128:3584 + (h + 1) * 128]
        wa = np.concatenate([hq, hf, dk, dk[:, perm], dq, dq[:, perm], hi, dv], axis=1)
        xo = x[c * SC:(c + 1) * SC]
        if j:
            xo = xo[::-1]
        m = dict(shared)
        m.update({
            "xs": np.ascontiguousarray(xs),
            "pos": np.ascontiguousarray(ps.reshape(S // 128, 128)),
            "wa": np.ascontiguousarray(wa),
            "lbp": np.ascontiguousarray(lbs[j, :, h * 128:(h + 1) * 128].T),
            "xo": np.ascontiguousarray(xo),
        })
        in_maps.append(m)
    return in_maps


def assemble(results):
    outs = []
    for c in range(NCORES):
        o = np.asarray(results[c]["out"], np.float32)
        if c // 4:
            o = o[::-1]
        outs.append(o)
    return np.concatenate(outs, axis=0)[None]


def kernel(**inputs):
    S = int(np.asarray(inputs["x"]).shape[1])
    nc = build(S)
    in_maps = make_in_maps(inputs, S)
    res = run_bass_kernel_spmd(nc, in_maps, core_ids=list(range(NCORES)))
    return np.ascontiguousarray(assemble(res.results)).astype(np.float32)
```
